# Optimizing a Trainium2 kernel written in Bass

```python
import jax, jax.numpy as jnp
from jax import lax
import numpy as np

D_MODEL = 1024
BATCH = 16
SEQ = 2048
DEPTH = 1

HEAD_DIM = 64
ATTN_HEADS = 8
RWKV_HEADS = 8
ATTN_WIDTH = ATTN_HEADS * HEAD_DIM
RWKV_WIDTH = RWKV_HEADS * HEAD_DIM
MIX_WIDTH = ATTN_WIDTH + RWKV_WIDTH
DECAY_LORA = 64
ICLR_LORA = 64
GATE_LORA = 128
RWKV_COLS = 3 * RWKV_WIDTH + DECAY_LORA + ICLR_LORA + GATE_LORA
IN_COLS = 3 * ATTN_WIDTH + RWKV_COLS
DILATED_PATTERNS = ((128, 1), (512, 4), (2048, 16))
ATTN_BLOCK = 128
D_FF = -(-8 * D_MODEL // (3 * 256)) * 256
NORM_EPS = 1e-6
LNX_EPS = 64e-5

kernel_name = 'hymba_dilated_attn_rwkv7_swiglu'


def rms_norm(x, g):
    xf = x.astype(jnp.float32)
    return xf * lax.rsqrt(jnp.mean(xf * xf, axis=-1, keepdims=True) + NORM_EPS) * g.astype(jnp.float32)


def alibi_slopes(n_heads):
    return jnp.exp2(-8.0 * jnp.arange(1, n_heads + 1, dtype=jnp.float32) / n_heads)


def dilated_band(q, k, v, slopes, window, dilation):
    B, S, H, Dh = q.shape
    steps = window // dilation
    bq = ATTN_BLOCK
    assert steps <= bq
    unit = dilation * bq
    s_pad = -(-S // unit) * unit
    n_sub = s_pad // dilation
    n_blk = n_sub // bq

    def to_blocks(t):
        t = jnp.pad(t, ((0, 0), (0, s_pad - S), (0, 0), (0, 0)))
        t = t.reshape(B, n_sub, dilation, H, Dh).transpose(0, 2, 3, 1, 4)
        return t.reshape(B, dilation, H, n_blk, bq, Dh)

    def with_prev(t):
        prev = jnp.pad(t, ((0, 0), (0, 0), (0, 0), (1, 0), (0, 0), (0, 0)))[:, :, :, :-1]
        return jnp.concatenate([prev, t], axis=4)

    qb = to_blocks(q)
    kw = with_prev(to_blocks(k))
    vw = with_prev(to_blocks(v))
    scores = jnp.einsum('bdhnqe,bdhnke->bdhnqk', qb, kw)
    q_idx = jnp.arange(bq)[:, None] + bq
    k_idx = jnp.arange(2 * bq)[None, :]
    diff = q_idx - k_idx
    key_abs = jnp.arange(n_blk)[:, None, None] * bq + k_idx[None] - bq
    valid = (diff >= 0) & (diff <= steps) & (key_abs >= 0)
    bias = -slopes[:, None, None, None] * (diff.astype(jnp.float32) * dilation)
    scores = jnp.where(valid, scores + bias, -jnp.inf)
    m = jnp.max(scores, axis=-1)
    p = jnp.exp(scores - m[..., None])
    den = jnp.sum(p, axis=-1)
    num = jnp.einsum('bdhnqk,bdhnke->bdhnqe', p, vw)

    def from_blocks(t):
        tail = t.shape[5:]
        t = t.reshape((B, dilation, H, n_sub) + tail)
        t = jnp.moveaxis(t, 3, 1)
        return t.reshape((B, s_pad, H) + tail)[:, :S]

    return from_blocks(num), from_blocks(m), from_blocks(den)


def dilated_attention(q, k, v, q_norm_g, k_norm_g):
    B, S, _ = q.shape
    shp = (B, S, ATTN_HEADS, HEAD_DIM)
    q = rms_norm(q.reshape(shp), q_norm_g) * HEAD_DIM ** -0.5
    k = rms_norm(k.reshape(shp), k_norm_g)
    v = v.reshape(shp).astype(jnp.float32)
    slopes = alibi_slopes(ATTN_HEADS)
    nums, maxs, dens = [], [], []
    for window, dilation in DILATED_PATTERNS:
        num, m, den = dilated_band(q, k, v, slopes, window, dilation)
        nums.append(num); maxs.append(m); dens.append(den)
    maxs = jnp.stack(maxs)
    scale = jnp.exp(maxs - jnp.max(maxs, axis=0))
    total_num = jnp.sum(scale[..., None] * jnp.stack(nums), axis=0)
    total_den = jnp.sum(scale * jnp.stack(dens), axis=0)
    return total_num / total_den[..., None]


def wkv7_scan(r, w, k, v, a, b):
    B, S, H, Dh = r.shape

    def step(state, inp):
        r_t, w_t, k_t, v_t, a_t, b_t = inp
        sa = jnp.einsum('bhij,bhj->bhi', state, a_t)
        state = (state * w_t[:, :, None, :] + sa[..., None] * b_t[:, :, None, :]
                 + v_t[..., None] * k_t[:, :, None, :])
        return state, jnp.einsum('bhij,bhj->bhi', state, r_t)

    xs = tuple(jnp.moveaxis(t, 1, 0) for t in (r, w, k, v, a, b))
    state0 = jnp.zeros((B, H, Dh, Dh), jnp.float32)
    _, y = lax.scan(step, state0, xs)
    return jnp.moveaxis(y, 0, 1)


def rwkv7_time_mix(p_rw, mu, w0, w2, a0, a2, g2, k_k, k_a, r_k, lnx_g, lnx_b):
    B, S, _ = p_rw.shape
    p = p_rw.astype(jnp.float32)
    prev = jnp.pad(p, ((0, 0), (1, 0), (0, 0)))[:, :-1]
    xs = p + (prev - p) * mu
    cuts = [RWKV_WIDTH, 2 * RWKV_WIDTH, 3 * RWKV_WIDTH,
            3 * RWKV_WIDTH + DECAY_LORA, 3 * RWKV_WIDTH + DECAY_LORA + ICLR_LORA]
    r, k, v, wl, al, gl = jnp.split(xs, cuts, axis=-1)
    w = -jax.nn.softplus(-(w0 + jnp.tanh(wl) @ w2)) - 0.5
    decay = jnp.exp(-jnp.exp(w))
    a = jax.nn.sigmoid(a0 + al @ a2)
    g = jax.nn.sigmoid(gl) @ g2

    def heads(t):
        return t.reshape(B, S, RWKV_HEADS, HEAD_DIM)

    kk = heads(k * k_k)
    kk = kk / jnp.maximum(jnp.sqrt(jnp.sum(kk * kk, axis=-1, keepdims=True)), 1e-12)
    k = k * (1.0 + (a - 1.0) * k_a)
    r_h, k_h, v_h, w_h, a_h = heads(r), heads(k), heads(v), heads(decay), heads(a)
    y = wkv7_scan(r_h, w_h, k_h, v_h, -kk, kk * a_h)
    mean = jnp.mean(y, axis=-1, keepdims=True)
    var = jnp.mean(jnp.square(y - mean), axis=-1, keepdims=True)
    y = ((y - mean) * lax.rsqrt(var + LNX_EPS)).reshape(B, S, RWKV_WIDTH) * lnx_g + lnx_b
    bonus = jnp.sum(r_h * k_h * r_k, axis=-1, keepdims=True) * v_h
    return (y + bonus.reshape(B, S, RWKV_WIDTH)) * g


def hybrid_layer(x, norm1_g, w_in, q_norm_g, k_norm_g, attn_out_g, rwkv_mu, w0, w2, a0, a2, g2,
                 k_k, k_a, r_k, lnx_g, lnx_b, w_out, norm2_g, w_gate, w_up, w_down):
    B, S, _ = x.shape
    xn = rms_norm(x, norm1_g).astype(x.dtype)
    proj = xn @ w_in
    q, k, v, p_rw = jnp.split(proj, [ATTN_WIDTH, 2 * ATTN_WIDTH, 3 * ATTN_WIDTH], axis=-1)
    attn = dilated_attention(q, k, v, q_norm_g, k_norm_g)
    attn = rms_norm(attn, attn_out_g.reshape(ATTN_HEADS, HEAD_DIM)).reshape(B, S, ATTN_WIDTH)
    rwkv = rwkv7_time_mix(p_rw, rwkv_mu, w0, w2, a0, a2, g2, k_k, k_a, r_k, lnx_g, lnx_b)
    mixed = jnp.concatenate([attn, rwkv], axis=-1).astype(x.dtype)
    x = x + mixed @ w_out
    xn2 = rms_norm(x, norm2_g).astype(x.dtype)
    ffn = (jax.nn.silu(xn2 @ w_gate) * (xn2 @ w_up)) @ w_down
    return x + ffn


def setup_inputs(seed: int = 0) -> dict:
    key = jax.random.key(seed)
    ks = jax.random.split(key, 24)
    L = DEPTH
    f32 = jnp.float32

    def nrm(k, shape, scale):
        return jax.random.normal(k, shape, f32) * scale

    return {
        'x': nrm(ks[0], (BATCH, SEQ, D_MODEL), 1.0),
        'norm1_g': 1.0 + nrm(ks[1], (L, D_MODEL), 0.02),
        'w_in': nrm(ks[2], (L, D_MODEL, IN_COLS), D_MODEL ** -0.5),
        'q_norm_g': 1.0 + nrm(ks[3], (L, HEAD_DIM), 0.02),
        'k_norm_g': 1.0 + nrm(ks[4], (L, HEAD_DIM), 0.02),
        'attn_out_g': 1.0 + nrm(ks[5], (L, ATTN_WIDTH), 0.02),
        'rwkv_mu': jax.random.uniform(ks[6], (L, RWKV_COLS), f32),
        'w0': jax.random.uniform(ks[7], (L, RWKV_WIDTH), f32, minval=-6.0, maxval=1.0),
        'w2': nrm(ks[8], (L, DECAY_LORA, RWKV_WIDTH), 0.5 * DECAY_LORA ** -0.5),
        'a0': nrm(ks[9], (L, RWKV_WIDTH), 0.1),
        'a2': nrm(ks[10], (L, ICLR_LORA, RWKV_WIDTH), ICLR_LORA ** -0.5),
        'g2': nrm(ks[11], (L, GATE_LORA, RWKV_WIDTH), GATE_LORA ** -0.5),
        'k_k': 0.85 + nrm(ks[12], (L, RWKV_WIDTH), 0.02),
        'k_a': 1.0 + nrm(ks[13], (L, RWKV_WIDTH), 0.02),
        'r_k': nrm(ks[14], (L, RWKV_HEADS, HEAD_DIM), 0.1),
        'lnx_g': 1.0 + nrm(ks[15], (L, RWKV_WIDTH), 0.02),
        'lnx_b': nrm(ks[16], (L, RWKV_WIDTH), 0.01),
        'w_out': nrm(ks[17], (L, MIX_WIDTH, D_MODEL), MIX_WIDTH ** -0.5),
        'norm2_g': 1.0 + nrm(ks[18], (L, D_MODEL), 0.02),
        'w_gate': nrm(ks[19], (L, D_MODEL, D_FF), D_MODEL ** -0.5),
        'w_up': nrm(ks[20], (L, D_MODEL, D_FF), D_MODEL ** -0.5),
        'w_down': nrm(ks[21], (L, D_FF, D_MODEL), D_FF ** -0.5),
    }


def reference(x, norm1_g, w_in, q_norm_g, k_norm_g, attn_out_g, rwkv_mu, w0, w2, a0, a2, g2,
              k_k, k_a, r_k, lnx_g, lnx_b, w_out, norm2_g, w_gate, w_up, w_down):
    h = x
    for layer in range(DEPTH):
        h = hybrid_layer(h, norm1_g[layer], w_in[layer], q_norm_g[layer], k_norm_g[layer],
                         attn_out_g[layer], rwkv_mu[layer], w0[layer], w2[layer], a0[layer],
                         a2[layer], g2[layer], k_k[layer], k_a[layer], r_k[layer], lnx_g[layer],
                         lnx_b[layer], w_out[layer], norm2_g[layer], w_gate[layer], w_up[layer],
                         w_down[layer])
    return h
```

```python
import contextlib
import numpy as np
import concourse.bass as bass
import concourse.mybir as mybir
from concourse.bass_utils import run_bass_kernel_spmd

F32 = mybir.dt.float32
BF16 = mybir.dt.bfloat16
ALU = mybir.AluOpType
AF = mybir.ActivationFunctionType
AX = mybir.AxisListType
COMPUTE = ("pe", "act", "dve", "pool")

S_LEN = 2048
D = 1024
DFF = 2816
C0 = float(np.exp(-0.5))


class Sched:
    def __init__(self, nc, stack, n_dma_sems=24):
        self.nc = nc
        self.eng_names = ["pe", "act", "dve", "pool", "sp"]
        self.ops = {e: [] for e in self.eng_names}
        self.sem = {e: stack.enter_context(nc.semaphore("s_" + e)) for e in COMPUTE}
        self.cnt = {e: 0 for e in COMPUTE}
        self.dma_sems = [stack.enter_context(nc.semaphore("s_dma%d" % i)) for i in range(n_dma_sems)]
        self.dma_val = [0] * n_dma_sems
        self.dma_rr = 0
        self.res_w = {}
        self.res_r = {}
        self.waited = {e: {} for e in self.eng_names}

    def _need(self, eng, waits, ev):
        sid, sh, val = ev
        if self.waited[eng].get(sid, 0) >= val:
            return
        cur = waits.get(sid)
        if cur is None or cur[1] < val:
            waits[sid] = (sh, val)

    def op(self, eng, fn, reads=(), writes=(), dma=False):
        waits = {}
        own = None if dma else ("c", eng)
        for k in reads:
            w = self.res_w.get(k)
            if w is not None:
                self._need(eng, waits, w)
            if isinstance(k, tuple) and k[0] == "ps":
                for r in self.res_r.get(k, ()):
                    if r[0] != own:
                        self._need(eng, waits, r)
        for k in writes:
            w = self.res_w.get(k)
            if w is not None and (dma or w[0] != own):
                self._need(eng, waits, w)
            for r in self.res_r.get(k, ()):
                if dma or r[0] != own:
                    self._need(eng, waits, r)
        if dma:
            k = self.dma_rr
            self.dma_rr = (self.dma_rr + 1) % len(self.dma_sems)
            if self.dma_val[k] > 0:
                self._need(eng, waits, (("d", k), self.dma_sems[k], self.dma_val[k]))
            self.dma_val[k] += 16
            ev = (("d", k), self.dma_sems[k], self.dma_val[k])
            inc = 16
        else:
            self.cnt[eng] += 1
            ev = (("c", eng), self.sem[eng], self.cnt[eng])
            inc = 1
        for sid, (sh, val) in waits.items():
            self.waited[eng][sid] = val
        self.ops[eng].append((fn, list(waits.values()), ev[1], inc))
        for k in reads:
            self.res_r.setdefault(k, []).append(ev)
        for k in writes:
            self.res_w[k] = ev
            self.res_r[k] = []
        return ev

    def barrier(self):
        for eng in self.eng_names:
            waits = {}
            for e2 in COMPUTE:
                if self.cnt[e2] > 0 and e2 != eng:
                    self._need(eng, waits, (("c", e2), self.sem[e2], self.cnt[e2]))
            for k, v in enumerate(self.dma_val):
                if v > 0:
                    self._need(eng, waits, (("d", k), self.dma_sems[k], v))
            for sid, (sh, val) in waits.items():
                self.waited[eng][sid] = val
            self.ops[eng].append((None, list(waits.values()), None, 0))
        self.res_w = {}
        self.res_r = {}

    def emit(self):
        sched = self

        def run(engname, engine):
            for fn, waits, sh, inc in sched.ops[engname]:
                for (wsh, val) in waits:
                    engine.wait_ge(wsh, val)
                if fn is not None:
                    fn(engine).then_inc(sh, inc)

        with self.nc.Block() as block:
            @block.tensor
            def _(e):
                run("pe", e)

            @block.scalar
            def _(e):
                run("act", e)

            @block.vector
            def _(e):
                run("dve", e)

            @block.gpsimd
            def _(e):
                run("pool", e)

            @block.sync
            def _(e):
                run("sp", e)


class Arena:
    def __init__(self, t, n):
        self.t, self.n, self.off = t, n, 0

    def reset(self):
        self.off = 0

    def bf(self, n):
        n = (n + 1) // 2 * 2
        a = self.t[:, self.off:self.off + n]
        self.off += n
        assert self.off <= self.n, ("arena overflow", self.off, self.n)
        return a

    def f32(self, n):
        return self.bf(2 * n).bitcast(F32)


NC_ID, NC_BLK, NC_M4, NC_ML4, NC_I2, NC_RST, NC_WN = 0, 128, 256, 768, 1280, 1536, 2048
NCONST = 2048 + 64


def make_consts():
    c = np.zeros((128, NCONST), np.float32)
    c[:, NC_ID:NC_ID + 128] = np.eye(128)
    c[0:64, NC_BLK:NC_BLK + 64] = 1.0
    c[64:128, NC_BLK + 64:NC_BLK + 128] = 1.0
    p = np.arange(128)[:, None]
    f = np.arange(128)[None, :]
    mus = (p < f).astype(np.float32)
    mui = (p <= f).astype(np.float32)
    mls = (p > f).astype(np.float32)
    c[:, NC_M4:NC_M4 + 512] = np.concatenate([mus, mui, mus, mui], 1)
    c[:, NC_ML4:NC_ML4 + 512] = np.concatenate([mls] * 4, 1)
    c[:, NC_I2:NC_I2 + 256] = np.concatenate([np.eye(128)] * 2, 1)
    r = np.ones((128, 512), np.float32)
    r[:, 0::128] = 0.0
    c[:, NC_RST:NC_RST + 512] = r
    c[0:64, NC_WN:NC_WN + 64] = 1.0 / 64
    c[64, NC_WN:NC_WN + 64] = 1e-6
    return c


def make_amask():
    slopes = np.exp2(-8.0 * np.arange(1, 9, dtype=np.float64) / 8)
    ki = np.arange(128)[:, None]
    cc = np.arange(2048)[None, :]
    dl = cc - ki
    m = ((dl >= 0) & (dl <= 128)).astype(np.float64) + ((dl >= 0) & (dl % 4 == 0) & (dl <= 512)) + ((dl >= 0) & (dl % 16 == 0))
    out = np.zeros((8, 128, 2048), np.float32)
    for h in range(8):
        out[h] = m * np.exp(-slopes[h] * np.maximum(dl, 0))
    return out


CV_MU, CV_W0, CV_A0, CV_KK, CV_KA, CV_RK, CV_LG, CV_LB, CV_GQ, CV_GK, CV_GA = 0, 14, 18, 22, 26, 30, 34, 38, 42, 43, 44
NCV = 52


def make_cols(inp):
    cv = np.zeros((128, NCV), np.float32)

    def put(off, v):
        v = np.asarray(v, np.float32).reshape(-1, 128)
        cv[:, off:off + v.shape[0]] = v.T

    put(CV_MU, inp["rwkv_mu"])
    put(CV_W0, inp["w0"])
    put(CV_A0, inp["a0"])
    put(CV_KK, inp["k_k"])
    put(CV_KA, inp["k_a"])
    put(CV_RK, inp["r_k"])
    put(CV_LG, inp["lnx_g"])
    put(CV_LB, inp["lnx_b"])
    put(CV_GQ, np.tile(np.asarray(inp["q_norm_g"], np.float32).reshape(-1), 2))
    put(CV_GK, np.tile(np.asarray(inp["k_norm_g"], np.float32).reshape(-1), 2))
    ga = np.asarray(inp["attn_out_g"], np.float32).reshape(8, 64)
    cv[0:64, CV_GA:CV_GA + 8] = ga.T
    return cv


class StopBuild(Exception):
    pass


def build(NSEQ=2, dbg=False, stop=None):
    try:
        return _build(NSEQ, dbg, stop)
    except StopBuild as e:
        return e.args[0]


def _build(NSEQ=2, dbg=False, stop=None):
    nc = bass.Bass("TRN2", target_bir_lowering=False)
    NT = NSEQ * S_LEN

    def din(name, shape):
        return nc.dram_tensor(name, shape, F32, kind="ExternalInput").ap()

    x = din("x", [NT, D])
    w_in = din("w_in", [D, 3328])
    w_out = din("w_out", [D, D])
    w_gate = din("w_gate", [D, DFF])
    w_up = din("w_up", [D, DFF])
    w_down = din("w_down", [DFF, D])
    g1v = din("norm1_g", [1, D])
    g2v = din("norm2_g", [1, D])
    w2d = din("w2", [64, 512])
    a2d = din("a2", [64, 512])
    g2d = din("g2", [128, 512])
    consts_d = din("consts", [128, NCONST])
    amask_d = din("amask", [8, 128, 2048])
    cols_d = din("cols", [128, NCV])
    out = nc.dram_tensor("out", [NT, D], F32, kind="ExternalOutput").ap()
    if dbg:
        dbg_mix = nc.dram_tensor("dbg_mix", [128, 8 * S_LEN], F32, kind="ExternalOutput").ap()

    with contextlib.ExitStack() as st:
        S = Sched(nc, st)
        T = lambda n, s, d: st.enter_context(nc.sbuf_tensor(n, s, d))
        psb = [st.enter_context(nc.psum_tensor("ps%d" % i, [128, 512], F32)) for i in range(8)]
        rr = {"i": 0}

        def ps(pool=(0, 1, 2, 3, 4, 5, 6, 7)):
            k = pool[rr["i"] % len(pool)]
            rr["i"] += 1
            return k

        ARN = 82000
        arena_t = T("arena", [128, ARN], BF16)
        A = Arena(arena_t, ARN)
        mixT = T("mixT", [128, 8 * S_LEN], BF16)
        mix3 = mixT[:].rearrange("p (c t) -> p c t", t=S_LEN)
        constb = T("constb", [128, NCONST], BF16)
        constf = T("constf", [128, 512], F32)
        cols = T("cols_sb", [128, NCV], F32)
        gq8 = T("gq8", [128, 1], F32)
        identb = constb[:, NC_ID:NC_ID + 128]
        blk = constb[:, NC_BLK:NC_BLK + 128]

        def dma(eng, o, i, reads=(), writes=()):
            S.op(eng, lambda e: e.dma_start(out=o, in_=i), reads, writes, dma=True)

        def mm(o, l, r, start, stop, reads, writes):
            S.op("pe", lambda e: e.matmul(o, lhsT=l, rhs=r, start=start, stop=stop, skip_group_check=True), reads, writes)

        def tr(o, i, reads, writes):
            S.op("pe", lambda e: e.transpose(o, i, identb), reads, writes)

        def act(o, i, func, reads, writes, bias=0.0, scale=1.0, accum=None):
            if accum is None:
                S.op("act", lambda e: e.activation(out=o, in_=i, func=func, bias=bias, scale=scale), reads, writes)
            else:
                S.op("act", lambda e: e.activation(out=o, in_=i, func=func, bias=bias, scale=scale, accum_out=accum), reads, writes)

        def tt(eng, o, a, b, op, reads, writes):
            S.op(eng, lambda e: e.tensor_tensor(out=o, in0=a, in1=b, op=op), reads, writes)

        def ts(eng, o, a, s1, op0, reads, writes, s2=None, op1=None):
            if op1 is None:
                S.op(eng, lambda e: e.tensor_scalar(out=o, in0=a, scalar1=s1, scalar2=None, op0=op0), reads, writes)
            else:
                S.op(eng, lambda e: e.tensor_scalar(out=o, in0=a, scalar1=s1, scalar2=s2, op0=op0, op1=op1), reads, writes)

        def stt(o, a, s, b, op0, op1, reads, writes):
            S.op("dve", lambda e: e.scalar_tensor_tensor(out=o, in0=a, scalar=s, in1=b, op0=op0, op1=op1), reads, writes)

        def cp(eng, o, i, reads, writes):
            if eng == "act":
                act(o, i, AF.Copy, reads, writes)
            else:
                S.op(eng, lambda e: e.tensor_copy(out=o, in_=i), reads, writes)

        def recip(o, i, reads, writes):
            S.op("dve", lambda e: e.reciprocal(out=o, in_=i), reads, writes)

        def P(k):
            return ("ps", k)

        def chk(name):
            if stop == name:
                S.barrier(); S.emit()
                raise StopBuild(nc)

        dma("pool", constb[:], consts_d, writes=["constb"])
        dma("sp", constf[:], consts_d[:, NC_RST:NC_RST + 512], writes=["constf"])
        dma("sp", cols[:], cols_d, writes=["cols"])
        ts("dve", gq8[:], cols[:, CV_GQ:CV_GQ + 1], 0.125, ALU.mult, ["cols"], ["gq8"])

        def rms_to_T(xt, xt_key, gbc, dstT, dst_key, t0, scr, scr_key, sm, sm_key, xsb, xsb_key):
            act(scr, xt, AF.Square, [xt_key], [scr_key, sm_key], accum=sm[:, 0:1])
            ts("dve", sm[:, 1:2], sm[:, 0:1], 1.0 / D, ALU.mult, [sm_key], [sm_key], s2=1e-6, op1=ALU.add)
            act(sm[:, 2:3], sm[:, 1:2], AF.Sqrt, [sm_key], [sm_key])
            recip(sm[:, 3:4], sm[:, 2:3], [sm_key], [sm_key])
            stt(xsb, xt, sm[:, 3:4], gbc, ALU.mult, ALU.mult, [xt_key, sm_key, "gbc"], [xsb_key])
            k = ps()
            pv = psb[k][:].bitcast(BF16)
            for kc in range(8):
                tr(pv[:, kc * 128:(kc + 1) * 128], xsb[:, kc * 128:(kc + 1) * 128], [xsb_key, "constb"], [P(k)])
            cp("act", dstT[:, :, t0:t0 + 128], pv.rearrange("p (c t) -> p c t", t=128), [P(k)], [dst_key])

        for s in range(NSEQ):
            tok0 = s * S_LEN
            S.barrier()
            A.reset()
            xnT = A.bf(8 * S_LEN)
            xn3 = xnT.rearrange("p (c t) -> p c t", t=S_LEN)
            mark_pa = A.off
            gbc = A.f32(D)
            dma("sp", gbc, g1v[0, :].partition_broadcast(128), writes=["gbc"])
            xts = [A.f32(D) for _ in range(2)]
            scr = A.bf(D)
            sms = [A.f32(4) for _ in range(2)]
            xsbs = [A.bf(D) for _ in range(2)]
            wqkv = A.bf(8 * 1536)
            wq3 = wqkv.rearrange("p (c n) -> p c n", n=1536)
            for kc in range(8):
                dma("pool", wq3[:, kc, :], w_in[kc * 128:(kc + 1) * 128, 0:1536], writes=["wqkv%d" % kc])
            amask = A.bf(8 * 2048)
            am3 = amask.rearrange("p (h c) -> p h c", c=2048)
            for h in range(8):
                dma("pool", am3[:, h, :], amask_d[h], writes=["amask%d" % h])
            for ti in range(16):
                b = ti % 2
                dma("sp", xts[b], x[tok0 + ti * 128: tok0 + (ti + 1) * 128, :], writes=["xt%d" % b])
                rms_to_T(xts[b], "xt%d" % b, gbc, xn3, "xnT", ti * 128, scr, "scr", sms[b], "sm%d" % b, xsbs[b], "xsb%d" % b)
            if stop in ('p0', 'p0d'):
                if stop == 'p0d':
                    S.barrier(); A.reset(); dbgf = A.f32(S_LEN)
                    for cc in range(8):
                        cp("dve", dbgf, mix3[:, cc, :], ["mixT"], ["dbgf"])
                        dma("sp", dbg_mix[:, cc * S_LEN:(cc + 1) * S_LEN], dbgf, reads=["dbgf"])
                S.barrier(); S.emit(); return nc
            WQ = ["wqkv%d" % kc for kc in range(8)]
            vaug = A.bf(16 * 8 * 65)
            va4 = vaug.rearrange("p (j h e) -> p j h e", h=8, e=65)
            S.op("pool", lambda e: e.memset(vaug, 1.0), (), ["vaug"])
            for j in range(16):
                k = ps((0, 1, 2, 3))
                for kc in range(8):
                    mm(psb[k][:], xn3[:, kc, j * 128:(j + 1) * 128], wq3[:, kc, 1024:1536], kc == 0, kc == 7, ["xnT"] + WQ, [P(k)])
                cp("act" if j % 2 else "dve", va4[:, j, :, 0:64], psb[k][:].rearrange("p (h e) -> p h e", e=64), [P(k)], ["vaug"])
            if stop == 'v':
                S.barrier(); S.emit(); return nc
            qts = [A.bf(S_LEN) for _ in range(2)]
            kts = [A.bf(S_LEN) for _ in range(2)]
            sqs = [A.bf(512) for _ in range(2)]
            rss = [A.f32(512) for _ in range(2)]
            ebs = [A.bf(512) for _ in range(3)]
            pbs = [A.bf(512) for _ in range(3)]
            wn = constb[0:65, NC_WN:NC_WN + 64]
            cnt = 0
            for i in range(4):
                qt, kt = qts[i % 2], kts[i % 2]
                qk, kk_ = "qt%d" % (i % 2), "kt%d" % (i % 2)
                for which, dst, dkey, gcol, c0 in ((0, qt, qk, gq8[:, 0:1], 0), (1, kt, kk_, cols[:, CV_GK:CV_GK + 1], 512)):
                    for tg in range(4):
                        k = ps((0, 1, 2, 3))
                        for kc in range(8):
                            mm(psb[k][:], wq3[:, kc, c0 + 128 * i: c0 + 128 * (i + 1)], xn3[:, kc, tg * 512:(tg + 1) * 512], kc == 0, kc == 7, ["xnT"] + WQ, [P(k)])
                        b = cnt % 2
                        cnt += 1
                        act(sqs[b], psb[k][:], AF.Square, [P(k)], ["sq%d" % b])
                        k2 = ps((0, 1, 2, 3))
                        mm(psb[k2][:], blk, sqs[b], True, True, ["sq%d" % b, "constb"], [P(k2)])
                        act(rss[b], psb[k2][:], AF.Sqrt, [P(k2)], ["rs%d" % b], bias=1e-6, scale=1.0 / 64)
                        recip(rss[b], rss[b], ["rs%d" % b], ["rs%d" % b])
                        stt(dst[:, tg * 512:(tg + 1) * 512], psb[k][:], gcol, rss[b], ALU.mult, ALU.mult, [P(k), "rs%d" % b, "gq8", "cols"], [dkey])
                for hh in range(2):
                    h = 2 * i + hh
                    pb = 64 * hh
                    ob = (4, 5, 6, 7)
                    ec = 0
                    for j in range(16):
                        for g in range(j // 4, 4):
                            cst = max(128 * j, 512 * g)
                            cen = 512 * (g + 1)
                            n = cen - cst
                            k = ps((0, 1, 2, 3))
                            mm(psb[k][:, 0:n], kt[pb:pb + 64, j * 128:(j + 1) * 128], qt[pb:pb + 64, cst:cen], True, True, [qk, kk_], [P(k)])
                            b = ec % 3
                            ec += 1
                            act(ebs[b][:, 0:n], psb[k][:, 0:n], AF.Exp, [P(k)], ["eb%d" % b])
                            tt("pool" if ec % 2 else "dve", pbs[b][:, 0:n], ebs[b][:, 0:n], am3[:, h, cst - 128 * j: cen - 128 * j], ALU.mult, ["eb%d" % b, "amask%d" % h], ["pb%d" % b])
                            mm(psb[ob[g]][0:65, cst - 512 * g: cen - 512 * g], va4[:, j, h, :], pbs[b][:, 0:n], j == 0, j == 4 * g + 3, ["vaug", "pb%d" % b], [P(ob[g])])
                    for g in range(4):
                        b = cnt % 2
                        cnt += 1
                        act(sqs[b][0:65, :], psb[ob[g]][0:65, :], AF.Square, [P(ob[g])], ["sq%d" % b])
                        k2 = ps((0, 1, 2, 3))
                        mm(psb[k2][0:64, :], wn, sqs[b][0:65, :], True, True, ["sq%d" % b, "constb"], [P(k2)])
                        act(rss[b][0:64, :], psb[k2][0:64, :], AF.Sqrt, [P(k2)], ["rs%d" % b])
                        recip(rss[b][0:64, :], rss[b][0:64, :], ["rs%d" % b], ["rs%d" % b])
                        stt(mix3[pb:pb + 64, i, g * 512:(g + 1) * 512], psb[ob[g]][0:64, :], cols[0:64, CV_GA + h:CV_GA + h + 1], rss[b][0:64, :], ALU.mult, ALU.mult,
                            [P(ob[g]), "rs%d" % b, "cols"], ["mixT"])

            if stop == 'pa':
                S.barrier(); A.reset(); dbgf = A.f32(S_LEN)
                for cc in range(8):
                    cp("dve", dbgf, mix3[:, cc, :], ["mixT"], ["dbgf"])
                    dma("sp", dbg_mix[:, cc * S_LEN:(cc + 1) * S_LEN], dbgf, reads=["dbgf"])
                S.barrier(); S.emit(); return nc
            S.barrier()
            A.off = mark_pa
            wr = A.bf(8 * 1792)
            wr3 = wr.rearrange("p (c n) -> p c n", n=1792)
            for kc in range(8):
                dma("pool", wr3[:, kc, :], w_in[kc * 128:(kc + 1) * 128, 1536:3328], writes=["wr%d" % kc])
            WR = ["wr%d" % kc for kc in range(8)]
            w2a2 = A.bf(512)
            g2b = A.bf(512)
            dma("pool", w2a2[0:64, :], w2d, writes=["w2a2a"])
            dma("pool", w2a2[64:128, :], a2d, writes=["w2a2b"])
            dma("pool", g2b, g2d, writes=["g2b"])
            lora1 = A.bf(S_LEN)
            lora2 = A.bf(S_LEN)
            halo = A.f32(16)
            ptsb = [A.f32(514) for _ in range(2)]
            dtmp = [A.f32(512) for _ in range(2)]
            M4 = constb[:, NC_M4:NC_M4 + 512]
            ML2 = constb[:, NC_ML4:NC_ML4 + 256]
            I2 = constb[:, NC_I2:NC_I2 + 256]
            pcount = [0]

            def proj_shift(cj, tg, dst_fn):
                k = ps()
                for kc in range(8):
                    mm(psb[k][:], wr3[:, kc, cj * 128:(cj + 1) * 128], xn3[:, kc, tg * 512:(tg + 1) * 512], kc == 0, kc == 7, ["xnT"] + WR, [P(k)])
                b = pcount[0] % 2
                pcount[0] += 1
                pk = "ptsb%d" % b
                if tg == 0:
                    S.op("pool", lambda e: e.memset(ptsb[b][:, 0:1], 0.0), (), [pk + "h"])
                else:
                    cp("pool", ptsb[b][:, 0:1], halo[:, cj:cj + 1], ["halo%d" % cj], [pk + "h"])
                cp("act", ptsb[b][:, 1:513], psb[k][:], [P(k)], [pk])
                cp("pool", halo[:, cj:cj + 1], ptsb[b][:, 512:513], [pk], ["halo%d" % cj])
                tt("pool", dtmp[b], ptsb[b][:, 0:512], ptsb[b][:, 1:513], ALU.subtract, [pk, pk + "h"], ["dtmp%d" % b])
                dst_fn(dtmp[b], cols[:, CV_MU + cj:CV_MU + cj + 1], ptsb[b][:, 1:513], ["dtmp%d" % b, pk, "cols"])

            A_l12 = A.f32(512)
            for tg in range(4):
                sl = slice(tg * 512, (tg + 1) * 512)
                tmpx = dtmp

                def f12(d, mu, p, rd, sl=sl):
                    xs = A_l12
                    stt(xs, d, mu, p, ALU.mult, ALU.add, rd, ["xs12"])
                    act(lora1[0:64, sl], xs[0:64, :], AF.Tanh, ["xs12"], ["lora1a"])
                    act(lora1[64:128, sl], xs[64:128, :], AF.Copy, ["xs12"], ["lora1b"])

                def f13(d, mu, p, rd, sl=sl):
                    xs = A_l12
                    stt(xs, d, mu, p, ALU.mult, ALU.add, rd, ["xs12"])
                    act(lora2[:, sl], xs, AF.Sigmoid, ["xs12"], ["lora2"])

                proj_shift(12, tg, f12)
                proj_shift(13, tg, f13)

            if stop == 'lora':
                S.barrier(); S.emit(); return nc
            rT, kT, vT = A.f32(512), A.f32(512), A.f32(512)
            sw, ia, gT4 = A.f32(512), A.f32(512), A.f32(S_LEN)
            Ls, D1, D2, kk0, kk, kp, bv = [A.f32(512) for _ in range(7)]
            E1, E2, E3, E4 = [A.f32(512) for _ in range(4)]
            rn = A.f32(512)
            sqb, rkr = A.bf(512), A.bf(512)
            bonus4 = A.f32(S_LEN)
            ART = A.bf(4 * 256)
            ART4 = ART.rearrange("p (c w t) -> p c w t", w=2, t=128)
            KBT = A.bf(4 * 256)
            KBT4 = KBT.rearrange("p (c w t) -> p c w t", w=2, t=128)
            kbarT, bbarT, vbT = A.bf(512), A.bf(512), A.bf(512)
            KBtm = A.bf(4 * 256)
            KBtm4 = KBtm.rearrange("p (c w n) -> p c w n", w=2, n=128)
            Vtm = A.bf(4 * 128)
            Vtm3 = Vtm.rearrange("p (c n) -> p c n", n=128)
            VtmZ = A.bf(4 * 256)
            VtmZ4 = VtmZ.rearrange("p (c h n) -> p c h n", h=2, n=128)
            sc = A.f32(8)
            sc3 = sc.rearrange("p (c w) -> p c w", w=2)
            AM = A.bf(2 * 512)
            AM3 = AM.rearrange("p (h n) -> p h n", n=512)
            NTt = A.bf(256)
            NT3 = NTt.rearrange("p (h n) -> p h n", n=128)
            ZT = A.bf(512)
            ZT3 = ZT.rearrange("p (h n) -> p h n", n=256)
            Wb, Ub = A.bf(128), A.bf(128)
            UbZ = A.bf(256)
            UbZ3 = UbZ.rearrange("p (h n) -> p h n", n=128)
            Hf = A.f32(64)
            HbP = A.bf(128)
            YT = A.f32(512)
            yb, sq2 = A.bf(512), A.bf(512)
            cen, sd = A.f32(512), A.f32(512)

            for i in range(4):
                S.op("pool", lambda e: e.memset(VtmZ, 0.0), (), ["VtmZ"])
                S.op("pool", lambda e: e.memset(UbZ, 0.0), (), ["UbZ"])
                S.op("pool", lambda e: e.memset(HbP, 0.0), (), ["HbP"])
                S.op("pool", lambda e: e.memset(Hf, 0.0), (), ["Hf"])
                for tg in range(4):
                    sl = slice(tg * 512, (tg + 1) * 512)

                    def mk(dst, key):
                        def f(d, mu, p, rd):
                            stt(dst, d, mu, p, ALU.mult, ALU.add, rd, [key])
                        return f

                    proj_shift(0 + i, tg, mk(rT, "rT"))
                    proj_shift(4 + i, tg, mk(kT, "kT"))
                    proj_shift(8 + i, tg, mk(vT, "vT"))
                    k = ps()
                    mm(psb[k][:], w2a2[0:64, 128 * i:128 * (i + 1)], lora1[0:64, sl], True, True, ["w2a2a", "lora1a"], [P(k)])
                    act(sw, psb[k][:], AF.Sigmoid, [P(k), "cols"], ["sw"], bias=cols[:, CV_W0 + i:CV_W0 + i + 1])
                    k = ps()
                    mm(psb[k][:], w2a2[64:128, 128 * i:128 * (i + 1)], lora1[64:128, sl], True, True, ["w2a2b", "lora1b"], [P(k)])
                    act(ia, psb[k][:], AF.Sigmoid, [P(k), "cols"], ["ia"], bias=cols[:, CV_A0 + i:CV_A0 + i + 1])
                    k = ps()
                    mm(psb[k][:], g2b[:, 128 * i:128 * (i + 1)], lora2[:, sl], True, True, ["g2b", "lora2"], [P(k)])
                    cp("act", gT4[:, sl], psb[k][:], [P(k)], ["gT4"])
                    S.op("dve", lambda e: e.tensor_tensor_scan(out=Ls, data0=constf[:], data1=sw, initial=0.0, op0=ALU.mult, op1=ALU.add), ["constf", "sw"], ["Ls"])
                    Ls3 = Ls.rearrange("p (c t) -> p c t", t=128)
                    tt("pool", D1.rearrange("p (c t) -> p c t", t=128), Ls3, Ls3[:, :, 63:64].to_broadcast([128, 4, 128]), ALU.subtract, ["Ls"], ["D1"])
                    act(E1, D1, AF.Exp, ["D1"], ["E1"], scale=-C0)
                    act(E3, D1, AF.Exp, ["D1"], ["E3"], scale=C0)
                    tt("pool", D2, D1, sw, ALU.subtract, ["D1", "sw"], ["D2"])
                    act(E2, D2, AF.Exp, ["D2"], ["E2"], scale=-C0)
                    tt("dve", D2.rearrange("p (c t) -> p c t", t=128), Ls3, Ls3[:, :, 127:128].to_broadcast([128, 4, 128]), ALU.subtract, ["Ls", "E2"], ["D2"])
                    act(E4, D2, AF.Exp, ["D2"], ["E4"], scale=C0)
                    act(sc3[:, :, 0:1], Ls3[:, :, 63:64], AF.Exp, ["Ls"], ["sc"], scale=-C0)
                    act(sc3[:, :, 1:2], Ls3[:, :, 127:128], AF.Exp, ["Ls"], ["sc"], scale=-C0)
                    chk("e1")
                    ts("dve", kk0, kT, cols[:, CV_KK + i:CV_KK + i + 1], ALU.mult, ["kT", "cols"], ["kk0"])
                    tt("pool", sqb, kk0, kk0, ALU.mult, ["kk0"], ["sqb"])
                    k = ps()
                    mm(psb[k][:], blk, sqb, True, True, ["sqb", "constb"], [P(k)])
                    act(rn, psb[k][:], AF.Sqrt, [P(k)], ["rn"])
                    ts("dve", rn, rn, 1e-12, ALU.max, ["rn"], ["rn"])
                    recip(rn, rn, ["rn"], ["rn"])
                    tt("dve", kk, kk0, rn, ALU.mult, ["kk0", "rn"], ["kk"])
                    ts("dve", kp, ia, -1.0, ALU.add, ["ia", "cols"], ["kp"], s2=cols[:, CV_KA + i:CV_KA + i + 1], op1=ALU.mult)
                    stt(kp, kp, 1.0, kT, ALU.add, ALU.mult, ["kp", "kT"], ["kp"])
                    tt("pool", bv, kk, ia, ALU.mult, ["kk", "ia"], ["bv"])
                    c3 = lambda ap: ap.rearrange("p (c t) -> p c t", t=128)
                    tt("dve", ART4[:, :, 1, :], c3(rT), c3(E1), ALU.mult, ["rT", "E1"], ["ART"])
                    stt(kk0, kk, -1.0, E2, ALU.mult, ALU.mult, ["kk", "E2"], ["kk0"])
                    cp("pool", ART4[:, :, 0, :], c3(kk0), ["kk0"], ["ART"])
                    tt("pool", KBT4[:, :, 1, :], c3(kp), c3(E3), ALU.mult, ["kp", "E3"], ["KBT"])
                    tt("dve", KBT4[:, :, 0, :], c3(bv), c3(E3), ALU.mult, ["bv", "E3"], ["KBT"])
                    tt("pool", kbarT, kp, E4, ALU.mult, ["kp", "E4"], ["kbarT"])
                    tt("dve", bbarT, bv, E4, ALU.mult, ["bv", "E4"], ["bbarT"])
                    cp("pool", vbT, vT, ["vT"], ["vbT"])
                    stt(rkr, rT, cols[:, CV_RK + i:CV_RK + i + 1], kp, ALU.mult, ALU.mult, ["rT", "kp", "cols"], ["rkr"])
                    k = ps()
                    mm(psb[k][:], blk, rkr, True, True, ["rkr", "constb"], [P(k)])
                    tt("dve", bonus4[:, sl], psb[k][:], vT, ALU.mult, [P(k), "vT"], ["bonus4"])
                    chk("e2")
                    for c in range(4):
                        k = ps()
                        pv = psb[k][:].bitcast(BF16)
                        cs = slice(c * 128, (c + 1) * 128)
                        tr(pv[:, 0:128], kbarT[:, cs], ["kbarT", "constb"], [P(k)])
                        tr(pv[:, 128:256], bbarT[:, cs], ["bbarT", "constb"], [P(k)])
                        tr(pv[:, 256:384], vbT[:, cs], ["vbT", "constb"], [P(k)])
                        cp("act", KBtm4[:, c, :, :], pv[:, 0:256].rearrange("p (w n) -> p w n", n=128), [P(k)], ["KBtm"])
                        cp("dve", Vtm3[:, c, :], pv[:, 256:384], [P(k)], ["Vtm"])
                        cp("act", VtmZ4[:, c, 0, 0:64], pv[:, 256:320], [P(k)], ["VtmZ"])
                        cp("dve", VtmZ4[:, c, 1, 64:128], pv[:, 320:384], [P(k)], ["VtmZ"])
                    chk("e3")
                    for c in range(4):
                        k = ps()
                        k2 = ps()
                        kn = ps()
                        for hh in range(2):
                            pb = 64 * hh
                            kk_b = k if hh == 0 else k2
                            rhs_ar = ART4[pb:pb + 64, c, :, :].rearrange("p w t -> p (w t)")
                            mm(psb[kk_b][:, 0:256], KBT4[pb:pb + 64, c, 0, :], rhs_ar, True, False, ["KBT", "ART"], [P(kk_b)])
                            mm(psb[kk_b][:, 256:512], KBT4[pb:pb + 64, c, 1, :], rhs_ar, False, True, ["KBT", "ART"], [P(kk_b)])
                            knh = kn if hh == 0 else ps()
                            mm(psb[knh][:, 0:128], ART4[pb:pb + 64, c, 0, :], KBT4[pb:pb + 64, c, 0, :], True, True, ["KBT", "ART"], [P(knh)])
                            tt("dve", AM3[:, hh, :], psb[kk_b][:], M4, ALU.mult, [P(kk_b), "constb"], ["AM"])
                            tt("dve", NT3[:, hh, :], psb[knh][:, 0:128], ML2[:, 0:128], ALU.mult, [P(knh), "constb"], ["NT"])
                        chk("e4")
                        cp("dve", ZT3[:, :, 0:128], AM3[:, :, 0:128], ["AM"], ["ZT"])
                        tt("dve", ZT3[:, :, 128:256], AM3[:, :, 0:128], I2.rearrange("p (h n) -> p h n", n=128), ALU.add, ["AM", "constb"], ["ZT"])
                        chk("e4b")
                        for m in range(7):
                            if m == 1:
                                chk("e4c")
                            if m == 2:
                                chk("e4d")
                            kz = ps()
                            kn2 = ps()
                            if m == 0:
                                for hh in range(2):
                                    mm(psb[kz][:, 256 * hh:256 * hh + 128], NT3[:, hh, :], ZT3[:, hh, 0:128], hh == 0, hh == 1, ["NT", "ZT"], [P(kz)])
                            elif m < 6:
                                for hh in range(2):
                                    mm(psb[kz][:, 256 * hh:256 * hh + 256], NT3[:, hh, :], ZT3[:, hh, :], hh == 0, hh == 1, ["NT", "ZT"], [P(kz)])
                            else:
                                for hh in range(2):
                                    mm(psb[kz][:, 256 * hh + 128:256 * hh + 256], NT3[:, hh, :], ZT3[:, hh, 128:256], hh == 0, hh == 1, ["NT", "ZT"], [P(kz)])
                            if m < 6:
                                for hh in range(2):
                                    mm(psb[kn2][:, 128 * hh:128 * hh + 128], ZT3[:, hh, 0:128], NT3[:, hh, :], hh == 0, hh == 1, ["NT", "ZT"], [P(kn2)])
                            pz3 = psb[kz][:].rearrange("p (h n) -> p h n", n=256)
                            if m >= 1:
                                tt("dve", ZT3[:, :, 128:256], ZT3[:, :, 128:256], pz3[:, :, 128:256], ALU.add, [P(kz), "ZT"], ["ZT"])
                            if m < 6:
                                cp("act", ZT3[:, :, 0:128], pz3[:, :, 0:128], [P(kz)], ["ZT"])
                                cp("act", NTt, psb[kn2][:, 0:256], [P(kn2)], ["NT"])
                        chk("e5")
                        if not (tg == 0 and c == 0):
                            nsc = sc3[:, c, 0:1]
                            ts("dve", HbP[0:64, 0:64], Hf[0:64, :], nsc[0:64], ALU.mult, ["Hf", "sc"], ["HbP"])
                            ts("dve", HbP[64:128, 64:128], Hf[64:128, :], nsc[64:128], ALU.mult, ["Hf", "sc"], ["HbP"])
                        kw = ps()
                        mm(psb[kw][:, 0:128], ART4[:, c, 0, :], HbP, True, False, ["ART", "HbP"], [P(kw)])
                        for hh in range(2):
                            mm(psb[kw][:, 64 * hh:64 * hh + 64], AM3[:, hh, 256:384], Vtm3[:, c, 64 * hh:64 * hh + 64], False, hh == 1, ["AM", "Vtm"], [P(kw)])
                        cp("act", Wb, psb[kw][:, 0:128], [P(kw)], ["Wb"])
                        ku = ps()
                        for hh in range(2):
                            mm(psb[ku][:, 64 * hh:64 * hh + 64], ZT3[:, hh, 128:256], Wb[:, 64 * hh:64 * hh + 64], hh == 0, hh == 1, ["ZT", "Wb"], [P(ku)])
                        cp("act", Ub, psb[ku][:, 0:128], [P(ku)], ["Ub"])
                        cp("dve", UbZ3[:, 0, 0:64], psb[ku][:, 0:64], [P(ku)], ["UbZ"])
                        cp("dve", UbZ3[:, 1, 64:128], psb[ku][:, 64:128], [P(ku)], ["UbZ"])
                        chk("e6")
                        ky = ps()
                        mm(psb[ky][:, 0:128], HbP, ART4[:, c, 1, :], True, False, ["HbP", "ART"], [P(ky)])
                        for hh in range(2):
                            mm(psb[ky][:, 0:128], UbZ3[:, hh, :], AM3[:, hh, 128:256], False, False, ["UbZ", "AM"], [P(ky)])
                            mm(psb[ky][:, 0:128], VtmZ4[:, c, hh, :], AM3[:, hh, 384:512], False, hh == 1, ["VtmZ", "AM"], [P(ky)])
                        cp("act", YT[:, c * 128:(c + 1) * 128], psb[ky][:, 0:128], [P(ky)], ["YT"])
                        kh = ps()
                        mm(psb[kh][:, 0:128], KBtm4[:, c, 1, :], Ub, True, False, ["KBtm", "Ub"], [P(kh)])
                        mm(psb[kh][:, 0:128], KBtm4[:, c, 0, :], Vtm3[:, c, :], False, True, ["KBtm", "Vtm"], [P(kh)])
                        ts("dve", Hf, Hf, sc3[:, c, 1:2], ALU.mult, ["Hf", "sc"], ["Hf"])
                        tt("dve", Hf[0:64, :], Hf[0:64, :], psb[kh][0:64, 0:64], ALU.add, ["Hf", P(kh)], ["Hf"])
                        tt("dve", Hf[64:128, :], Hf[64:128, :], psb[kh][64:128, 64:128], ALU.add, ["Hf", P(kh)], ["Hf"])
                    chk("e7")
                    cp("pool", yb, YT, ["YT"], ["yb"])
                    k = ps()
                    mm(psb[k][:], blk, yb, True, True, ["yb", "constb"], [P(k)])
                    stt(cen, psb[k][:], -1.0 / 64, YT, ALU.mult, ALU.add, [P(k), "YT"], ["cen"])
                    tt("pool", sq2, cen, cen, ALU.mult, ["cen"], ["sq2"])
                    k = ps()
                    mm(psb[k][:], blk, sq2, True, True, ["sq2", "constb"], [P(k)])
                    act(sd, psb[k][:], AF.Sqrt, [P(k)], ["sd"], bias=64e-5, scale=1.0 / 64)
                    recip(sd, sd, ["sd"], ["sd"])
                    tt("dve", cen, cen, sd, ALU.mult, ["cen", "sd"], ["cen"])
                    ts("dve", cen, cen, cols[:, CV_LG + i:CV_LG + i + 1], ALU.mult, ["cen", "cols"], ["cen"], s2=cols[:, CV_LB + i:CV_LB + i + 1], op1=ALU.add)
                    tt("pool", cen, cen, bonus4[:, sl], ALU.add, ["cen", "bonus4"], ["cen"])
                    tt("dve", mix3[:, 4 + i, sl], cen, gT4[:, sl], ALU.mult, ["cen", "gT4"], ["mixT"])
            if stop == 'pr':
                S.barrier(); A.reset(); dbgf = A.f32(S_LEN)
                for cc in range(8):
                    cp("dve", dbgf, mix3[:, cc, :], ["mixT"], ["dbgf"])
                    dma("sp", dbg_mix[:, cc * S_LEN:(cc + 1) * S_LEN], dbgf, reads=["dbgf"])
                S.barrier(); S.emit(); return nc
            if dbg and s == 0:
                S.barrier()
                A.reset()
                dbgf = A.f32(S_LEN)
                for cc in range(8):
                    cp("dve", dbgf, mix3[:, cc, :], ["mixT"], ["dbgf"])
                    dma("sp", dbg_mix[:, cc * S_LEN:(cc + 1) * S_LEN], dbgf, reads=["dbgf"])

            S.barrier()
            A.reset()
            wo = A.bf(8 * D)
            wo3 = wo.rearrange("p (c n) -> p c n", n=D)
            for kc in range(8):
                dma("pool", wo3[:, kc, :], w_out[kc * 128:(kc + 1) * 128, :], writes=["wo%d" % kc])
            WO = ["wo%d" % kc for kc in range(8)]
            gbc2 = A.f32(D)
            dma("sp", gbc2, g2v[0, :].partition_broadcast(128), writes=["gbc"])
            x1 = A.f32(8 * D)
            x13 = x1.rearrange("p (t n) -> p t n", n=D)
            xn2T = A.bf(8 * 1024)
            xn23 = xn2T.rearrange("p (c t) -> p c t", t=1024)
            hT = A.bf(11 * 1024)
            hT3 = hT.rearrange("p (f t) -> p f t", t=1024)
            wd = A.bf(11 * D)
            wd3 = wd.rearrange("p (f n) -> p f n", n=D)
            wgs = [A.bf(8 * 128) for _ in range(2)]
            wus = [A.bf(8 * 128) for _ in range(2)]
            scr2 = A.bf(D)
            sms2 = [A.f32(4) for _ in range(2)]
            xsb2 = [A.bf(D) for _ in range(2)]
            sgs = [A.f32(512) for _ in range(2)]
            for half in range(2):
                ht0 = tok0 + half * 1024
                for tt_ in range(8):
                    t0 = half * 1024 + tt_ * 128
                    xk = "x1_%d" % tt_
                    dma("sp", x13[:, tt_, :], x[ht0 + tt_ * 128: ht0 + (tt_ + 1) * 128, :], writes=[xk])
                    for nh in range(2):
                        k = ps()
                        for kc in range(8):
                            mm(psb[k][:], mix3[:, kc, t0:t0 + 128], wo3[:, kc, nh * 512:(nh + 1) * 512], kc == 0, kc == 7, ["mixT"] + WO, [P(k)])
                        tt("dve", x13[:, tt_, nh * 512:(nh + 1) * 512], x13[:, tt_, nh * 512:(nh + 1) * 512], psb[k][:], ALU.add, [xk, P(k)], [xk])
                    b = tt_ % 2
                    rms_to_T(x13[:, tt_, :], xk, gbc2, xn23, "xn2T", tt_ * 128, scr2, "scr2", sms2[b], "sm2%d" % b, xsb2[b], "xsb2%d" % b)
                for fh in range(2):
                    for fi in range(11):
                        f = fh * 11 + fi
                        dma("pool", wd3[:, fi, :], w_down[f * 128:(f + 1) * 128, :], writes=["wd%d" % fi])
                    for fi in range(11):
                        f = fh * 11 + fi
                        b = fi % 2
                        wg3 = wgs[b].rearrange("p (c n) -> p c n", n=128)
                        wu3 = wus[b].rearrange("p (c n) -> p c n", n=128)
                        dma("pool", wg3, w_gate[:, f * 128:(f + 1) * 128].rearrange("(c p) n -> p c n", p=128), writes=["wg%d" % b])
                        dma("pool", wu3, w_up[:, f * 128:(f + 1) * 128].rearrange("(c p) n -> p c n", p=128), writes=["wu%d" % b])
                        for tg in range(2):
                            kg = ps()
                            ku = ps()
                            for kc in range(8):
                                mm(psb[kg][:], wg3[:, kc, :], xn23[:, kc, tg * 512:(tg + 1) * 512], kc == 0, kc == 7, ["wg%d" % b, "xn2T"], [P(kg)])
                            for kc in range(8):
                                mm(psb[ku][:], wu3[:, kc, :], xn23[:, kc, tg * 512:(tg + 1) * 512], kc == 0, kc == 7, ["wu%d" % b, "xn2T"], [P(ku)])
                            sb_ = tg
                            act(sgs[sb_], psb[kg][:], AF.Silu, [P(kg)], ["sg%d" % sb_])
                            tt("dve", hT3[:, fi, tg * 512:(tg + 1) * 512], sgs[sb_], psb[ku][:], ALU.mult, ["sg%d" % sb_, P(ku)], ["hT%d" % fi])
                    for tt_ in range(8):
                        xk = "x1_%d" % tt_
                        for nh in range(2):
                            k = ps()
                            for fi in range(11):
                                mm(psb[k][:], hT3[:, fi, tt_ * 128:(tt_ + 1) * 128], wd3[:, fi, nh * 512:(nh + 1) * 512], fi == 0, fi == 10, ["hT%d" % fi, "wd%d" % fi], [P(k)])
                            tt("dve", x13[:, tt_, nh * 512:(nh + 1) * 512], x13[:, tt_, nh * 512:(nh + 1) * 512], psb[k][:], ALU.add, [xk, P(k)], [xk])
                        if fh == 1:
                            dma("sp", out[ht0 + tt_ * 128: ht0 + (tt_ + 1) * 128, :], x13[:, tt_, :], reads=[xk])
        S.barrier()
        S.emit()
    return nc


_NC_CACHE = {}


def kernel(**inputs):
    inp = {k: np.asarray(v) for k, v in inputs.items()}
    ncores = 8
    nseq = 2
    if "nc" not in _NC_CACHE:
        _NC_CACHE["nc"] = build(nseq)
    nc = _NC_CACHE["nc"]
    x = np.ascontiguousarray(inp["x"], dtype=np.float32)
    B = x.shape[0]
    sq = lambda k: np.ascontiguousarray(inp[k][0], dtype=np.float32)
    shared = {
        "w_in": sq("w_in"), "w_out": sq("w_out"), "w_gate": sq("w_gate"), "w_up": sq("w_up"), "w_down": sq("w_down"),
        "norm1_g": sq("norm1_g").reshape(1, D), "norm2_g": sq("norm2_g").reshape(1, D),
        "w2": sq("w2"), "a2": sq("a2"), "g2": sq("g2"),
        "consts": make_consts(), "amask": make_amask(),
        "cols": make_cols({k: inp[k][0] for k in ("rwkv_mu", "w0", "a0", "k_k", "k_a", "r_k", "lnx_g", "lnx_b", "q_norm_g", "k_norm_g", "attn_out_g")}),
    }
    in_maps = []
    for c in range(ncores):
        m = dict(shared)
        m["x"] = x[c * nseq:(c + 1) * nseq].reshape(nseq * S_LEN, D)
        in_maps.append(m)
    res = run_bass_kernel_spmd(nc, in_maps, core_ids=list(range(ncores)))
    outs = [np.asarray(r["out"]).reshape(nseq, S_LEN, D) for r in res.results]
    return np.concatenate(outs, axis=0).astype(np.float32)
```

```python
import contextlib
import numpy as np
import concourse.bass as bass
import concourse.mybir as mybir
from concourse.bass_utils import run_bass_kernel_spmd

F32 = mybir.dt.float32
BF16 = mybir.dt.bfloat16
ALU = mybir.AluOpType
AF = mybir.ActivationFunctionType
AX = mybir.AxisListType
COMPUTE = ("pe", "act", "dve", "pool")

S_LEN = 2048
D = 1024
DFF = 2816
C0 = float(np.exp(-0.5))


class Sched:
    def __init__(self, nc, stack, n_dma_sems=24):
        self.nc = nc
        self.eng_names = ["pe", "act", "dve", "pool", "sp"]
        self.ops = {e: [] for e in self.eng_names}
        self.sem = {e: stack.enter_context(nc.semaphore("s_" + e)) for e in COMPUTE}
        self.cnt = {e: 0 for e in COMPUTE}
        self.dma_sems = [stack.enter_context(nc.semaphore("s_dma%d" % i)) for i in range(n_dma_sems)]
        self.dma_val = [0] * n_dma_sems
        self.dma_rr = 0
        self.res_w = {}
        self.res_r = {}
        self.waited = {e: {} for e in self.eng_names}

    def _need(self, eng, waits, ev):
        sid, sh, val = ev
        if self.waited[eng].get(sid, 0) >= val:
            return
        cur = waits.get(sid)
        if cur is None or cur[1] < val:
            waits[sid] = (sh, val)

    def op(self, eng, fn, reads=(), writes=(), dma=False):
        waits = {}
        own = None if dma else ("c", eng)
        for k in reads:
            w = self.res_w.get(k)
            if w is not None:
                self._need(eng, waits, w)
            if isinstance(k, tuple) and k[0] == "ps":
                for r in self.res_r.get(k, ()):
                    if r[0] != own:
                        self._need(eng, waits, r)
        for k in writes:
            w = self.res_w.get(k)
            if w is not None and (dma or w[0] != own):
                self._need(eng, waits, w)
            for r in self.res_r.get(k, ()):
                if dma or r[0] != own:
                    self._need(eng, waits, r)
        if dma:
            k = self.dma_rr
            self.dma_rr = (self.dma_rr + 1) % len(self.dma_sems)
            if self.dma_val[k] > 0:
                self._need(eng, waits, (("d", k), self.dma_sems[k], self.dma_val[k]))
            self.dma_val[k] += 16
            ev = (("d", k), self.dma_sems[k], self.dma_val[k])
            inc = 16
        else:
            self.cnt[eng] += 1
            ev = (("c", eng), self.sem[eng], self.cnt[eng])
            inc = 1
        for sid, (sh, val) in waits.items():
            self.waited[eng][sid] = val
        self.ops[eng].append((fn, list(waits.values()), ev[1], inc))
        for k in reads:
            self.res_r.setdefault(k, []).append(ev)
        for k in writes:
            self.res_w[k] = ev
            self.res_r[k] = []
        return ev

    def barrier(self):
        for eng in self.eng_names:
            waits = {}
            for e2 in COMPUTE:
                if self.cnt[e2] > 0 and e2 != eng:
                    self._need(eng, waits, (("c", e2), self.sem[e2], self.cnt[e2]))
            for k, v in enumerate(self.dma_val):
                if v > 0:
                    self._need(eng, waits, (("d", k), self.dma_sems[k], v))
            for sid, (sh, val) in waits.items():
                self.waited[eng][sid] = val
            self.ops[eng].append((None, list(waits.values()), None, 0))
        self.res_w = {}
        self.res_r = {}

    def emit(self):
        sched = self

        def run(engname, engine):
            for fn, waits, sh, inc in sched.ops[engname]:
                for (wsh, val) in waits:
                    engine.wait_ge(wsh, val)
                if fn is not None:
                    fn(engine).then_inc(sh, inc)

        with self.nc.Block() as block:
            @block.tensor
            def _(e):
                run("pe", e)

            @block.scalar
            def _(e):
                run("act", e)

            @block.vector
            def _(e):
                run("dve", e)

            @block.gpsimd
            def _(e):
                run("pool", e)

            @block.sync
            def _(e):
                run("sp", e)


class Arena:
    def __init__(self, t, n):
        self.t, self.n, self.off = t, n, 0

    def reset(self):
        self.off = 0

    def bf(self, n):
        n = (n + 1) // 2 * 2
        a = self.t[:, self.off:self.off + n]
        self.off += n
        assert self.off <= self.n, ("arena overflow", self.off, self.n)
        return a

    def f32(self, n):
        return self.bf(2 * n).bitcast(F32)


NC_ID, NC_BLK, NC_M4, NC_ML4, NC_I2, NC_RST, NC_WN = 0, 128, 256, 768, 1280, 1536, 2048
NCONST = 2048 + 64


def make_consts():
    c = np.zeros((128, NCONST), np.float32)
    c[:, NC_ID:NC_ID + 128] = np.eye(128)
    c[0:64, NC_BLK:NC_BLK + 64] = 1.0
    c[64:128, NC_BLK + 64:NC_BLK + 128] = 1.0
    p = np.arange(128)[:, None]
    f = np.arange(128)[None, :]
    mus = (p < f).astype(np.float32)
    mui = (p <= f).astype(np.float32)
    mls = (p > f).astype(np.float32)
    c[:, NC_M4:NC_M4 + 512] = np.concatenate([mus, mui, mus, mui], 1)
    c[:, NC_ML4:NC_ML4 + 512] = np.concatenate([mls] * 4, 1)
    c[:, NC_I2:NC_I2 + 256] = np.concatenate([np.eye(128)] * 2, 1)
    r = np.ones((128, 512), np.float32)
    r[:, 0::128] = 0.0
    c[:, NC_RST:NC_RST + 512] = r
    c[0:64, NC_WN:NC_WN + 64] = 1.0 / 64
    c[64, NC_WN:NC_WN + 64] = 1e-6
    return c


def make_amask():
    slopes = np.exp2(-8.0 * np.arange(1, 9, dtype=np.float64) / 8)
    ki = np.arange(128)[:, None]
    cc = np.arange(2048)[None, :]
    dl = cc - ki
    m = ((dl >= 0) & (dl <= 128)).astype(np.float64) + ((dl >= 0) & (dl % 4 == 0) & (dl <= 512)) + ((dl >= 0) & (dl % 16 == 0))
    out = np.zeros((8, 128, 2048), np.float32)
    for h in range(8):
        out[h] = m * np.exp(-slopes[h] * np.maximum(dl, 0))
    return out


CV_MU, CV_W0, CV_A0, CV_KK, CV_KA, CV_RK, CV_LG, CV_LB, CV_GQ, CV_GK, CV_GA = 0, 14, 18, 22, 26, 30, 34, 38, 42, 43, 44
NCV = 52


def make_cols(inp):
    cv = np.zeros((128, NCV), np.float32)

    def put(off, v):
        v = np.asarray(v, np.float32).reshape(-1, 128)
        cv[:, off:off + v.shape[0]] = v.T

    put(CV_MU, inp["rwkv_mu"])
    put(CV_W0, inp["w0"])
    put(CV_A0, inp["a0"])
    put(CV_KK, inp["k_k"])
    put(CV_KA, inp["k_a"])
    put(CV_RK, inp["r_k"])
    put(CV_LG, inp["lnx_g"])
    put(CV_LB, inp["lnx_b"])
    put(CV_GQ, np.tile(np.asarray(inp["q_norm_g"], np.float32).reshape(-1), 2))
    put(CV_GK, np.tile(np.asarray(inp["k_norm_g"], np.float32).reshape(-1), 2))
    ga = np.asarray(inp["attn_out_g"], np.float32).reshape(8, 64)
    cv[0:64, CV_GA:CV_GA + 8] = ga.T
    return cv


class StopBuild(Exception):
    pass


def build(NSEQ=2, dbg=False, stop=None):
    try:
        return _build(NSEQ, dbg, stop)
    except StopBuild as e:
        return e.args[0]


def _build(NSEQ=2, dbg=False, stop=None):
    nc = bass.Bass("TRN2", target_bir_lowering=False)
    NT = NSEQ * S_LEN

    def din(name, shape):
        return nc.dram_tensor(name, shape, F32, kind="ExternalInput").ap()

    x = din("x", [NT, D])
    w_in = din("w_in", [D, 3328])
    w_out = din("w_out", [D, D])
    w_gate = din("w_gate", [D, DFF])
    w_up = din("w_up", [D, DFF])
    w_down = din("w_down", [DFF, D])
    g1v = din("norm1_g", [1, D])
    g2v = din("norm2_g", [1, D])
    w2d = din("w2", [64, 512])
    a2d = din("a2", [64, 512])
    g2d = din("g2", [128, 512])
    consts_d = din("consts", [128, NCONST])
    amask_d = din("amask", [8, 128, 2048])
    cols_d = din("cols", [128, NCV])
    out = nc.dram_tensor("out", [NT, D], F32, kind="ExternalOutput").ap()
    if dbg:
        dbg_mix = nc.dram_tensor("dbg_mix", [128, 8 * S_LEN], F32, kind="ExternalOutput").ap()

    with contextlib.ExitStack() as st:
        S = Sched(nc, st)
        T = lambda n, s, d: st.enter_context(nc.sbuf_tensor(n, s, d))
        psb = [st.enter_context(nc.psum_tensor("ps%d" % i, [128, 512], F32)) for i in range(8)]
        rr = {"i": 0}

        def ps(pool=(0, 1, 2, 3, 4, 5, 6, 7)):
            k = pool[rr["i"] % len(pool)]
            rr["i"] += 1
            return k

        ARN = 82000
        arena_t = T("arena", [128, ARN], BF16)
        A = Arena(arena_t, ARN)
        mixT = T("mixT", [128, 8 * S_LEN], BF16)
        mix3 = mixT[:].rearrange("p (c t) -> p c t", t=S_LEN)
        constb = T("constb", [128, NCONST], BF16)
        constf = T("constf", [128, 512], F32)
        cols = T("cols_sb", [128, NCV], F32)
        gq8 = T("gq8", [128, 1], F32)
        identb = constb[:, NC_ID:NC_ID + 128]
        blk = constb[:, NC_BLK:NC_BLK + 128]

        def dma(eng, o, i, reads=(), writes=()):
            S.op(eng, lambda e: e.dma_start(out=o, in_=i), reads, writes, dma=True)

        def mm(o, l, r, start, stop, reads, writes):
            S.op("pe", lambda e: e.matmul(o, lhsT=l, rhs=r, start=start, stop=stop, skip_group_check=True), reads, writes)

        def tr(o, i, reads, writes):
            S.op("pe", lambda e: e.transpose(o, i, identb), reads, writes)

        def act(o, i, func, reads, writes, bias=0.0, scale=1.0, accum=None):
            if accum is None:
                S.op("act", lambda e: e.activation(out=o, in_=i, func=func, bias=bias, scale=scale), reads, writes)
            else:
                S.op("act", lambda e: e.activation(out=o, in_=i, func=func, bias=bias, scale=scale, accum_out=accum), reads, writes)

        def tt(eng, o, a, b, op, reads, writes):
            S.op(eng, lambda e: e.tensor_tensor(out=o, in0=a, in1=b, op=op), reads, writes)

        def ts(eng, o, a, s1, op0, reads, writes, s2=None, op1=None):
            if op1 is None:
                S.op(eng, lambda e: e.tensor_scalar(out=o, in0=a, scalar1=s1, scalar2=None, op0=op0), reads, writes)
            else:
                S.op(eng, lambda e: e.tensor_scalar(out=o, in0=a, scalar1=s1, scalar2=s2, op0=op0, op1=op1), reads, writes)

        def stt(o, a, s, b, op0, op1, reads, writes):
            S.op("dve", lambda e: e.scalar_tensor_tensor(out=o, in0=a, scalar=s, in1=b, op0=op0, op1=op1), reads, writes)

        def cp(eng, o, i, reads, writes):
            if eng == "act":
                act(o, i, AF.Copy, reads, writes)
            else:
                S.op(eng, lambda e: e.tensor_copy(out=o, in_=i), reads, writes)

        def recip(o, i, reads, writes):
            S.op("dve", lambda e: e.reciprocal(out=o, in_=i), reads, writes)

        def P(k):
            return ("ps", k)

        def chk(name):
            if stop == name:
                S.barrier(); S.emit()
                raise StopBuild(nc)

        dma("pool", constb[:], consts_d, writes=["constb"])
        dma("sp", constf[:], consts_d[:, NC_RST:NC_RST + 512], writes=["constf"])
        dma("sp", cols[:], cols_d, writes=["cols"])
        ts("dve", gq8[:], cols[:, CV_GQ:CV_GQ + 1], 0.125, ALU.mult, ["cols"], ["gq8"])

        def rms_to_T(xt, xt_key, gbc, dstT, dst_key, t0, scr, scr_key, sm, sm_key, xsb, xsb_key):
            act(scr, xt, AF.Square, [xt_key], [scr_key, sm_key], accum=sm[:, 0:1])
            ts("dve", sm[:, 1:2], sm[:, 0:1], 1.0 / D, ALU.mult, [sm_key], [sm_key], s2=1e-6, op1=ALU.add)
            act(sm[:, 2:3], sm[:, 1:2], AF.Sqrt, [sm_key], [sm_key])
            recip(sm[:, 3:4], sm[:, 2:3], [sm_key], [sm_key])
            stt(xsb, xt, sm[:, 3:4], gbc, ALU.mult, ALU.mult, [xt_key, sm_key, "gbc"], [xsb_key])
            k = ps()
            pv = psb[k][:].bitcast(BF16)
            for kc in range(8):
                tr(pv[:, kc * 128:(kc + 1) * 128], xsb[:, kc * 128:(kc + 1) * 128], [xsb_key, "constb"], [P(k)])
            cp("act", dstT[:, :, t0:t0 + 128], pv.rearrange("p (c t) -> p c t", t=128), [P(k)], [dst_key])

        for s in range(NSEQ):
            tok0 = s * S_LEN
            S.barrier()
            A.reset()
            xnT = A.bf(8 * S_LEN)
            xn3 = xnT.rearrange("p (c t) -> p c t", t=S_LEN)
            mark_pa = A.off
            gbc = A.f32(D)
            dma("sp", gbc, g1v[0, :].partition_broadcast(128), writes=["gbc"])
            xts = [A.f32(D) for _ in range(2)]
            scr = A.bf(D)
            sms = [A.f32(4) for _ in range(2)]
            xsbs = [A.bf(D) for _ in range(2)]
            wqkv = A.bf(8 * 1536)
            wq3 = wqkv.rearrange("p (c n) -> p c n", n=1536)
            for kc in range(8):
                dma("pool", wq3[:, kc, :], w_in[kc * 128:(kc + 1) * 128, 0:1536], writes=["wqkv%d" % kc])
            amask = A.bf(8 * 2048)
            am3 = amask.rearrange("p (h c) -> p h c", c=2048)
            for h in range(8):
                dma("pool", am3[:, h, :], amask_d[h], writes=["amask%d" % h])
            for ti in range(16):
                b = ti % 2
                dma("sp", xts[b], x[tok0 + ti * 128: tok0 + (ti + 1) * 128, :], writes=["xt%d" % b])
                rms_to_T(xts[b], "xt%d" % b, gbc, xn3, "xnT", ti * 128, scr, "scr", sms[b], "sm%d" % b, xsbs[b], "xsb%d" % b)
            if stop in ('p0', 'p0d'):
                if stop == 'p0d':
                    S.barrier(); A.reset(); dbgf = A.f32(S_LEN)
                    for cc in range(8):
                        cp("dve", dbgf, mix3[:, cc, :], ["mixT"], ["dbgf"])
                        dma("sp", dbg_mix[:, cc * S_LEN:(cc + 1) * S_LEN], dbgf, reads=["dbgf"])
                S.barrier(); S.emit(); return nc
            WQ = ["wqkv%d" % kc for kc in range(8)]
            vaug = A.bf(16 * 8 * 65)
            va4 = vaug.rearrange("p (j h e) -> p j h e", h=8, e=65)
            S.op("pool", lambda e: e.memset(vaug, 1.0), (), ["vaug"])
            for j in range(16):
                k = ps((0, 1, 2, 3))
                for kc in range(8):
                    mm(psb[k][:], xn3[:, kc, j * 128:(j + 1) * 128], wq3[:, kc, 1024:1536], kc == 0, kc == 7, ["xnT"] + WQ, [P(k)])
                cp("act" if j % 2 else "dve", va4[:, j, :, 0:64], psb[k][:].rearrange("p (h e) -> p h e", e=64), [P(k)], ["vaug"])
            if stop == 'v':
                S.barrier(); S.emit(); return nc
            qts = [A.bf(S_LEN) for _ in range(2)]
            kts = [A.bf(S_LEN) for _ in range(2)]
            sqs = [A.bf(512) for _ in range(2)]
            rss = [A.f32(512) for _ in range(2)]
            ebs = [A.bf(512) for _ in range(4)]
            pbs = [A.bf(512) for _ in range(4)]
            wn = constb[0:65, NC_WN:NC_WN + 64]
            cnt = 0
            for i in range(4):
                qt, kt = qts[i % 2], kts[i % 2]
                qk, kk_ = "qt%d" % (i % 2), "kt%d" % (i % 2)
                for which, dst, dkey, gcol, c0 in ((0, qt, qk, gq8[:, 0:1], 0), (1, kt, kk_, cols[:, CV_GK:CV_GK + 1], 512)):
                    for tg in range(4):
                        k = ps((0, 1, 2, 3))
                        for kc in range(8):
                            mm(psb[k][:], wq3[:, kc, c0 + 128 * i: c0 + 128 * (i + 1)], xn3[:, kc, tg * 512:(tg + 1) * 512], kc == 0, kc == 7, ["xnT"] + WQ, [P(k)])
                        b = cnt % 2
                        cnt += 1
                        act(sqs[b], psb[k][:], AF.Square, [P(k)], ["sq%d" % b])
                        k2 = ps((0, 1, 2, 3))
                        mm(psb[k2][:], blk, sqs[b], True, True, ["sq%d" % b, "constb"], [P(k2)])
                        act(rss[b], psb[k2][:], AF.Sqrt, [P(k2)], ["rs%d" % b], bias=1e-6, scale=1.0 / 64)
                        recip(rss[b], rss[b], ["rs%d" % b], ["rs%d" % b])
                        stt(dst[:, tg * 512:(tg + 1) * 512], psb[k][:], gcol, rss[b], ALU.mult, ALU.mult, [P(k), "rs%d" % b, "gq8", "cols"], [dkey])
                for hh in range(2):
                    h = 2 * i + hh
                    pb = 64 * hh
                    ob = (4, 5, 6, 7)
                    ec = 0
                    groups = []
                    for j in range(16):
                        for g in range(j // 4, 4):
                            cst = max(128 * j, 512 * g)
                            groups.append((j, g, cst, 512 * (g + 1)))
                    LA = 3
                    for t in range(len(groups) + LA):
                        if t < len(groups):
                            j, g, cst, cen = groups[t]
                            n = cen - cst
                            k = ps((0, 1, 2, 3))
                            mm(psb[k][:, 0:n], kt[pb:pb + 64, j * 128:(j + 1) * 128], qt[pb:pb + 64, cst:cen], True, True, [qk, kk_], [P(k)])
                            b = t % 4
                            act(ebs[b][:, 0:n], psb[k][:, 0:n], AF.Exp, [P(k)], ["eb%d" % b])
                            tt("pool" if t % 2 else "dve", pbs[b][:, 0:n], ebs[b][:, 0:n], am3[:, h, cst - 128 * j: cen - 128 * j], ALU.mult, ["eb%d" % b, "amask%d" % h], ["pb%d" % b])
                        if t >= LA:
                            j, g, cst, cen = groups[t - LA]
                            n = cen - cst
                            b = (t - LA) % 4
                            mm(psb[ob[g]][0:65, cst - 512 * g: cen - 512 * g], va4[:, j, h, :], pbs[b][:, 0:n], j == 0, j == 4 * g + 3, ["vaug", "pb%d" % b], [P(ob[g])])
                    for g in range(4):
                        b = cnt % 2
                        cnt += 1
                        act(sqs[b][0:65, :], psb[ob[g]][0:65, :], AF.Square, [P(ob[g])], ["sq%d" % b])
                        k2 = ps((0, 1, 2, 3))
                        mm(psb[k2][0:64, :], wn, sqs[b][0:65, :], True, True, ["sq%d" % b, "constb"], [P(k2)])
                        act(rss[b][0:64, :], psb[k2][0:64, :], AF.Sqrt, [P(k2)], ["rs%d" % b])
                        recip(rss[b][0:64, :], rss[b][0:64, :], ["rs%d" % b], ["rs%d" % b])
                        stt(mix3[pb:pb + 64, i, g * 512:(g + 1) * 512], psb[ob[g]][0:64, :], cols[0:64, CV_GA + h:CV_GA + h + 1], rss[b][0:64, :], ALU.mult, ALU.mult,
                            [P(ob[g]), "rs%d" % b, "cols"], ["mixT"])

            if stop == 'pa':
                S.barrier(); A.reset(); dbgf = A.f32(S_LEN)
                for cc in range(8):
                    cp("dve", dbgf, mix3[:, cc, :], ["mixT"], ["dbgf"])
                    dma("sp", dbg_mix[:, cc * S_LEN:(cc + 1) * S_LEN], dbgf, reads=["dbgf"])
                S.barrier(); S.emit(); return nc
            S.barrier()
            A.off = mark_pa
            wr = A.bf(8 * 1792)
            wr3 = wr.rearrange("p (c n) -> p c n", n=1792)
            for kc in range(8):
                dma("pool", wr3[:, kc, :], w_in[kc * 128:(kc + 1) * 128, 1536:3328], writes=["wr%d" % kc])
            WR = ["wr%d" % kc for kc in range(8)]
            w2a2 = A.bf(512)
            g2b = A.bf(512)
            dma("pool", w2a2[0:64, :], w2d, writes=["w2a2a"])
            dma("pool", w2a2[64:128, :], a2d, writes=["w2a2b"])
            dma("pool", g2b, g2d, writes=["g2b"])
            lora1 = A.bf(S_LEN)
            lora2 = A.bf(S_LEN)
            halo = A.f32(16)
            ptsb = [A.f32(514) for _ in range(2)]
            dtmp = [A.f32(512) for _ in range(2)]
            M4 = constb[:, NC_M4:NC_M4 + 512]
            ML2 = constb[:, NC_ML4:NC_ML4 + 256]
            I2 = constb[:, NC_I2:NC_I2 + 256]
            pcount = [0]

            def proj_shift(cj, tg, dst_fn):
                k = ps()
                for kc in range(8):
                    mm(psb[k][:], wr3[:, kc, cj * 128:(cj + 1) * 128], xn3[:, kc, tg * 512:(tg + 1) * 512], kc == 0, kc == 7, ["xnT"] + WR, [P(k)])
                b = pcount[0] % 2
                pcount[0] += 1
                pk = "ptsb%d" % b
                if tg == 0:
                    S.op("pool", lambda e: e.memset(ptsb[b][:, 0:1], 0.0), (), [pk + "h"])
                else:
                    cp("pool", ptsb[b][:, 0:1], halo[:, cj:cj + 1], ["halo%d" % cj], [pk + "h"])
                cp("act", ptsb[b][:, 1:513], psb[k][:], [P(k)], [pk])
                cp("pool", halo[:, cj:cj + 1], ptsb[b][:, 512:513], [pk], ["halo%d" % cj])
                tt("pool", dtmp[b], ptsb[b][:, 0:512], ptsb[b][:, 1:513], ALU.subtract, [pk, pk + "h"], ["dtmp%d" % b])
                dst_fn(dtmp[b], cols[:, CV_MU + cj:CV_MU + cj + 1], ptsb[b][:, 1:513], ["dtmp%d" % b, pk, "cols"])

            A_l12 = A.f32(512)
            for tg in range(4):
                sl = slice(tg * 512, (tg + 1) * 512)
                tmpx = dtmp

                def f12(d, mu, p, rd, sl=sl):
                    xs = A_l12
                    stt(xs, d, mu, p, ALU.mult, ALU.add, rd, ["xs12"])
                    act(lora1[0:64, sl], xs[0:64, :], AF.Tanh, ["xs12"], ["lora1a"])
                    act(lora1[64:128, sl], xs[64:128, :], AF.Copy, ["xs12"], ["lora1b"])

                def f13(d, mu, p, rd, sl=sl):
                    xs = A_l12
                    stt(xs, d, mu, p, ALU.mult, ALU.add, rd, ["xs12"])
                    act(lora2[:, sl], xs, AF.Sigmoid, ["xs12"], ["lora2"])

                proj_shift(12, tg, f12)
                proj_shift(13, tg, f13)

            if stop == 'lora':
                S.barrier(); S.emit(); return nc
            rT, kT, vT = A.f32(512), A.f32(512), A.f32(512)
            sw, ia, gT4 = A.f32(512), A.f32(512), A.f32(S_LEN)
            Ls, D1, D2, kk0, kk, kp, bv = [A.f32(512) for _ in range(7)]
            E1, E2, E3, E4 = [A.f32(512) for _ in range(4)]
            rn = A.f32(512)
            sqb, rkr = A.bf(512), A.bf(512)
            bonus4 = A.f32(S_LEN)
            ART = A.bf(4 * 256)
            ART4 = ART.rearrange("p (c w t) -> p c w t", w=2, t=128)
            KBT = A.bf(4 * 256)
            KBT4 = KBT.rearrange("p (c w t) -> p c w t", w=2, t=128)
            kbarT, bbarT, vbT = A.bf(512), A.bf(512), A.bf(512)
            KBtm = A.bf(4 * 256)
            KBtm4 = KBtm.rearrange("p (c w n) -> p c w n", w=2, n=128)
            Vtm = A.bf(4 * 128)
            Vtm3 = Vtm.rearrange("p (c n) -> p c n", n=128)
            VtmZ = A.bf(4 * 256)
            VtmZ4 = VtmZ.rearrange("p (c h n) -> p c h n", h=2, n=128)
            sc = A.f32(8)
            sc3 = sc.rearrange("p (c w) -> p c w", w=2)
            AM = A.bf(2 * 512)
            AM3 = AM.rearrange("p (h n) -> p h n", n=512)
            NTt = A.bf(256)
            NT3 = NTt.rearrange("p (h n) -> p h n", n=128)
            ZT = A.bf(512)
            ZT3 = ZT.rearrange("p (h n) -> p h n", n=256)
            Wb, Ub = A.bf(128), A.bf(128)
            UbZ = A.bf(256)
            UbZ3 = UbZ.rearrange("p (h n) -> p h n", n=128)
            Hf = A.f32(64)
            HbP = A.bf(128)
            YT = A.f32(512)
            yb, sq2 = A.bf(512), A.bf(512)
            cen, sd = A.f32(512), A.f32(512)

            for i in range(4):
                S.op("pool", lambda e: e.memset(VtmZ, 0.0), (), ["VtmZ"])
                S.op("pool", lambda e: e.memset(UbZ, 0.0), (), ["UbZ"])
                S.op("pool", lambda e: e.memset(HbP, 0.0), (), ["HbP"])
                S.op("pool", lambda e: e.memset(Hf, 0.0), (), ["Hf"])
                for tg in range(4):
                    sl = slice(tg * 512, (tg + 1) * 512)

                    def mk(dst, key):
                        def f(d, mu, p, rd):
                            stt(dst, d, mu, p, ALU.mult, ALU.add, rd, [key])
                        return f

                    proj_shift(0 + i, tg, mk(rT, "rT"))
                    proj_shift(4 + i, tg, mk(kT, "kT"))
                    proj_shift(8 + i, tg, mk(vT, "vT"))
                    k = ps()
                    mm(psb[k][:], w2a2[0:64, 128 * i:128 * (i + 1)], lora1[0:64, sl], True, True, ["w2a2a", "lora1a"], [P(k)])
                    act(sw, psb[k][:], AF.Sigmoid, [P(k), "cols"], ["sw"], bias=cols[:, CV_W0 + i:CV_W0 + i + 1])
                    k = ps()
                    mm(psb[k][:], w2a2[64:128, 128 * i:128 * (i + 1)], lora1[64:128, sl], True, True, ["w2a2b", "lora1b"], [P(k)])
                    act(ia, psb[k][:], AF.Sigmoid, [P(k), "cols"], ["ia"], bias=cols[:, CV_A0 + i:CV_A0 + i + 1])
                    k = ps()
                    mm(psb[k][:], g2b[:, 128 * i:128 * (i + 1)], lora2[:, sl], True, True, ["g2b", "lora2"], [P(k)])
                    cp("act", gT4[:, sl], psb[k][:], [P(k)], ["gT4"])
                    S.op("dve", lambda e: e.tensor_tensor_scan(out=Ls, data0=constf[:], data1=sw, initial=0.0, op0=ALU.mult, op1=ALU.add), ["constf", "sw"], ["Ls"])
                    Ls3 = Ls.rearrange("p (c t) -> p c t", t=128)
                    tt("pool", D1.rearrange("p (c t) -> p c t", t=128), Ls3, Ls3[:, :, 63:64].to_broadcast([128, 4, 128]), ALU.subtract, ["Ls"], ["D1"])
                    act(E1, D1, AF.Exp, ["D1"], ["E1"], scale=-C0)
                    act(E3, D1, AF.Exp, ["D1"], ["E3"], scale=C0)
                    tt("pool", D2, D1, sw, ALU.subtract, ["D1", "sw"], ["D2"])
                    act(E2, D2, AF.Exp, ["D2"], ["E2"], scale=-C0)
                    tt("dve", D2.rearrange("p (c t) -> p c t", t=128), Ls3, Ls3[:, :, 127:128].to_broadcast([128, 4, 128]), ALU.subtract, ["Ls", "E2"], ["D2"])
                    act(E4, D2, AF.Exp, ["D2"], ["E4"], scale=C0)
                    act(sc3[:, :, 0:1], Ls3[:, :, 63:64], AF.Exp, ["Ls"], ["sc"], scale=-C0)
                    act(sc3[:, :, 1:2], Ls3[:, :, 127:128], AF.Exp, ["Ls"], ["sc"], scale=-C0)
                    chk("e1")
                    ts("dve", kk0, kT, cols[:, CV_KK + i:CV_KK + i + 1], ALU.mult, ["kT", "cols"], ["kk0"])
                    tt("pool", sqb, kk0, kk0, ALU.mult, ["kk0"], ["sqb"])
                    k = ps()
                    mm(psb[k][:], blk, sqb, True, True, ["sqb", "constb"], [P(k)])
                    act(rn, psb[k][:], AF.Sqrt, [P(k)], ["rn"])
                    ts("dve", rn, rn, 1e-12, ALU.max, ["rn"], ["rn"])
                    recip(rn, rn, ["rn"], ["rn"])
                    tt("dve", kk, kk0, rn, ALU.mult, ["kk0", "rn"], ["kk"])
                    ts("dve", kp, ia, -1.0, ALU.add, ["ia", "cols"], ["kp"], s2=cols[:, CV_KA + i:CV_KA + i + 1], op1=ALU.mult)
                    stt(kp, kp, 1.0, kT, ALU.add, ALU.mult, ["kp", "kT"], ["kp"])
                    tt("pool", bv, kk, ia, ALU.mult, ["kk", "ia"], ["bv"])
                    c3 = lambda ap: ap.rearrange("p (c t) -> p c t", t=128)
                    tt("dve", ART4[:, :, 1, :], c3(rT), c3(E1), ALU.mult, ["rT", "E1"], ["ART"])
                    stt(kk0, kk, -1.0, E2, ALU.mult, ALU.mult, ["kk", "E2"], ["kk0"])
                    cp("pool", ART4[:, :, 0, :], c3(kk0), ["kk0"], ["ART"])
                    tt("pool", KBT4[:, :, 1, :], c3(kp), c3(E3), ALU.mult, ["kp", "E3"], ["KBT"])
                    tt("dve", KBT4[:, :, 0, :], c3(bv), c3(E3), ALU.mult, ["bv", "E3"], ["KBT"])
                    tt("pool", kbarT, kp, E4, ALU.mult, ["kp", "E4"], ["kbarT"])
                    tt("dve", bbarT, bv, E4, ALU.mult, ["bv", "E4"], ["bbarT"])
                    cp("pool", vbT, vT, ["vT"], ["vbT"])
                    stt(rkr, rT, cols[:, CV_RK + i:CV_RK + i + 1], kp, ALU.mult, ALU.mult, ["rT", "kp", "cols"], ["rkr"])
                    k = ps()
                    mm(psb[k][:], blk, rkr, True, True, ["rkr", "constb"], [P(k)])
                    tt("dve", bonus4[:, sl], psb[k][:], vT, ALU.mult, [P(k), "vT"], ["bonus4"])
                    chk("e2")
                    for c in range(4):
                        k = ps()
                        pv = psb[k][:].bitcast(BF16)
                        cs = slice(c * 128, (c + 1) * 128)
                        tr(pv[:, 0:128], kbarT[:, cs], ["kbarT", "constb"], [P(k)])
                        tr(pv[:, 128:256], bbarT[:, cs], ["bbarT", "constb"], [P(k)])
                        tr(pv[:, 256:384], vbT[:, cs], ["vbT", "constb"], [P(k)])
                        cp("act", KBtm4[:, c, :, :], pv[:, 0:256].rearrange("p (w n) -> p w n", n=128), [P(k)], ["KBtm"])
                        cp("dve", Vtm3[:, c, :], pv[:, 256:384], [P(k)], ["Vtm"])
                        cp("act", VtmZ4[:, c, 0, 0:64], pv[:, 256:320], [P(k)], ["VtmZ"])
                        cp("dve", VtmZ4[:, c, 1, 64:128], pv[:, 320:384], [P(k)], ["VtmZ"])
                    chk("e3")
                    for c in range(4):
                        k = ps()
                        k2 = ps()
                        kn = ps()
                        for hh in range(2):
                            pb = 64 * hh
                            kk_b = k if hh == 0 else k2
                            rhs_ar = ART4[pb:pb + 64, c, :, :].rearrange("p w t -> p (w t)")
                            mm(psb[kk_b][:, 0:256], KBT4[pb:pb + 64, c, 0, :], rhs_ar, True, False, ["KBT", "ART"], [P(kk_b)])
                            mm(psb[kk_b][:, 256:512], KBT4[pb:pb + 64, c, 1, :], rhs_ar, False, True, ["KBT", "ART"], [P(kk_b)])
                            knh = kn if hh == 0 else ps()
                            mm(psb[knh][:, 0:128], ART4[pb:pb + 64, c, 0, :], KBT4[pb:pb + 64, c, 0, :], True, True, ["KBT", "ART"], [P(knh)])
                            tt("dve", AM3[:, hh, :], psb[kk_b][:], M4, ALU.mult, [P(kk_b), "constb"], ["AM"])
                            tt("dve", NT3[:, hh, :], psb[knh][:, 0:128], ML2[:, 0:128], ALU.mult, [P(knh), "constb"], ["NT"])
                        chk("e4")
                        cp("dve", ZT3[:, :, 0:128], AM3[:, :, 0:128], ["AM"], ["ZT"])
                        tt("dve", ZT3[:, :, 128:256], AM3[:, :, 0:128], I2.rearrange("p (h n) -> p h n", n=128), ALU.add, ["AM", "constb"], ["ZT"])
                        chk("e4b")
                        for m in range(7):
                            if m == 1:
                                chk("e4c")
                            if m == 2:
                                chk("e4d")
                            kz = ps()
                            kn2 = ps()
                            if m == 0:
                                for hh in range(2):
                                    mm(psb[kz][:, 256 * hh:256 * hh + 128], NT3[:, hh, :], ZT3[:, hh, 0:128], hh == 0, hh == 1, ["NT", "ZT"], [P(kz)])
                            elif m < 6:
                                for hh in range(2):
                                    mm(psb[kz][:, 256 * hh:256 * hh + 256], NT3[:, hh, :], ZT3[:, hh, :], hh == 0, hh == 1, ["NT", "ZT"], [P(kz)])
                            else:
                                for hh in range(2):
                                    mm(psb[kz][:, 256 * hh + 128:256 * hh + 256], NT3[:, hh, :], ZT3[:, hh, 128:256], hh == 0, hh == 1, ["NT", "ZT"], [P(kz)])
                            if m < 6:
                                for hh in range(2):
                                    mm(psb[kn2][:, 128 * hh:128 * hh + 128], ZT3[:, hh, 0:128], NT3[:, hh, :], hh == 0, hh == 1, ["NT", "ZT"], [P(kn2)])
                            pz3 = psb[kz][:].rearrange("p (h n) -> p h n", n=256)
                            if m >= 1:
                                tt("dve", ZT3[:, :, 128:256], ZT3[:, :, 128:256], pz3[:, :, 128:256], ALU.add, [P(kz), "ZT"], ["ZT"])
                            if m < 6:
                                cp("act", ZT3[:, :, 0:128], pz3[:, :, 0:128], [P(kz)], ["ZT"])
                                cp("act", NTt, psb[kn2][:, 0:256], [P(kn2)], ["NT"])
                        chk("e5")
                        if not (tg == 0 and c == 0):
                            nsc = sc3[:, c, 0:1]
                            ts("dve", HbP[0:64, 0:64], Hf[0:64, :], nsc[0:64], ALU.mult, ["Hf", "sc"], ["HbP"])
                            ts("dve", HbP[64:128, 64:128], Hf[64:128, :], nsc[64:128], ALU.mult, ["Hf", "sc"], ["HbP"])
                        kw = ps()
                        mm(psb[kw][:, 0:128], ART4[:, c, 0, :], HbP, True, False, ["ART", "HbP"], [P(kw)])
                        for hh in range(2):
                            mm(psb[kw][:, 64 * hh:64 * hh + 64], AM3[:, hh, 256:384], Vtm3[:, c, 64 * hh:64 * hh + 64], False, hh == 1, ["AM", "Vtm"], [P(kw)])
                        cp("act", Wb, psb[kw][:, 0:128], [P(kw)], ["Wb"])
                        ku = ps()
                        for hh in range(2):
                            mm(psb[ku][:, 64 * hh:64 * hh + 64], ZT3[:, hh, 128:256], Wb[:, 64 * hh:64 * hh + 64], hh == 0, hh == 1, ["ZT", "Wb"], [P(ku)])
                        cp("act", Ub, psb[ku][:, 0:128], [P(ku)], ["Ub"])
                        cp("dve", UbZ3[:, 0, 0:64], psb[ku][:, 0:64], [P(ku)], ["UbZ"])
                        cp("dve", UbZ3[:, 1, 64:128], psb[ku][:, 64:128], [P(ku)], ["UbZ"])
                        chk("e6")
                        ky = ps()
                        mm(psb[ky][:, 0:128], HbP, ART4[:, c, 1, :], True, False, ["HbP", "ART"], [P(ky)])
                        for hh in range(2):
                            mm(psb[ky][:, 0:128], UbZ3[:, hh, :], AM3[:, hh, 128:256], False, False, ["UbZ", "AM"], [P(ky)])
                            mm(psb[ky][:, 0:128], VtmZ4[:, c, hh, :], AM3[:, hh, 384:512], False, hh == 1, ["VtmZ", "AM"], [P(ky)])
                        cp("act", YT[:, c * 128:(c + 1) * 128], psb[ky][:, 0:128], [P(ky)], ["YT"])
                        kh = ps()
                        mm(psb[kh][:, 0:128], KBtm4[:, c, 1, :], Ub, True, False, ["KBtm", "Ub"], [P(kh)])
                        mm(psb[kh][:, 0:128], KBtm4[:, c, 0, :], Vtm3[:, c, :], False, True, ["KBtm", "Vtm"], [P(kh)])
                        ts("dve", Hf, Hf, sc3[:, c, 1:2], ALU.mult, ["Hf", "sc"], ["Hf"])
                        tt("dve", Hf[0:64, :], Hf[0:64, :], psb[kh][0:64, 0:64], ALU.add, ["Hf", P(kh)], ["Hf"])
                        tt("dve", Hf[64:128, :], Hf[64:128, :], psb[kh][64:128, 64:128], ALU.add, ["Hf", P(kh)], ["Hf"])
                    chk("e7")
                    cp("pool", yb, YT, ["YT"], ["yb"])
                    k = ps()
                    mm(psb[k][:], blk, yb, True, True, ["yb", "constb"], [P(k)])
                    stt(cen, psb[k][:], -1.0 / 64, YT, ALU.mult, ALU.add, [P(k), "YT"], ["cen"])
                    tt("pool", sq2, cen, cen, ALU.mult, ["cen"], ["sq2"])
                    k = ps()
                    mm(psb[k][:], blk, sq2, True, True, ["sq2", "constb"], [P(k)])
                    act(sd, psb[k][:], AF.Sqrt, [P(k)], ["sd"], bias=64e-5, scale=1.0 / 64)
                    recip(sd, sd, ["sd"], ["sd"])
                    tt("dve", cen, cen, sd, ALU.mult, ["cen", "sd"], ["cen"])
                    ts("dve", cen, cen, cols[:, CV_LG + i:CV_LG + i + 1], ALU.mult, ["cen", "cols"], ["cen"], s2=cols[:, CV_LB + i:CV_LB + i + 1], op1=ALU.add)
                    tt("pool", cen, cen, bonus4[:, sl], ALU.add, ["cen", "bonus4"], ["cen"])
                    tt("dve", mix3[:, 4 + i, sl], cen, gT4[:, sl], ALU.mult, ["cen", "gT4"], ["mixT"])
            if stop == 'pr':
                S.barrier(); A.reset(); dbgf = A.f32(S_LEN)
                for cc in range(8):
                    cp("dve", dbgf, mix3[:, cc, :], ["mixT"], ["dbgf"])
                    dma("sp", dbg_mix[:, cc * S_LEN:(cc + 1) * S_LEN], dbgf, reads=["dbgf"])
                S.barrier(); S.emit(); return nc
            if dbg and s == 0:
                S.barrier()
                A.reset()
                dbgf = A.f32(S_LEN)
                for cc in range(8):
                    cp("dve", dbgf, mix3[:, cc, :], ["mixT"], ["dbgf"])
                    dma("sp", dbg_mix[:, cc * S_LEN:(cc + 1) * S_LEN], dbgf, reads=["dbgf"])

            S.barrier()
            A.reset()
            wo = A.bf(8 * D)
            wo3 = wo.rearrange("p (c n) -> p c n", n=D)
            for kc in range(8):
                dma("pool", wo3[:, kc, :], w_out[kc * 128:(kc + 1) * 128, :], writes=["wo%d" % kc])
            WO = ["wo%d" % kc for kc in range(8)]
            gbc2 = A.f32(D)
            dma("sp", gbc2, g2v[0, :].partition_broadcast(128), writes=["gbc"])
            x1 = A.f32(8 * D)
            x13 = x1.rearrange("p (t n) -> p t n", n=D)
            xn2T = A.bf(8 * 1024)
            xn23 = xn2T.rearrange("p (c t) -> p c t", t=1024)
            hT = A.bf(11 * 1024)
            hT3 = hT.rearrange("p (f t) -> p f t", t=1024)
            wd = A.bf(11 * D)
            wd3 = wd.rearrange("p (f n) -> p f n", n=D)
            wgs = [A.bf(8 * 128) for _ in range(2)]
            wus = [A.bf(8 * 128) for _ in range(2)]
            scr2 = A.bf(D)
            sms2 = [A.f32(4) for _ in range(2)]
            xsb2 = [A.bf(D) for _ in range(2)]
            sgs = [A.f32(512) for _ in range(2)]
            for half in range(2):
                ht0 = tok0 + half * 1024
                for tt_ in range(8):
                    t0 = half * 1024 + tt_ * 128
                    xk = "x1_%d" % tt_
                    dma("sp", x13[:, tt_, :], x[ht0 + tt_ * 128: ht0 + (tt_ + 1) * 128, :], writes=[xk])
                    for nh in range(2):
                        k = ps()
                        for kc in range(8):
                            mm(psb[k][:], mix3[:, kc, t0:t0 + 128], wo3[:, kc, nh * 512:(nh + 1) * 512], kc == 0, kc == 7, ["mixT"] + WO, [P(k)])
                        tt("dve", x13[:, tt_, nh * 512:(nh + 1) * 512], x13[:, tt_, nh * 512:(nh + 1) * 512], psb[k][:], ALU.add, [xk, P(k)], [xk])
                    b = tt_ % 2
                    rms_to_T(x13[:, tt_, :], xk, gbc2, xn23, "xn2T", tt_ * 128, scr2, "scr2", sms2[b], "sm2%d" % b, xsb2[b], "xsb2%d" % b)
                for fh in range(2):
                    for fi in range(11):
                        f = fh * 11 + fi
                        dma("pool", wd3[:, fi, :], w_down[f * 128:(f + 1) * 128, :], writes=["wd%d" % fi])
                    for fi in range(11):
                        f = fh * 11 + fi
                        b = fi % 2
                        wg3 = wgs[b].rearrange("p (c n) -> p c n", n=128)
                        wu3 = wus[b].rearrange("p (c n) -> p c n", n=128)
                        dma("pool", wg3, w_gate[:, f * 128:(f + 1) * 128].rearrange("(c p) n -> p c n", p=128), writes=["wg%d" % b])
                        dma("pool", wu3, w_up[:, f * 128:(f + 1) * 128].rearrange("(c p) n -> p c n", p=128), writes=["wu%d" % b])
                        for tg in range(2):
                            kg = ps()
                            ku = ps()
                            for kc in range(8):
                                mm(psb[kg][:], wg3[:, kc, :], xn23[:, kc, tg * 512:(tg + 1) * 512], kc == 0, kc == 7, ["wg%d" % b, "xn2T"], [P(kg)])
                            for kc in range(8):
                                mm(psb[ku][:], wu3[:, kc, :], xn23[:, kc, tg * 512:(tg + 1) * 512], kc == 0, kc == 7, ["wu%d" % b, "xn2T"], [P(ku)])
                            sb_ = tg
                            act(sgs[sb_], psb[kg][:], AF.Silu, [P(kg)], ["sg%d" % sb_])
                            tt("dve", hT3[:, fi, tg * 512:(tg + 1) * 512], sgs[sb_], psb[ku][:], ALU.mult, ["sg%d" % sb_, P(ku)], ["hT%d" % fi])
                    for tt_ in range(8):
                        xk = "x1_%d" % tt_
                        for nh in range(2):
                            k = ps()
                            for fi in range(11):
                                mm(psb[k][:], hT3[:, fi, tt_ * 128:(tt_ + 1) * 128], wd3[:, fi, nh * 512:(nh + 1) * 512], fi == 0, fi == 10, ["hT%d" % fi, "wd%d" % fi], [P(k)])
                            tt("dve", x13[:, tt_, nh * 512:(nh + 1) * 512], x13[:, tt_, nh * 512:(nh + 1) * 512], psb[k][:], ALU.add, [xk, P(k)], [xk])
                        if fh == 1:
                            dma("sp", out[ht0 + tt_ * 128: ht0 + (tt_ + 1) * 128, :], x13[:, tt_, :], reads=[xk])
        S.barrier()
        S.emit()
    return nc


_NC_CACHE = {}


def kernel(**inputs):
    inp = {k: np.asarray(v) for k, v in inputs.items()}
    ncores = 8
    nseq = 2
    if "nc" not in _NC_CACHE:
        _NC_CACHE["nc"] = build(nseq)
    nc = _NC_CACHE["nc"]
    x = np.ascontiguousarray(inp["x"], dtype=np.float32)
    B = x.shape[0]
    sq = lambda k: np.ascontiguousarray(inp[k][0], dtype=np.float32)
    shared = {
        "w_in": sq("w_in"), "w_out": sq("w_out"), "w_gate": sq("w_gate"), "w_up": sq("w_up"), "w_down": sq("w_down"),
        "norm1_g": sq("norm1_g").reshape(1, D), "norm2_g": sq("norm2_g").reshape(1, D),
        "w2": sq("w2"), "a2": sq("a2"), "g2": sq("g2"),
        "consts": make_consts(), "amask": make_amask(),
        "cols": make_cols({k: inp[k][0] for k in ("rwkv_mu", "w0", "a0", "k_k", "k_a", "r_k", "lnx_g", "lnx_b", "q_norm_g", "k_norm_g", "attn_out_g")}),
    }
    in_maps = []
    for c in range(ncores):
        m = dict(shared)
        m["x"] = x[c * nseq:(c + 1) * nseq].reshape(nseq * S_LEN, D)
        in_maps.append(m)
    res = run_bass_kernel_spmd(nc, in_maps, core_ids=list(range(ncores)))
    outs = [np.asarray(r["out"]).reshape(nseq, S_LEN, D) for r in res.results]
    return np.concatenate(outs, axis=0).astype(np.float32)
```

```python
import contextlib
import numpy as np
import concourse.bass as bass
import concourse.mybir as mybir
from concourse.bass_utils import run_bass_kernel_spmd

F32 = mybir.dt.float32
BF16 = mybir.dt.bfloat16
ALU = mybir.AluOpType
AF = mybir.ActivationFunctionType
AX = mybir.AxisListType
COMPUTE = ("pe", "act", "dve", "pool")

S_LEN = 2048
D = 1024
DFF = 2816
C0 = float(np.exp(-0.5))


class Sched:
    def __init__(self, nc, stack, n_dma_sems=24):
        self.nc = nc
        self.eng_names = ["pe", "act", "dve", "pool", "sp"]
        self.ops = {e: [] for e in self.eng_names}
        self.sem = {e: stack.enter_context(nc.semaphore("s_" + e)) for e in COMPUTE}
        self.cnt = {e: 0 for e in COMPUTE}
        self.dma_sems = [stack.enter_context(nc.semaphore("s_dma%d" % i)) for i in range(n_dma_sems)]
        self.dma_val = [0] * n_dma_sems
        self.dma_rr = 0
        self.res_w = {}
        self.res_r = {}
        self.waited = {e: {} for e in self.eng_names}

    def _need(self, eng, waits, ev):
        sid, sh, val = ev
        if self.waited[eng].get(sid, 0) >= val:
            return
        cur = waits.get(sid)
        if cur is None or cur[1] < val:
            waits[sid] = (sh, val)

    def op(self, eng, fn, reads=(), writes=(), dma=False):
        waits = {}
        own = None if dma else ("c", eng)
        for k in reads:
            w = self.res_w.get(k)
            if w is not None:
                self._need(eng, waits, w)
            if isinstance(k, tuple) and k[0] == "ps":
                for r in self.res_r.get(k, ()):
                    if r[0] != own:
                        self._need(eng, waits, r)
        for k in writes:
            w = self.res_w.get(k)
            if w is not None and (dma or w[0] != own):
                self._need(eng, waits, w)
            for r in self.res_r.get(k, ()):
                if dma or r[0] != own:
                    self._need(eng, waits, r)
        if dma:
            k = self.dma_rr
            self.dma_rr = (self.dma_rr + 1) % len(self.dma_sems)
            if self.dma_val[k] > 0:
                self._need(eng, waits, (("d", k), self.dma_sems[k], self.dma_val[k]))
            self.dma_val[k] += 16
            ev = (("d", k), self.dma_sems[k], self.dma_val[k])
            inc = 16
        else:
            self.cnt[eng] += 1
            ev = (("c", eng), self.sem[eng], self.cnt[eng])
            inc = 1
        for sid, (sh, val) in waits.items():
            self.waited[eng][sid] = val
        self.ops[eng].append((fn, list(waits.values()), ev[1], inc))
        for k in reads:
            self.res_r.setdefault(k, []).append(ev)
        for k in writes:
            self.res_w[k] = ev
            self.res_r[k] = []
        return ev

    def barrier(self):
        for eng in self.eng_names:
            waits = {}
            for e2 in COMPUTE:
                if self.cnt[e2] > 0 and e2 != eng:
                    self._need(eng, waits, (("c", e2), self.sem[e2], self.cnt[e2]))
            for k, v in enumerate(self.dma_val):
                if v > 0:
                    self._need(eng, waits, (("d", k), self.dma_sems[k], v))
            for sid, (sh, val) in waits.items():
                self.waited[eng][sid] = val
            self.ops[eng].append((None, list(waits.values()), None, 0))
        self.res_w = {}
        self.res_r = {}

    def emit(self):
        sched = self

        def run(engname, engine):
            for fn, waits, sh, inc in sched.ops[engname]:
                for (wsh, val) in waits:
                    engine.wait_ge(wsh, val)
                if fn is not None:
                    fn(engine).then_inc(sh, inc)

        with self.nc.Block() as block:
            @block.tensor
            def _(e):
                run("pe", e)

            @block.scalar
            def _(e):
                run("act", e)

            @block.vector
            def _(e):
                run("dve", e)

            @block.gpsimd
            def _(e):
                run("pool", e)

            @block.sync
            def _(e):
                run("sp", e)


class Arena:
    def __init__(self, t, n):
        self.t, self.n, self.off = t, n, 0

    def reset(self):
        self.off = 0

    def bf(self, n):
        n = (n + 1) // 2 * 2
        a = self.t[:, self.off:self.off + n]
        self.off += n
        assert self.off <= self.n, ("arena overflow", self.off, self.n)
        return a

    def f32(self, n):
        return self.bf(2 * n).bitcast(F32)


NC_ID, NC_BLK, NC_M4, NC_ML4, NC_I2, NC_RST, NC_WN = 0, 128, 256, 768, 1280, 1536, 2048
NCONST = 2048 + 64


def make_consts():
    c = np.zeros((128, NCONST), np.float32)
    c[:, NC_ID:NC_ID + 128] = np.eye(128)
    c[0:64, NC_BLK:NC_BLK + 64] = 1.0
    c[64:128, NC_BLK + 64:NC_BLK + 128] = 1.0
    p = np.arange(128)[:, None]
    f = np.arange(128)[None, :]
    mus = (p < f).astype(np.float32)
    mui = (p <= f).astype(np.float32)
    mls = (p > f).astype(np.float32)
    c[:, NC_M4:NC_M4 + 512] = np.concatenate([mus, mui, mus, mui], 1)
    c[:, NC_ML4:NC_ML4 + 512] = np.concatenate([mls] * 4, 1)
    c[:, NC_I2:NC_I2 + 256] = np.concatenate([np.eye(128)] * 2, 1)
    r = np.ones((128, 512), np.float32)
    r[:, 0::128] = 0.0
    c[:, NC_RST:NC_RST + 512] = r
    c[0:64, NC_WN:NC_WN + 64] = 1.0 / 64
    c[64, NC_WN:NC_WN + 64] = 1e-6
    return c


def make_amask():
    slopes = np.exp2(-8.0 * np.arange(1, 9, dtype=np.float64) / 8)
    ki = np.arange(128)[:, None]
    cc = np.arange(2048)[None, :]
    dl = cc - ki
    m = ((dl >= 0) & (dl <= 128)).astype(np.float64) + ((dl >= 0) & (dl % 4 == 0) & (dl <= 512)) + ((dl >= 0) & (dl % 16 == 0))
    out = np.zeros((8, 128, 2048), np.float32)
    for h in range(8):
        out[h] = m * np.exp(-slopes[h] * np.maximum(dl, 0))
    return out


CV_MU, CV_W0, CV_A0, CV_KK, CV_KA, CV_RK, CV_LG, CV_LB, CV_GQ, CV_GK, CV_GA = 0, 14, 18, 22, 26, 30, 34, 38, 42, 43, 44
NCV = 52


def make_cols(inp):
    cv = np.zeros((128, NCV), np.float32)

    def put(off, v):
        v = np.asarray(v, np.float32).reshape(-1, 128)
        cv[:, off:off + v.shape[0]] = v.T

    put(CV_MU, inp["rwkv_mu"])
    put(CV_W0, inp["w0"])
    put(CV_A0, inp["a0"])
    put(CV_KK, inp["k_k"])
    put(CV_KA, inp["k_a"])
    put(CV_RK, inp["r_k"])
    put(CV_LG, inp["lnx_g"])
    put(CV_LB, inp["lnx_b"])
    put(CV_GQ, np.tile(np.asarray(inp["q_norm_g"], np.float32).reshape(-1), 2))
    put(CV_GK, np.tile(np.asarray(inp["k_norm_g"], np.float32).reshape(-1), 2))
    ga = np.asarray(inp["attn_out_g"], np.float32).reshape(8, 64)
    cv[0:64, CV_GA:CV_GA + 8] = ga.T
    return cv


class StopBuild(Exception):
    pass


def build(NSEQ=2, dbg=False, stop=None):
    try:
        return _build(NSEQ, dbg, stop)
    except StopBuild as e:
        return e.args[0]


def _build(NSEQ=2, dbg=False, stop=None):
    nc = bass.Bass("TRN2", target_bir_lowering=False)
    NT = NSEQ * S_LEN

    def din(name, shape):
        return nc.dram_tensor(name, shape, F32, kind="ExternalInput").ap()

    x = din("x", [NT, D])
    w_in = din("w_in", [D, 3328])
    w_out = din("w_out", [D, D])
    w_gate = din("w_gate", [D, DFF])
    w_up = din("w_up", [D, DFF])
    w_down = din("w_down", [DFF, D])
    g1v = din("norm1_g", [1, D])
    g2v = din("norm2_g", [1, D])
    w2d = din("w2", [64, 512])
    a2d = din("a2", [64, 512])
    g2d = din("g2", [128, 512])
    consts_d = din("consts", [128, NCONST])
    amask_d = din("amask", [8, 128, 2048])
    cols_d = din("cols", [128, NCV])
    out = nc.dram_tensor("out", [NT, D], F32, kind="ExternalOutput").ap()
    if dbg:
        dbg_mix = nc.dram_tensor("dbg_mix", [128, 8 * S_LEN], F32, kind="ExternalOutput").ap()

    with contextlib.ExitStack() as st:
        S = Sched(nc, st)
        T = lambda n, s, d: st.enter_context(nc.sbuf_tensor(n, s, d))
        psb = [st.enter_context(nc.psum_tensor("ps%d" % i, [128, 512], F32)) for i in range(8)]
        rr = {"i": 0}

        def ps(pool=(0, 1, 2, 3, 4, 5, 6, 7)):
            k = pool[rr["i"] % len(pool)]
            rr["i"] += 1
            return k

        ARN = 86000
        arena_t = T("arena", [128, ARN], BF16)
        A = Arena(arena_t, ARN)
        mixT = T("mixT", [128, 8 * S_LEN], BF16)
        mix3 = mixT[:].rearrange("p (c t) -> p c t", t=S_LEN)
        constb = T("constb", [128, NCONST], BF16)
        constf = T("constf", [128, 512], F32)
        cols = T("cols_sb", [128, NCV], F32)
        gq8 = T("gq8", [128, 1], F32)
        identb = constb[:, NC_ID:NC_ID + 128]
        blk = constb[:, NC_BLK:NC_BLK + 128]

        def dma(eng, o, i, reads=(), writes=()):
            S.op(eng, lambda e: e.dma_start(out=o, in_=i), reads, writes, dma=True)

        def mm(o, l, r, start, stop, reads, writes):
            S.op("pe", lambda e: e.matmul(o, lhsT=l, rhs=r, start=start, stop=stop, skip_group_check=True), reads, writes)

        def tr(o, i, reads, writes):
            S.op("pe", lambda e: e.transpose(o, i, identb), reads, writes)

        def act(o, i, func, reads, writes, bias=0.0, scale=1.0, accum=None):
            if accum is None:
                S.op("act", lambda e: e.activation(out=o, in_=i, func=func, bias=bias, scale=scale), reads, writes)
            else:
                S.op("act", lambda e: e.activation(out=o, in_=i, func=func, bias=bias, scale=scale, accum_out=accum), reads, writes)

        def tt(eng, o, a, b, op, reads, writes):
            S.op(eng, lambda e: e.tensor_tensor(out=o, in0=a, in1=b, op=op), reads, writes)

        def ts(eng, o, a, s1, op0, reads, writes, s2=None, op1=None):
            if op1 is None:
                S.op(eng, lambda e: e.tensor_scalar(out=o, in0=a, scalar1=s1, scalar2=None, op0=op0), reads, writes)
            else:
                S.op(eng, lambda e: e.tensor_scalar(out=o, in0=a, scalar1=s1, scalar2=s2, op0=op0, op1=op1), reads, writes)

        def stt(o, a, s, b, op0, op1, reads, writes):
            S.op("dve", lambda e: e.scalar_tensor_tensor(out=o, in0=a, scalar=s, in1=b, op0=op0, op1=op1), reads, writes)

        def cp(eng, o, i, reads, writes):
            if eng == "act":
                act(o, i, AF.Copy, reads, writes)
            else:
                S.op(eng, lambda e: e.tensor_copy(out=o, in_=i), reads, writes)

        def recip(o, i, reads, writes):
            S.op("dve", lambda e: e.reciprocal(out=o, in_=i), reads, writes)

        def P(k):
            return ("ps", k)

        def chk(name):
            if stop == name:
                S.barrier(); S.emit()
                raise StopBuild(nc)

        dma("pool", constb[:], consts_d, writes=["constb"])
        dma("sp", constf[:], consts_d[:, NC_RST:NC_RST + 512], writes=["constf"])
        dma("sp", cols[:], cols_d, writes=["cols"])
        ts("dve", gq8[:], cols[:, CV_GQ:CV_GQ + 1], 0.125, ALU.mult, ["cols"], ["gq8"])

        def rms_to_T(xt, xt_key, gbc, dstT, dst_key, t0, scr, scr_key, sm, sm_key, xsb, xsb_key):
            act(scr, xt, AF.Square, [xt_key], [scr_key, sm_key], accum=sm[:, 0:1])
            ts("dve", sm[:, 1:2], sm[:, 0:1], 1.0 / D, ALU.mult, [sm_key], [sm_key], s2=1e-6, op1=ALU.add)
            act(sm[:, 2:3], sm[:, 1:2], AF.Sqrt, [sm_key], [sm_key])
            recip(sm[:, 3:4], sm[:, 2:3], [sm_key], [sm_key])
            stt(xsb, xt, sm[:, 3:4], gbc, ALU.mult, ALU.mult, [xt_key, sm_key, "gbc"], [xsb_key])
            k = ps()
            pv = psb[k][:].bitcast(BF16)
            for kc in range(8):
                tr(pv[:, kc * 128:(kc + 1) * 128], xsb[:, kc * 128:(kc + 1) * 128], [xsb_key, "constb"], [P(k)])
            cp("act", dstT[:, :, t0:t0 + 128], pv.rearrange("p (c t) -> p c t", t=128), [P(k)], [dst_key])

        for s in range(NSEQ):
            tok0 = s * S_LEN
            S.barrier()
            A.reset()
            xnT = A.bf(8 * S_LEN)
            xn3 = xnT.rearrange("p (c t) -> p c t", t=S_LEN)
            mark_pa = A.off
            gbc = A.f32(D)
            dma("sp", gbc, g1v[0, :].partition_broadcast(128), writes=["gbc"])
            xts = [A.f32(D) for _ in range(2)]
            scr = A.bf(D)
            sms = [A.f32(4) for _ in range(2)]
            xsbs = [A.bf(D) for _ in range(2)]
            wqkv = A.bf(8 * 1536)
            wq3 = wqkv.rearrange("p (c n) -> p c n", n=1536)
            for kc in range(8):
                dma("pool", wq3[:, kc, :], w_in[kc * 128:(kc + 1) * 128, 0:1536], writes=["wqkv%d" % kc])
            amask = A.bf(8 * 2048)
            am3 = amask.rearrange("p (h c) -> p h c", c=2048)
            for h in range(8):
                dma("pool", am3[:, h, :], amask_d[h], writes=["amask%d" % h])
            for ti in range(16):
                b = ti % 2
                dma("sp", xts[b], x[tok0 + ti * 128: tok0 + (ti + 1) * 128, :], writes=["xt%d" % b])
                rms_to_T(xts[b], "xt%d" % b, gbc, xn3, "xnT", ti * 128, scr, "scr", sms[b], "sm%d" % b, xsbs[b], "xsb%d" % b)
            if stop in ('p0', 'p0d'):
                if stop == 'p0d':
                    S.barrier(); A.reset(); dbgf = A.f32(S_LEN)
                    for cc in range(8):
                        cp("dve", dbgf, mix3[:, cc, :], ["mixT"], ["dbgf"])
                        dma("sp", dbg_mix[:, cc * S_LEN:(cc + 1) * S_LEN], dbgf, reads=["dbgf"])
                S.barrier(); S.emit(); return nc
            WQ = ["wqkv%d" % kc for kc in range(8)]
            vaug = A.bf(16 * 8 * 65)
            va4 = vaug.rearrange("p (j h e) -> p j h e", h=8, e=65)
            S.op("pool", lambda e: e.memset(vaug, 1.0), (), ["vaug"])
            for j in range(16):
                k = ps((0, 1, 2, 3))
                for kc in range(8):
                    mm(psb[k][:], xn3[:, kc, j * 128:(j + 1) * 128], wq3[:, kc, 1024:1536], kc == 0, kc == 7, ["xnT"] + WQ, [P(k)])
                cp("act" if j % 2 else "dve", va4[:, j, :, 0:64], psb[k][:].rearrange("p (h e) -> p h e", e=64), [P(k)], ["vaug"])
            if stop == 'v':
                S.barrier(); S.emit(); return nc
            qts = [A.bf(S_LEN) for _ in range(2)]
            kts = [A.bf(S_LEN) for _ in range(2)]
            sqs = [A.bf(512) for _ in range(2)]
            rss = [A.f32(512) for _ in range(2)]
            ebs = [A.bf(512) for _ in range(4)]
            pbs = [A.bf(512) for _ in range(4)]
            wn = constb[0:65, NC_WN:NC_WN + 64]
            cnt = 0
            for i in range(4):
                qt, kt = qts[i % 2], kts[i % 2]
                qk, kk_ = "qt%d" % (i % 2), "kt%d" % (i % 2)
                for which, dst, dkey, gcol, c0 in ((0, qt, qk, gq8[:, 0:1], 0), (1, kt, kk_, cols[:, CV_GK:CV_GK + 1], 512)):
                    for tg in range(4):
                        k = ps((0, 1, 2, 3))
                        for kc in range(8):
                            mm(psb[k][:], wq3[:, kc, c0 + 128 * i: c0 + 128 * (i + 1)], xn3[:, kc, tg * 512:(tg + 1) * 512], kc == 0, kc == 7, ["xnT"] + WQ, [P(k)])
                        b = cnt % 2
                        cnt += 1
                        act(sqs[b], psb[k][:], AF.Square, [P(k)], ["sq%d" % b])
                        k2 = ps((0, 1, 2, 3))
                        mm(psb[k2][:], blk, sqs[b], True, True, ["sq%d" % b, "constb"], [P(k2)])
                        act(rss[b], psb[k2][:], AF.Sqrt, [P(k2)], ["rs%d" % b], bias=1e-6, scale=1.0 / 64)
                        recip(rss[b], rss[b], ["rs%d" % b], ["rs%d" % b])
                        stt(dst[:, tg * 512:(tg + 1) * 512], psb[k][:], gcol, rss[b], ALU.mult, ALU.mult, [P(k), "rs%d" % b, "gq8", "cols"], [dkey])
                for hh in range(2):
                    h = 2 * i + hh
                    pb = 64 * hh
                    ob = (4, 5, 6, 7)
                    ec = 0
                    groups = []
                    for j in range(16):
                        for g in range(j // 4, 4):
                            cst = max(128 * j, 512 * g)
                            groups.append((j, g, cst, 512 * (g + 1)))
                    LA = 3
                    for t in range(len(groups) + LA):
                        if t < len(groups):
                            j, g, cst, cen = groups[t]
                            n = cen - cst
                            k = ps((0, 1, 2, 3))
                            mm(psb[k][:, 0:n], kt[pb:pb + 64, j * 128:(j + 1) * 128], qt[pb:pb + 64, cst:cen], True, True, [qk, kk_], [P(k)])
                            b = t % 4
                            act(ebs[b][:, 0:n], psb[k][:, 0:n], AF.Exp, [P(k)], ["eb%d" % b])
                            tt("pool" if t % 2 else "dve", pbs[b][:, 0:n], ebs[b][:, 0:n], am3[:, h, cst - 128 * j: cen - 128 * j], ALU.mult, ["eb%d" % b, "amask%d" % h], ["pb%d" % b])
                        if t >= LA:
                            j, g, cst, cen = groups[t - LA]
                            n = cen - cst
                            b = (t - LA) % 4
                            mm(psb[ob[g]][0:65, cst - 512 * g: cen - 512 * g], va4[:, j, h, :], pbs[b][:, 0:n], j == 0, j == 4 * g + 3, ["vaug", "pb%d" % b], [P(ob[g])])
                    for g in range(4):
                        b = cnt % 2
                        cnt += 1
                        act(sqs[b][0:65, :], psb[ob[g]][0:65, :], AF.Square, [P(ob[g])], ["sq%d" % b])
                        k2 = ps((0, 1, 2, 3))
                        mm(psb[k2][0:64, :], wn, sqs[b][0:65, :], True, True, ["sq%d" % b, "constb"], [P(k2)])
                        act(rss[b][0:64, :], psb[k2][0:64, :], AF.Sqrt, [P(k2)], ["rs%d" % b])
                        recip(rss[b][0:64, :], rss[b][0:64, :], ["rs%d" % b], ["rs%d" % b])
                        stt(mix3[pb:pb + 64, i, g * 512:(g + 1) * 512], psb[ob[g]][0:64, :], cols[0:64, CV_GA + h:CV_GA + h + 1], rss[b][0:64, :], ALU.mult, ALU.mult,
                            [P(ob[g]), "rs%d" % b, "cols"], ["mixT"])

            if stop == 'pa':
                S.barrier(); A.reset(); dbgf = A.f32(S_LEN)
                for cc in range(8):
                    cp("dve", dbgf, mix3[:, cc, :], ["mixT"], ["dbgf"])
                    dma("sp", dbg_mix[:, cc * S_LEN:(cc + 1) * S_LEN], dbgf, reads=["dbgf"])
                S.barrier(); S.emit(); return nc
            S.barrier()
            A.off = mark_pa
            wr = A.bf(8 * 1792)
            wr3 = wr.rearrange("p (c n) -> p c n", n=1792)
            for kc in range(8):
                dma("pool", wr3[:, kc, :], w_in[kc * 128:(kc + 1) * 128, 1536:3328], writes=["wr%d" % kc])
            WR = ["wr%d" % kc for kc in range(8)]
            w2a2 = A.bf(512)
            g2b = A.bf(512)
            dma("pool", w2a2[0:64, :], w2d, writes=["w2a2a"])
            dma("pool", w2a2[64:128, :], a2d, writes=["w2a2b"])
            dma("pool", g2b, g2d, writes=["g2b"])
            lora1 = A.bf(S_LEN)
            lora2 = A.bf(S_LEN)
            halo = A.f32(16)
            ptsb = [A.f32(514) for _ in range(2)]
            dtmp = [A.f32(512) for _ in range(2)]
            M4 = constb[:, NC_M4:NC_M4 + 512]
            ML2 = constb[:, NC_ML4:NC_ML4 + 256]
            I2 = constb[:, NC_I2:NC_I2 + 256]
            pcount = [0]

            def proj_shift(cj, tg, dst_fn):
                k = ps()
                for kc in range(8):
                    mm(psb[k][:], wr3[:, kc, cj * 128:(cj + 1) * 128], xn3[:, kc, tg * 512:(tg + 1) * 512], kc == 0, kc == 7, ["xnT"] + WR, [P(k)])
                b = pcount[0] % 2
                pcount[0] += 1
                pk = "ptsb%d" % b
                if tg == 0:
                    S.op("pool", lambda e: e.memset(ptsb[b][:, 0:1], 0.0), (), [pk + "h"])
                else:
                    cp("pool", ptsb[b][:, 0:1], halo[:, cj:cj + 1], ["halo%d" % cj], [pk + "h"])
                cp("act", ptsb[b][:, 1:513], psb[k][:], [P(k)], [pk])
                cp("pool", halo[:, cj:cj + 1], ptsb[b][:, 512:513], [pk], ["halo%d" % cj])
                tt("pool", dtmp[b], ptsb[b][:, 0:512], ptsb[b][:, 1:513], ALU.subtract, [pk, pk + "h"], ["dtmp%d" % b])
                dst_fn(dtmp[b], cols[:, CV_MU + cj:CV_MU + cj + 1], ptsb[b][:, 1:513], ["dtmp%d" % b, pk, "cols"])

            A_l12 = A.f32(512)
            for tg in range(4):
                sl = slice(tg * 512, (tg + 1) * 512)
                tmpx = dtmp

                def f12(d, mu, p, rd, sl=sl):
                    xs = A_l12
                    stt(xs, d, mu, p, ALU.mult, ALU.add, rd, ["xs12"])
                    act(lora1[0:64, sl], xs[0:64, :], AF.Tanh, ["xs12"], ["lora1a"])
                    act(lora1[64:128, sl], xs[64:128, :], AF.Copy, ["xs12"], ["lora1b"])

                def f13(d, mu, p, rd, sl=sl):
                    xs = A_l12
                    stt(xs, d, mu, p, ALU.mult, ALU.add, rd, ["xs12"])
                    act(lora2[:, sl], xs, AF.Sigmoid, ["xs12"], ["lora2"])

                proj_shift(12, tg, f12)
                proj_shift(13, tg, f13)

            if stop == 'lora':
                S.barrier(); S.emit(); return nc
            rT, kT, vT = A.f32(512), A.f32(512), A.f32(512)
            sw, ia, gT4 = A.f32(512), A.f32(512), A.f32(S_LEN)
            Ls, D1, D2, kk0, kk, kp, bv = [A.f32(512) for _ in range(7)]
            E1, E2, E3, E4 = [A.f32(512) for _ in range(4)]
            rn = A.f32(512)
            sqb, rkr = A.bf(512), A.bf(512)
            bonus4 = A.f32(S_LEN)
            ART = A.bf(4 * 256)
            ART4 = ART.rearrange("p (c w t) -> p c w t", w=2, t=128)
            KBT = A.bf(4 * 256)
            KBT4 = KBT.rearrange("p (c w t) -> p c w t", w=2, t=128)
            kbarT, bbarT, vbT = A.bf(512), A.bf(512), A.bf(512)
            KBtm = A.bf(4 * 256)
            KBtm4 = KBtm.rearrange("p (c w n) -> p c w n", w=2, n=128)
            Vtm = A.bf(4 * 128)
            Vtm3 = Vtm.rearrange("p (c n) -> p c n", n=128)
            VtmZ = A.bf(4 * 256)
            VtmZ4 = VtmZ.rearrange("p (c h n) -> p c h n", h=2, n=128)
            sc = A.f32(8)
            sc3 = sc.rearrange("p (c w) -> p c w", w=2)
            AM3s = [A.bf(2 * 512).rearrange("p (h n) -> p h n", n=512) for _ in range(4)]
            NT3s = [A.bf(256).rearrange("p (h n) -> p h n", n=128) for _ in range(4)]
            ZT3s = [A.bf(512).rearrange("p (h n) -> p h n", n=256) for _ in range(4)]
            Wb, Ub = A.bf(128), A.bf(128)
            UbZ = A.bf(256)
            UbZ3 = UbZ.rearrange("p (h n) -> p h n", n=128)
            Hf = A.f32(64)
            HbP = A.bf(128)
            YT = A.f32(512)
            yb, sq2 = A.bf(512), A.bf(512)
            cen, sd = A.f32(512), A.f32(512)

            for i in range(4):
                S.op("pool", lambda e: e.memset(VtmZ, 0.0), (), ["VtmZ"])
                S.op("pool", lambda e: e.memset(UbZ, 0.0), (), ["UbZ"])
                S.op("pool", lambda e: e.memset(HbP, 0.0), (), ["HbP"])
                S.op("pool", lambda e: e.memset(Hf, 0.0), (), ["Hf"])
                for tg in range(4):
                    sl = slice(tg * 512, (tg + 1) * 512)

                    def mk(dst, key):
                        def f(d, mu, p, rd):
                            stt(dst, d, mu, p, ALU.mult, ALU.add, rd, [key])
                        return f

                    proj_shift(0 + i, tg, mk(rT, "rT"))
                    proj_shift(4 + i, tg, mk(kT, "kT"))
                    proj_shift(8 + i, tg, mk(vT, "vT"))
                    k = ps()
                    mm(psb[k][:], w2a2[0:64, 128 * i:128 * (i + 1)], lora1[0:64, sl], True, True, ["w2a2a", "lora1a"], [P(k)])
                    act(sw, psb[k][:], AF.Sigmoid, [P(k), "cols"], ["sw"], bias=cols[:, CV_W0 + i:CV_W0 + i + 1])
                    k = ps()
                    mm(psb[k][:], w2a2[64:128, 128 * i:128 * (i + 1)], lora1[64:128, sl], True, True, ["w2a2b", "lora1b"], [P(k)])
                    act(ia, psb[k][:], AF.Sigmoid, [P(k), "cols"], ["ia"], bias=cols[:, CV_A0 + i:CV_A0 + i + 1])
                    k = ps()
                    mm(psb[k][:], g2b[:, 128 * i:128 * (i + 1)], lora2[:, sl], True, True, ["g2b", "lora2"], [P(k)])
                    cp("act", gT4[:, sl], psb[k][:], [P(k)], ["gT4"])
                    S.op("dve", lambda e: e.tensor_tensor_scan(out=Ls, data0=constf[:], data1=sw, initial=0.0, op0=ALU.mult, op1=ALU.add), ["constf", "sw"], ["Ls"])
                    Ls3 = Ls.rearrange("p (c t) -> p c t", t=128)
                    tt("pool", D1.rearrange("p (c t) -> p c t", t=128), Ls3, Ls3[:, :, 63:64].to_broadcast([128, 4, 128]), ALU.subtract, ["Ls"], ["D1"])
                    act(E1, D1, AF.Exp, ["D1"], ["E1"], scale=-C0)
                    act(E3, D1, AF.Exp, ["D1"], ["E3"], scale=C0)
                    tt("pool", D2, D1, sw, ALU.subtract, ["D1", "sw"], ["D2"])
                    act(E2, D2, AF.Exp, ["D2"], ["E2"], scale=-C0)
                    tt("dve", D2.rearrange("p (c t) -> p c t", t=128), Ls3, Ls3[:, :, 127:128].to_broadcast([128, 4, 128]), ALU.subtract, ["Ls", "E2"], ["D2"])
                    act(E4, D2, AF.Exp, ["D2"], ["E4"], scale=C0)
                    act(sc3[:, :, 0:1], Ls3[:, :, 63:64], AF.Exp, ["Ls"], ["sc"], scale=-C0)
                    act(sc3[:, :, 1:2], Ls3[:, :, 127:128], AF.Exp, ["Ls"], ["sc"], scale=-C0)
                    chk("e1")
                    ts("dve", kk0, kT, cols[:, CV_KK + i:CV_KK + i + 1], ALU.mult, ["kT", "cols"], ["kk0"])
                    tt("pool", sqb, kk0, kk0, ALU.mult, ["kk0"], ["sqb"])
                    k = ps()
                    mm(psb[k][:], blk, sqb, True, True, ["sqb", "constb"], [P(k)])
                    act(rn, psb[k][:], AF.Sqrt, [P(k)], ["rn"])
                    ts("dve", rn, rn, 1e-12, ALU.max, ["rn"], ["rn"])
                    recip(rn, rn, ["rn"], ["rn"])
                    tt("dve", kk, kk0, rn, ALU.mult, ["kk0", "rn"], ["kk"])
                    ts("dve", kp, ia, -1.0, ALU.add, ["ia", "cols"], ["kp"], s2=cols[:, CV_KA + i:CV_KA + i + 1], op1=ALU.mult)
                    stt(kp, kp, 1.0, kT, ALU.add, ALU.mult, ["kp", "kT"], ["kp"])
                    tt("pool", bv, kk, ia, ALU.mult, ["kk", "ia"], ["bv"])
                    c3 = lambda ap: ap.rearrange("p (c t) -> p c t", t=128)
                    tt("dve", ART4[:, :, 1, :], c3(rT), c3(E1), ALU.mult, ["rT", "E1"], ["ART"])
                    stt(kk0, kk, -1.0, E2, ALU.mult, ALU.mult, ["kk", "E2"], ["kk0"])
                    cp("pool", ART4[:, :, 0, :], c3(kk0), ["kk0"], ["ART"])
                    tt("pool", KBT4[:, :, 1, :], c3(kp), c3(E3), ALU.mult, ["kp", "E3"], ["KBT"])
                    tt("dve", KBT4[:, :, 0, :], c3(bv), c3(E3), ALU.mult, ["bv", "E3"], ["KBT"])
                    tt("pool", kbarT, kp, E4, ALU.mult, ["kp", "E4"], ["kbarT"])
                    tt("dve", bbarT, bv, E4, ALU.mult, ["bv", "E4"], ["bbarT"])
                    cp("pool", vbT, vT, ["vT"], ["vbT"])
                    stt(rkr, rT, cols[:, CV_RK + i:CV_RK + i + 1], kp, ALU.mult, ALU.mult, ["rT", "kp", "cols"], ["rkr"])
                    k = ps()
                    mm(psb[k][:], blk, rkr, True, True, ["rkr", "constb"], [P(k)])
                    tt("dve", bonus4[:, sl], psb[k][:], vT, ALU.mult, [P(k), "vT"], ["bonus4"])
                    chk("e2")
                    for c in range(4):
                        k = ps()
                        pv = psb[k][:].bitcast(BF16)
                        cs = slice(c * 128, (c + 1) * 128)
                        tr(pv[:, 0:128], kbarT[:, cs], ["kbarT", "constb"], [P(k)])
                        tr(pv[:, 128:256], bbarT[:, cs], ["bbarT", "constb"], [P(k)])
                        tr(pv[:, 256:384], vbT[:, cs], ["vbT", "constb"], [P(k)])
                        cp("act", KBtm4[:, c, :, :], pv[:, 0:256].rearrange("p (w n) -> p w n", n=128), [P(k)], ["KBtm"])
                        cp("dve", Vtm3[:, c, :], pv[:, 256:384], [P(k)], ["Vtm"])
                        cp("act", VtmZ4[:, c, 0, 0:64], pv[:, 256:320], [P(k)], ["VtmZ"])
                        cp("dve", VtmZ4[:, c, 1, 64:128], pv[:, 320:384], [P(k)], ["VtmZ"])
                    chk("e3")
                    I2v = I2.rearrange("p (h n) -> p h n", n=128)
                    for c in range(4):
                        AM3, NT3, ZT3 = AM3s[c], NT3s[c], ZT3s[c]
                        for hh in range(2):
                            pb = 64 * hh
                            kk_b = ps()
                            knh = ps()
                            rhs_ar = ART4[pb:pb + 64, c, :, :].rearrange("p w t -> p (w t)")
                            mm(psb[kk_b][:, 0:256], KBT4[pb:pb + 64, c, 0, :], rhs_ar, True, False, ["KBT", "ART"], [P(kk_b)])
                            mm(psb[kk_b][:, 256:512], KBT4[pb:pb + 64, c, 1, :], rhs_ar, False, True, ["KBT", "ART"], [P(kk_b)])
                            mm(psb[knh][:, 0:128], ART4[pb:pb + 64, c, 0, :], KBT4[pb:pb + 64, c, 0, :], True, True, ["KBT", "ART"], [P(knh)])
                            tt("dve", AM3[:, hh, :], psb[kk_b][:], M4, ALU.mult, [P(kk_b), "constb"], ["AM%d" % c])
                            tt("dve", NT3[:, hh, :], psb[knh][:, 0:128], ML2[:, 0:128], ALU.mult, [P(knh), "constb"], ["NT%d" % c])
                        cp("pool", ZT3[:, 0, 0:128], AM3[:, 0, 0:128], ["AM%d" % c], ["ZT%d" % c])
                        cp("pool", ZT3[:, 1, 0:128], AM3[:, 1, 0:128], ["AM%d" % c], ["ZT%d" % c])
                        tt("dve", ZT3[:, :, 128:256], AM3[:, :, 0:128], I2v, ALU.add, ["AM%d" % c, "constb"], ["ZT%d" % c])
                    for m in range(7):
                        banks = []
                        for c in range(4):
                            NT3, ZT3 = NT3s[c], ZT3s[c]
                            kz = ps()
                            kn2 = ps() if m < 6 else None
                            banks.append((kz, kn2))
                            rd, = [["NT%d" % c, "ZT%d" % c]]
                            if m == 0:
                                for hh in range(2):
                                    mm(psb[kz][:, 256 * hh:256 * hh + 128], NT3[:, hh, :], ZT3[:, hh, 0:128], hh == 0, hh == 1, rd, [P(kz)])
                            elif m < 6:
                                for hh in range(2):
                                    mm(psb[kz][:, 256 * hh:256 * hh + 256], NT3[:, hh, :], ZT3[:, hh, :], hh == 0, hh == 1, rd, [P(kz)])
                            else:
                                for hh in range(2):
                                    mm(psb[kz][:, 256 * hh + 128:256 * hh + 256], NT3[:, hh, :], ZT3[:, hh, 128:256], hh == 0, hh == 1, rd, [P(kz)])
                            if m < 6:
                                for hh in range(2):
                                    mm(psb[kn2][:, 128 * hh:128 * hh + 128], ZT3[:, hh, 0:128], NT3[:, hh, :], hh == 0, hh == 1, rd, [P(kn2)])
                        for c in range(4):
                            NT3, ZT3 = NT3s[c], ZT3s[c]
                            kz, kn2 = banks[c]
                            pz3 = psb[kz][:].rearrange("p (h n) -> p h n", n=256)
                            if m >= 1:
                                tt("dve", ZT3[:, :, 128:256], ZT3[:, :, 128:256], pz3[:, :, 128:256], ALU.add, [P(kz), "ZT%d" % c], ["ZT%d" % c])
                            if m < 6:
                                cp("act", ZT3[:, :, 0:128], pz3[:, :, 0:128], [P(kz)], ["ZT%d" % c])
                                cp("act" if c % 2 else "dve", NT3[:, :, :], psb[kn2][:, 0:256].rearrange("p (h n) -> p h n", n=128), [P(kn2)], ["NT%d" % c])
                    for c in range(4):
                        AM3, ZT3 = AM3s[c], ZT3s[c]
                        AMk, ZTk = "AM%d" % c, "ZT%d" % c
                        if not (tg == 0 and c == 0):
                            nsc = sc3[:, c, 0:1]
                            ts("dve", HbP[0:64, 0:64], Hf[0:64, :], nsc[0:64], ALU.mult, ["Hf", "sc"], ["HbP"])
                            ts("dve", HbP[64:128, 64:128], Hf[64:128, :], nsc[64:128], ALU.mult, ["Hf", "sc"], ["HbP"])
                        kw = ps()
                        mm(psb[kw][:, 0:128], ART4[:, c, 0, :], HbP, True, False, ["ART", "HbP"], [P(kw)])
                        for hh in range(2):
                            mm(psb[kw][:, 64 * hh:64 * hh + 64], AM3[:, hh, 256:384], Vtm3[:, c, 64 * hh:64 * hh + 64], False, hh == 1, [AMk, "Vtm"], [P(kw)])
                        cp("act", Wb, psb[kw][:, 0:128], [P(kw)], ["Wb"])
                        ku = ps()
                        for hh in range(2):
                            mm(psb[ku][:, 64 * hh:64 * hh + 64], ZT3[:, hh, 128:256], Wb[:, 64 * hh:64 * hh + 64], hh == 0, hh == 1, [ZTk, "Wb"], [P(ku)])
                        cp("act", Ub, psb[ku][:, 0:128], [P(ku)], ["Ub"])
                        kh = ps()
                        mm(psb[kh][:, 0:128], KBtm4[:, c, 1, :], Ub, True, False, ["KBtm", "Ub"], [P(kh)])
                        mm(psb[kh][:, 0:128], KBtm4[:, c, 0, :], Vtm3[:, c, :], False, True, ["KBtm", "Vtm"], [P(kh)])
                        ts("dve", Hf, Hf, sc3[:, c, 1:2], ALU.mult, ["Hf", "sc"], ["Hf"])
                        tt("dve", Hf[0:64, :], Hf[0:64, :], psb[kh][0:64, 0:64], ALU.add, ["Hf", P(kh)], ["Hf"])
                        tt("dve", Hf[64:128, :], Hf[64:128, :], psb[kh][64:128, 64:128], ALU.add, ["Hf", P(kh)], ["Hf"])
                        cp("pool", UbZ3[:, 0, 0:64], Ub[:, 0:64], ["Ub"], ["UbZ"])
                        cp("pool", UbZ3[:, 1, 64:128], Ub[:, 64:128], ["Ub"], ["UbZ"])
                        ky = ps()
                        mm(psb[ky][:, 0:128], HbP, ART4[:, c, 1, :], True, False, ["HbP", "ART"], [P(ky)])
                        for hh in range(2):
                            mm(psb[ky][:, 0:128], UbZ3[:, hh, :], AM3[:, hh, 128:256], False, False, ["UbZ", AMk], [P(ky)])
                            mm(psb[ky][:, 0:128], VtmZ4[:, c, hh, :], AM3[:, hh, 384:512], False, hh == 1, ["VtmZ", AMk], [P(ky)])
                        cp("act", YT[:, c * 128:(c + 1) * 128], psb[ky][:, 0:128], [P(ky)], ["YT"])
                    chk("e7")
                    cp("pool", yb, YT, ["YT"], ["yb"])
                    k = ps()
                    mm(psb[k][:], blk, yb, True, True, ["yb", "constb"], [P(k)])
                    stt(cen, psb[k][:], -1.0 / 64, YT, ALU.mult, ALU.add, [P(k), "YT"], ["cen"])
                    tt("pool", sq2, cen, cen, ALU.mult, ["cen"], ["sq2"])
                    k = ps()
                    mm(psb[k][:], blk, sq2, True, True, ["sq2", "constb"], [P(k)])
                    act(sd, psb[k][:], AF.Sqrt, [P(k)], ["sd"], bias=64e-5, scale=1.0 / 64)
                    recip(sd, sd, ["sd"], ["sd"])
                    tt("dve", cen, cen, sd, ALU.mult, ["cen", "sd"], ["cen"])
                    ts("dve", cen, cen, cols[:, CV_LG + i:CV_LG + i + 1], ALU.mult, ["cen", "cols"], ["cen"], s2=cols[:, CV_LB + i:CV_LB + i + 1], op1=ALU.add)
                    tt("pool", cen, cen, bonus4[:, sl], ALU.add, ["cen", "bonus4"], ["cen"])
                    tt("dve", mix3[:, 4 + i, sl], cen, gT4[:, sl], ALU.mult, ["cen", "gT4"], ["mixT"])
            if stop == 'pr':
                S.barrier(); A.reset(); dbgf = A.f32(S_LEN)
                for cc in range(8):
                    cp("dve", dbgf, mix3[:, cc, :], ["mixT"], ["dbgf"])
                    dma("sp", dbg_mix[:, cc * S_LEN:(cc + 1) * S_LEN], dbgf, reads=["dbgf"])
                S.barrier(); S.emit(); return nc
            if dbg and s == 0:
                S.barrier()
                A.reset()
                dbgf = A.f32(S_LEN)
                for cc in range(8):
                    cp("dve", dbgf, mix3[:, cc, :], ["mixT"], ["dbgf"])
                    dma("sp", dbg_mix[:, cc * S_LEN:(cc + 1) * S_LEN], dbgf, reads=["dbgf"])

            S.barrier()
            A.reset()
            wo = A.bf(8 * D)
            wo3 = wo.rearrange("p (c n) -> p c n", n=D)
            for kc in range(8):
                dma("pool", wo3[:, kc, :], w_out[kc * 128:(kc + 1) * 128, :], writes=["wo%d" % kc])
            WO = ["wo%d" % kc for kc in range(8)]
            gbc2 = A.f32(D)
            dma("sp", gbc2, g2v[0, :].partition_broadcast(128), writes=["gbc"])
            x1 = A.f32(8 * D)
            x13 = x1.rearrange("p (t n) -> p t n", n=D)
            xn2T = A.bf(8 * 1024)
            xn23 = xn2T.rearrange("p (c t) -> p c t", t=1024)
            hT = A.bf(11 * 1024)
            hT3 = hT.rearrange("p (f t) -> p f t", t=1024)
            wd = A.bf(11 * D)
            wd3 = wd.rearrange("p (f n) -> p f n", n=D)
            wgs = [A.bf(8 * 128) for _ in range(2)]
            wus = [A.bf(8 * 128) for _ in range(2)]
            scr2 = A.bf(D)
            sms2 = [A.f32(4) for _ in range(2)]
            xsb2 = [A.bf(D) for _ in range(2)]
            sgs = [A.f32(512) for _ in range(2)]
            for half in range(2):
                ht0 = tok0 + half * 1024
                for tt_ in range(8):
                    t0 = half * 1024 + tt_ * 128
                    xk = "x1_%d" % tt_
                    dma("sp", x13[:, tt_, :], x[ht0 + tt_ * 128: ht0 + (tt_ + 1) * 128, :], writes=[xk])
                    for nh in range(2):
                        k = ps()
                        for kc in range(8):
                            mm(psb[k][:], mix3[:, kc, t0:t0 + 128], wo3[:, kc, nh * 512:(nh + 1) * 512], kc == 0, kc == 7, ["mixT"] + WO, [P(k)])
                        tt("dve", x13[:, tt_, nh * 512:(nh + 1) * 512], x13[:, tt_, nh * 512:(nh + 1) * 512], psb[k][:], ALU.add, [xk, P(k)], [xk])
                    b = tt_ % 2
                    rms_to_T(x13[:, tt_, :], xk, gbc2, xn23, "xn2T", tt_ * 128, scr2, "scr2", sms2[b], "sm2%d" % b, xsb2[b], "xsb2%d" % b)
                for fh in range(2):
                    for fi in range(11):
                        f = fh * 11 + fi
                        dma("pool", wd3[:, fi, :], w_down[f * 128:(f + 1) * 128, :], writes=["wd%d" % fi])
                    for fi in range(11):
                        f = fh * 11 + fi
                        b = fi % 2
                        wg3 = wgs[b].rearrange("p (c n) -> p c n", n=128)
                        wu3 = wus[b].rearrange("p (c n) -> p c n", n=128)
                        dma("pool", wg3, w_gate[:, f * 128:(f + 1) * 128].rearrange("(c p) n -> p c n", p=128), writes=["wg%d" % b])
                        dma("pool", wu3, w_up[:, f * 128:(f + 1) * 128].rearrange("(c p) n -> p c n", p=128), writes=["wu%d" % b])
                        for tg in range(2):
                            kg = ps()
                            ku = ps()
                            for kc in range(8):
                                mm(psb[kg][:], wg3[:, kc, :], xn23[:, kc, tg * 512:(tg + 1) * 512], kc == 0, kc == 7, ["wg%d" % b, "xn2T"], [P(kg)])
                            for kc in range(8):
                                mm(psb[ku][:], wu3[:, kc, :], xn23[:, kc, tg * 512:(tg + 1) * 512], kc == 0, kc == 7, ["wu%d" % b, "xn2T"], [P(ku)])
                            sb_ = tg
                            act(sgs[sb_], psb[kg][:], AF.Silu, [P(kg)], ["sg%d" % sb_])
                            tt("dve", hT3[:, fi, tg * 512:(tg + 1) * 512], sgs[sb_], psb[ku][:], ALU.mult, ["sg%d" % sb_, P(ku)], ["hT%d" % fi])
                    for tt_ in range(8):
                        xk = "x1_%d" % tt_
                        for nh in range(2):
                            k = ps()
                            for fi in range(11):
                                mm(psb[k][:], hT3[:, fi, tt_ * 128:(tt_ + 1) * 128], wd3[:, fi, nh * 512:(nh + 1) * 512], fi == 0, fi == 10, ["hT%d" % fi, "wd%d" % fi], [P(k)])
                            tt("dve", x13[:, tt_, nh * 512:(nh + 1) * 512], x13[:, tt_, nh * 512:(nh + 1) * 512], psb[k][:], ALU.add, [xk, P(k)], [xk])
                        if fh == 1:
                            dma("sp", out[ht0 + tt_ * 128: ht0 + (tt_ + 1) * 128, :], x13[:, tt_, :], reads=[xk])
        S.barrier()
        S.emit()
    return nc


_NC_CACHE = {}


def kernel(**inputs):
    inp = {k: np.asarray(v) for k, v in inputs.items()}
    ncores = 8
    nseq = 2
    if "nc" not in _NC_CACHE:
        _NC_CACHE["nc"] = build(nseq)
    nc = _NC_CACHE["nc"]
    x = np.ascontiguousarray(inp["x"], dtype=np.float32)
    B = x.shape[0]
    sq = lambda k: np.ascontiguousarray(inp[k][0], dtype=np.float32)
    shared = {
        "w_in": sq("w_in"), "w_out": sq("w_out"), "w_gate": sq("w_gate"), "w_up": sq("w_up"), "w_down": sq("w_down"),
        "norm1_g": sq("norm1_g").reshape(1, D), "norm2_g": sq("norm2_g").reshape(1, D),
        "w2": sq("w2"), "a2": sq("a2"), "g2": sq("g2"),
        "consts": make_consts(), "amask": make_amask(),
        "cols": make_cols({k: inp[k][0] for k in ("rwkv_mu", "w0", "a0", "k_k", "k_a", "r_k", "lnx_g", "lnx_b", "q_norm_g", "k_norm_g", "attn_out_g")}),
    }
    in_maps = []
    for c in range(ncores):
        m = dict(shared)
        m["x"] = x[c * nseq:(c + 1) * nseq].reshape(nseq * S_LEN, D)
        in_maps.append(m)
    res = run_bass_kernel_spmd(nc, in_maps, core_ids=list(range(ncores)))
    outs = [np.asarray(r["out"]).reshape(nseq, S_LEN, D) for r in res.results]
    return np.concatenate(outs, axis=0).astype(np.float32)
```

```python
import contextlib
import numpy as np
import concourse.bass as bass
import concourse.mybir as mybir
from concourse.bass_utils import run_bass_kernel_spmd

F32 = mybir.dt.float32
BF16 = mybir.dt.bfloat16
ALU = mybir.AluOpType
AF = mybir.ActivationFunctionType
AX = mybir.AxisListType
COMPUTE = ("pe", "act", "dve", "pool")

S_LEN = 2048
D = 1024
DFF = 2816
C0 = float(np.exp(-0.5))


class Sched:
    def __init__(self, nc, stack, n_dma_sems=24):
        self.nc = nc
        self.eng_names = ["pe", "act", "dve", "pool", "sp"]
        self.ops = {e: [] for e in self.eng_names}
        self.sem = {e: stack.enter_context(nc.semaphore("s_" + e)) for e in COMPUTE}
        self.cnt = {e: 0 for e in COMPUTE}
        self.dma_sems = [stack.enter_context(nc.semaphore("s_dma%d" % i)) for i in range(n_dma_sems)]
        self.dma_val = [0] * n_dma_sems
        self.dma_rr = 0
        self.res_w = {}
        self.res_r = {}
        self.waited = {e: {} for e in self.eng_names}

    def _need(self, eng, waits, ev):
        sid, sh, val = ev
        if self.waited[eng].get(sid, 0) >= val:
            return
        cur = waits.get(sid)
        if cur is None or cur[1] < val:
            waits[sid] = (sh, val)

    def op(self, eng, fn, reads=(), writes=(), dma=False):
        waits = {}
        own = None if dma else ("c", eng)
        for k in reads:
            w = self.res_w.get(k)
            if w is not None:
                self._need(eng, waits, w)
            if isinstance(k, tuple) and k[0] == "ps":
                for r in self.res_r.get(k, ()):
                    if r[0] != own:
                        self._need(eng, waits, r)
        for k in writes:
            w = self.res_w.get(k)
            if w is not None and (dma or w[0] != own):
                self._need(eng, waits, w)
            for r in self.res_r.get(k, ()):
                if dma or r[0] != own:
                    self._need(eng, waits, r)
        if dma:
            k = self.dma_rr
            self.dma_rr = (self.dma_rr + 1) % len(self.dma_sems)
            if self.dma_val[k] > 0:
                self._need(eng, waits, (("d", k), self.dma_sems[k], self.dma_val[k]))
            self.dma_val[k] += 16
            ev = (("d", k), self.dma_sems[k], self.dma_val[k])
            inc = 16
        else:
            self.cnt[eng] += 1
            ev = (("c", eng), self.sem[eng], self.cnt[eng])
            inc = 1
        for sid, (sh, val) in waits.items():
            self.waited[eng][sid] = val
        self.ops[eng].append((fn, list(waits.values()), ev[1], inc))
        for k in reads:
            self.res_r.setdefault(k, []).append(ev)
        for k in writes:
            self.res_w[k] = ev
            self.res_r[k] = []
        return ev

    def barrier(self):
        for eng in self.eng_names:
            waits = {}
            for e2 in COMPUTE:
                if self.cnt[e2] > 0 and e2 != eng:
                    self._need(eng, waits, (("c", e2), self.sem[e2], self.cnt[e2]))
            for k, v in enumerate(self.dma_val):
                if v > 0:
                    self._need(eng, waits, (("d", k), self.dma_sems[k], v))
            for sid, (sh, val) in waits.items():
                self.waited[eng][sid] = val
            self.ops[eng].append((None, list(waits.values()), None, 0))
        self.res_w = {}
        self.res_r = {}

    def emit(self):
        sched = self

        def run(engname, engine):
            for fn, waits, sh, inc in sched.ops[engname]:
                for (wsh, val) in waits:
                    engine.wait_ge(wsh, val)
                if fn is not None:
                    fn(engine).then_inc(sh, inc)

        with self.nc.Block() as block:
            @block.tensor
            def _(e):
                run("pe", e)

            @block.scalar
            def _(e):
                run("act", e)

            @block.vector
            def _(e):
                run("dve", e)

            @block.gpsimd
            def _(e):
                run("pool", e)

            @block.sync
            def _(e):
                run("sp", e)


class Arena:
    def __init__(self, t, n):
        self.t, self.n, self.off = t, n, 0

    def reset(self):
        self.off = 0

    def bf(self, n):
        n = (n + 1) // 2 * 2
        a = self.t[:, self.off:self.off + n]
        self.off += n
        assert self.off <= self.n, ("arena overflow", self.off, self.n)
        return a

    def f32(self, n):
        return self.bf(2 * n).bitcast(F32)


NC_ID, NC_BLK, NC_M4, NC_ML4, NC_I2, NC_RST, NC_WN = 0, 128, 256, 768, 1280, 1536, 2048
NCONST = 2048 + 64


def make_consts():
    c = np.zeros((128, NCONST), np.float32)
    c[:, NC_ID:NC_ID + 128] = np.eye(128)
    c[0:64, NC_BLK:NC_BLK + 64] = 1.0
    c[64:128, NC_BLK + 64:NC_BLK + 128] = 1.0
    p = np.arange(128)[:, None]
    f = np.arange(128)[None, :]
    mus = (p < f).astype(np.float32)
    mui = (p <= f).astype(np.float32)
    mls = (p > f).astype(np.float32)
    c[:, NC_M4:NC_M4 + 512] = np.concatenate([mus, mui, mus, mui], 1)
    c[:, NC_ML4:NC_ML4 + 512] = np.concatenate([mls] * 4, 1)
    c[:, NC_I2:NC_I2 + 256] = np.concatenate([np.eye(128)] * 2, 1)
    r = np.ones((128, 512), np.float32)
    r[:, 0::128] = 0.0
    c[:, NC_RST:NC_RST + 512] = r
    c[0:64, NC_WN:NC_WN + 64] = 1.0 / 64
    c[64, NC_WN:NC_WN + 64] = 1e-6
    return c


def make_amask():
    slopes = np.exp2(-8.0 * np.arange(1, 9, dtype=np.float64) / 8)
    ki = np.arange(128)[:, None]
    cc = np.arange(2048)[None, :]
    dl = cc - ki
    m = ((dl >= 0) & (dl <= 128)).astype(np.float64) + ((dl >= 0) & (dl % 4 == 0) & (dl <= 512)) + ((dl >= 0) & (dl % 16 == 0))
    out = np.zeros((8, 128, 2048), np.float32)
    for h in range(8):
        out[h] = m * np.exp(-slopes[h] * np.maximum(dl, 0))
    return out


CV_MU, CV_W0, CV_A0, CV_KK, CV_KA, CV_RK, CV_LG, CV_LB, CV_GQ, CV_GK, CV_GA = 0, 14, 18, 22, 26, 30, 34, 38, 42, 43, 44
NCV = 52


def make_cols(inp):
    cv = np.zeros((128, NCV), np.float32)

    def put(off, v):
        v = np.asarray(v, np.float32).reshape(-1, 128)
        cv[:, off:off + v.shape[0]] = v.T

    put(CV_MU, inp["rwkv_mu"])
    put(CV_W0, inp["w0"])
    put(CV_A0, inp["a0"])
    put(CV_KK, inp["k_k"])
    put(CV_KA, inp["k_a"])
    put(CV_RK, inp["r_k"])
    put(CV_LG, inp["lnx_g"])
    put(CV_LB, inp["lnx_b"])
    put(CV_GQ, np.tile(np.asarray(inp["q_norm_g"], np.float32).reshape(-1), 2))
    put(CV_GK, np.tile(np.asarray(inp["k_norm_g"], np.float32).reshape(-1), 2))
    ga = np.asarray(inp["attn_out_g"], np.float32).reshape(8, 64)
    cv[0:64, CV_GA:CV_GA + 8] = ga.T
    return cv


class StopBuild(Exception):
    pass


def build(NSEQ=2, dbg=False, stop=None):
    try:
        return _build(NSEQ, dbg, stop)
    except StopBuild as e:
        return e.args[0]


def _build(NSEQ=2, dbg=False, stop=None):
    nc = bass.Bass("TRN2", target_bir_lowering=False)
    NT = NSEQ * S_LEN

    def din(name, shape):
        return nc.dram_tensor(name, shape, F32, kind="ExternalInput").ap()

    x = din("x", [NT, D])
    w_in = din("w_in", [D, 3328])
    w_out = din("w_out", [D, D])
    w_gate = din("w_gate", [D, DFF])
    w_up = din("w_up", [D, DFF])
    w_down = din("w_down", [DFF, D])
    g1v = din("norm1_g", [1, D])
    g2v = din("norm2_g", [1, D])
    w2d = din("w2", [64, 512])
    a2d = din("a2", [64, 512])
    g2d = din("g2", [128, 512])
    consts_d = din("consts", [128, NCONST])
    amask_d = din("amask", [8, 128, 2048])
    cols_d = din("cols", [128, NCV])
    out = nc.dram_tensor("out", [NT, D], F32, kind="ExternalOutput").ap()
    if dbg:
        dbg_mix = nc.dram_tensor("dbg_mix", [128, 8 * S_LEN], F32, kind="ExternalOutput").ap()

    with contextlib.ExitStack() as st:
        S = Sched(nc, st)
        T = lambda n, s, d: st.enter_context(nc.sbuf_tensor(n, s, d))
        psb = [st.enter_context(nc.psum_tensor("ps%d" % i, [128, 512], F32)) for i in range(8)]
        rr = {"i": 0}

        def ps(pool=(0, 1, 2, 3, 4, 5, 6, 7)):
            k = pool[rr["i"] % len(pool)]
            rr["i"] += 1
            return k

        ARN = 86000
        arena_t = T("arena", [128, ARN], BF16)
        A = Arena(arena_t, ARN)
        mixT = T("mixT", [128, 8 * S_LEN], BF16)
        mix3 = mixT[:].rearrange("p (c t) -> p c t", t=S_LEN)
        constb = T("constb", [128, NCONST], BF16)
        constf = T("constf", [128, 512], F32)
        cols = T("cols_sb", [128, NCV], F32)
        gq8 = T("gq8", [128, 1], F32)
        identb = constb[:, NC_ID:NC_ID + 128]
        blk = constb[:, NC_BLK:NC_BLK + 128]

        def dma(eng, o, i, reads=(), writes=()):
            S.op(eng, lambda e: e.dma_start(out=o, in_=i), reads, writes, dma=True)

        def mm(o, l, r, start, stop, reads, writes):
            S.op("pe", lambda e: e.matmul(o, lhsT=l, rhs=r, start=start, stop=stop, skip_group_check=True), reads, writes)

        def tr(o, i, reads, writes):
            S.op("pe", lambda e: e.transpose(o, i, identb), reads, writes)

        def act(o, i, func, reads, writes, bias=0.0, scale=1.0, accum=None):
            if accum is None:
                S.op("act", lambda e: e.activation(out=o, in_=i, func=func, bias=bias, scale=scale), reads, writes)
            else:
                S.op("act", lambda e: e.activation(out=o, in_=i, func=func, bias=bias, scale=scale, accum_out=accum), reads, writes)

        def tt(eng, o, a, b, op, reads, writes):
            S.op(eng, lambda e: e.tensor_tensor(out=o, in0=a, in1=b, op=op), reads, writes)

        def ts(eng, o, a, s1, op0, reads, writes, s2=None, op1=None):
            if op1 is None:
                S.op(eng, lambda e: e.tensor_scalar(out=o, in0=a, scalar1=s1, scalar2=None, op0=op0), reads, writes)
            else:
                S.op(eng, lambda e: e.tensor_scalar(out=o, in0=a, scalar1=s1, scalar2=s2, op0=op0, op1=op1), reads, writes)

        def stt(o, a, s, b, op0, op1, reads, writes):
            S.op("dve", lambda e: e.scalar_tensor_tensor(out=o, in0=a, scalar=s, in1=b, op0=op0, op1=op1), reads, writes)

        def cp(eng, o, i, reads, writes):
            if eng == "act":
                act(o, i, AF.Copy, reads, writes)
            else:
                S.op(eng, lambda e: e.tensor_copy(out=o, in_=i), reads, writes)

        def recip(o, i, reads, writes):
            S.op("dve", lambda e: e.reciprocal(out=o, in_=i), reads, writes)

        def P(k):
            return ("ps", k)

        def chk(name):
            if stop == name:
                S.barrier(); S.emit()
                raise StopBuild(nc)

        dma("pool", constb[:], consts_d, writes=["constb"])
        dma("sp", constf[:], consts_d[:, NC_RST:NC_RST + 512], writes=["constf"])
        dma("sp", cols[:], cols_d, writes=["cols"])
        ts("dve", gq8[:], cols[:, CV_GQ:CV_GQ + 1], 0.125, ALU.mult, ["cols"], ["gq8"])

        def rms_to_T(xt, xt_key, gbc, dstT, dst_key, t0, scr, scr_key, sm, sm_key, xsb, xsb_key):
            act(scr, xt, AF.Square, [xt_key], [scr_key, sm_key], accum=sm[:, 0:1])
            ts("dve", sm[:, 1:2], sm[:, 0:1], 1.0 / D, ALU.mult, [sm_key], [sm_key], s2=1e-6, op1=ALU.add)
            act(sm[:, 2:3], sm[:, 1:2], AF.Sqrt, [sm_key], [sm_key])
            recip(sm[:, 3:4], sm[:, 2:3], [sm_key], [sm_key])
            stt(xsb, xt, sm[:, 3:4], gbc, ALU.mult, ALU.mult, [xt_key, sm_key, "gbc"], [xsb_key])
            k = ps()
            pv = psb[k][:].bitcast(BF16)
            for kc in range(8):
                tr(pv[:, kc * 128:(kc + 1) * 128], xsb[:, kc * 128:(kc + 1) * 128], [xsb_key, "constb"], [P(k)])
            cp("act", dstT[:, :, t0:t0 + 128], pv.rearrange("p (c t) -> p c t", t=128), [P(k)], [dst_key])

        for s in range(NSEQ):
            tok0 = s * S_LEN
            S.barrier()
            A.reset()
            xnT = A.bf(8 * S_LEN)
            xn3 = xnT.rearrange("p (c t) -> p c t", t=S_LEN)
            mark_pa = A.off
            gbc = A.f32(D)
            dma("sp", gbc, g1v[0, :].partition_broadcast(128), writes=["gbc"])
            xts = [A.f32(D) for _ in range(2)]
            scr = A.bf(D)
            sms = [A.f32(4) for _ in range(2)]
            xsbs = [A.bf(D) for _ in range(2)]
            wqkv = A.bf(8 * 1536)
            wq3 = wqkv.rearrange("p (c n) -> p c n", n=1536)
            for kc in range(8):
                dma("pool", wq3[:, kc, :], w_in[kc * 128:(kc + 1) * 128, 0:1536], writes=["wqkv%d" % kc])
            amask = A.bf(8 * 2048)
            am3 = amask.rearrange("p (h c) -> p h c", c=2048)
            for h in range(8):
                dma("pool", am3[:, h, :], amask_d[h], writes=["amask%d" % h])
            for ti in range(16):
                b = ti % 2
                dma("sp", xts[b], x[tok0 + ti * 128: tok0 + (ti + 1) * 128, :], writes=["xt%d" % b])
                rms_to_T(xts[b], "xt%d" % b, gbc, xn3, "xnT", ti * 128, scr, "scr", sms[b], "sm%d" % b, xsbs[b], "xsb%d" % b)
            if stop in ('p0', 'p0d'):
                if stop == 'p0d':
                    S.barrier(); A.reset(); dbgf = A.f32(S_LEN)
                    for cc in range(8):
                        cp("dve", dbgf, mix3[:, cc, :], ["mixT"], ["dbgf"])
                        dma("sp", dbg_mix[:, cc * S_LEN:(cc + 1) * S_LEN], dbgf, reads=["dbgf"])
                S.barrier(); S.emit(); return nc
            WQ = ["wqkv%d" % kc for kc in range(8)]
            vaug = A.bf(16 * 8 * 65)
            va4 = vaug.rearrange("p (j h e) -> p j h e", h=8, e=65)
            S.op("pool", lambda e: e.memset(vaug, 1.0), (), ["vaug"])
            for j in range(16):
                k = ps((0, 1, 2, 3))
                for kc in range(8):
                    mm(psb[k][:], xn3[:, kc, j * 128:(j + 1) * 128], wq3[:, kc, 1024:1536], kc == 0, kc == 7, ["xnT"] + WQ, [P(k)])
                cp("act" if j % 2 else "dve", va4[:, j, :, 0:64], psb[k][:].rearrange("p (h e) -> p h e", e=64), [P(k)], ["vaug"])
            if stop == 'v':
                S.barrier(); S.emit(); return nc
            qts = [A.bf(S_LEN) for _ in range(2)]
            kts = [A.bf(S_LEN) for _ in range(2)]
            sqs = [A.bf(512) for _ in range(4)]
            rss = [A.f32(512) for _ in range(4)]
            ebs = [A.bf(512) for _ in range(4)]
            pbs = [A.bf(512) for _ in range(4)]
            wn = constb[0:65, NC_WN:NC_WN + 64]
            cnt = 0
            for i in range(4):
                qt, kt = qts[i % 2], kts[i % 2]
                qk, kk_ = "qt%d" % (i % 2), "kt%d" % (i % 2)
                jobs = []
                for which, dst, dkey, gcol, c0 in ((0, qt, qk, gq8[:, 0:1], 0), (1, kt, kk_, cols[:, CV_GK:CV_GK + 1], 512)):
                    for tg in range(4):
                        jobs.append((dst, dkey, gcol, c0, tg))
                live = {}
                for t in range(len(jobs) + 1):
                    if t < len(jobs):
                        dst, dkey, gcol, c0, tg = jobs[t]
                        k = ps((0, 1, 2, 3))
                        for kc in range(8):
                            mm(psb[k][:], wq3[:, kc, c0 + 128 * i: c0 + 128 * (i + 1)], xn3[:, kc, tg * 512:(tg + 1) * 512], kc == 0, kc == 7, ["xnT"] + WQ, [P(k)])
                        b = t % 4
                        act(sqs[b], psb[k][:], AF.Square, [P(k)], ["sq%d" % b])
                        live[t] = (k, b)
                    if t >= 1:
                        dst, dkey, gcol, c0, tg = jobs[t - 1]
                        k, b = live.pop(t - 1)
                        k2 = ps((0, 1, 2, 3))
                        mm(psb[k2][:], blk, sqs[b], True, True, ["sq%d" % b, "constb"], [P(k2)])
                        act(rss[b], psb[k2][:], AF.Ln, [P(k2)], ["rs%d" % b], bias=1e-6, scale=1.0 / 64)
                        act(rss[b], rss[b], AF.Exp, ["rs%d" % b], ["rs%d" % b], scale=-0.5)
                        stt(dst[:, tg * 512:(tg + 1) * 512], psb[k][:], gcol, rss[b], ALU.mult, ALU.mult, [P(k), "rs%d" % b, "gq8", "cols"], [dkey])
                for hh in range(2):
                    h = 2 * i + hh
                    pb = 64 * hh
                    ob = (4, 5, 6, 7)
                    ec = 0
                    groups = []
                    for j in range(16):
                        for g in range(j // 4, 4):
                            cst = max(128 * j, 512 * g)
                            groups.append((j, g, cst, 512 * (g + 1)))
                    LA = 3
                    for t in range(len(groups) + LA):
                        if t < len(groups):
                            j, g, cst, cen = groups[t]
                            n = cen - cst
                            k = ps((0, 1, 2, 3))
                            mm(psb[k][:, 0:n], kt[pb:pb + 64, j * 128:(j + 1) * 128], qt[pb:pb + 64, cst:cen], True, True, [qk, kk_], [P(k)])
                            b = t % 4
                            act(ebs[b][:, 0:n], psb[k][:, 0:n], AF.Exp, [P(k)], ["eb%d" % b])
                            tt("pool" if t % 2 else "dve", pbs[b][:, 0:n], ebs[b][:, 0:n], am3[:, h, cst - 128 * j: cen - 128 * j], ALU.mult, ["eb%d" % b, "amask%d" % h], ["pb%d" % b])
                        if t >= LA:
                            j, g, cst, cen = groups[t - LA]
                            n = cen - cst
                            b = (t - LA) % 4
                            mm(psb[ob[g]][0:65, cst - 512 * g: cen - 512 * g], va4[:, j, h, :], pbs[b][:, 0:n], j == 0, j == 4 * g + 3, ["vaug", "pb%d" % b], [P(ob[g])])
                    k2s = []
                    for g in range(4):
                        act(sqs[g][0:65, :], psb[ob[g]][0:65, :], AF.Square, [P(ob[g])], ["sq%d" % g])
                    for g in range(4):
                        k2 = ps((0, 1, 2, 3))
                        k2s.append(k2)
                        mm(psb[k2][0:64, :], wn, sqs[g][0:65, :], True, True, ["sq%d" % g, "constb"], [P(k2)])
                    for g in range(4):
                        act(rss[g][0:64, :], psb[k2s[g]][0:64, :], AF.Ln, [P(k2s[g])], ["rs%d" % g])
                    for g in range(4):
                        act(rss[g][0:64, :], rss[g][0:64, :], AF.Exp, ["rs%d" % g], ["rs%d" % g], scale=-0.5)
                    for g in range(4):
                        stt(mix3[pb:pb + 64, i, g * 512:(g + 1) * 512], psb[ob[g]][0:64, :], cols[0:64, CV_GA + h:CV_GA + h + 1], rss[g][0:64, :], ALU.mult, ALU.mult,
                            [P(ob[g]), "rs%d" % g, "cols"], ["mixT"])

            if stop == 'pa':
                S.barrier(); A.reset(); dbgf = A.f32(S_LEN)
                for cc in range(8):
                    cp("dve", dbgf, mix3[:, cc, :], ["mixT"], ["dbgf"])
                    dma("sp", dbg_mix[:, cc * S_LEN:(cc + 1) * S_LEN], dbgf, reads=["dbgf"])
                S.barrier(); S.emit(); return nc
            S.barrier()
            A.off = mark_pa
            wr = A.bf(8 * 1792)
            wr3 = wr.rearrange("p (c n) -> p c n", n=1792)
            for kc in range(8):
                dma("pool", wr3[:, kc, :], w_in[kc * 128:(kc + 1) * 128, 1536:3328], writes=["wr%d" % kc])
            WR = ["wr%d" % kc for kc in range(8)]
            w2a2 = A.bf(512)
            g2b = A.bf(512)
            dma("pool", w2a2[0:64, :], w2d, writes=["w2a2a"])
            dma("pool", w2a2[64:128, :], a2d, writes=["w2a2b"])
            dma("pool", g2b, g2d, writes=["g2b"])
            lora1 = A.bf(S_LEN)
            lora2 = A.bf(S_LEN)
            halo = A.f32(16)
            ptsb = [A.f32(514) for _ in range(2)]
            dtmp = [A.f32(512) for _ in range(2)]
            M4 = constb[:, NC_M4:NC_M4 + 512]
            ML2 = constb[:, NC_ML4:NC_ML4 + 256]
            I2 = constb[:, NC_I2:NC_I2 + 256]
            pcount = [0]

            def proj_shift(cj, tg, dst_fn):
                k = ps()
                for kc in range(8):
                    mm(psb[k][:], wr3[:, kc, cj * 128:(cj + 1) * 128], xn3[:, kc, tg * 512:(tg + 1) * 512], kc == 0, kc == 7, ["xnT"] + WR, [P(k)])
                b = pcount[0] % 2
                pcount[0] += 1
                pk = "ptsb%d" % b
                if tg == 0:
                    S.op("pool", lambda e: e.memset(ptsb[b][:, 0:1], 0.0), (), [pk + "h"])
                else:
                    cp("pool", ptsb[b][:, 0:1], halo[:, cj:cj + 1], ["halo%d" % cj], [pk + "h"])
                cp("act", ptsb[b][:, 1:513], psb[k][:], [P(k)], [pk])
                cp("pool", halo[:, cj:cj + 1], ptsb[b][:, 512:513], [pk], ["halo%d" % cj])
                tt("pool", dtmp[b], ptsb[b][:, 0:512], ptsb[b][:, 1:513], ALU.subtract, [pk, pk + "h"], ["dtmp%d" % b])
                dst_fn(dtmp[b], cols[:, CV_MU + cj:CV_MU + cj + 1], ptsb[b][:, 1:513], ["dtmp%d" % b, pk, "cols"])

            A_l12 = A.f32(512)
            for tg in range(4):
                sl = slice(tg * 512, (tg + 1) * 512)
                tmpx = dtmp

                def f12(d, mu, p, rd, sl=sl):
                    xs = A_l12
                    stt(xs, d, mu, p, ALU.mult, ALU.add, rd, ["xs12"])
                    act(lora1[0:64, sl], xs[0:64, :], AF.Tanh, ["xs12"], ["lora1a"])
                    act(lora1[64:128, sl], xs[64:128, :], AF.Copy, ["xs12"], ["lora1b"])

                def f13(d, mu, p, rd, sl=sl):
                    xs = A_l12
                    stt(xs, d, mu, p, ALU.mult, ALU.add, rd, ["xs12"])
                    act(lora2[:, sl], xs, AF.Sigmoid, ["xs12"], ["lora2"])

                proj_shift(12, tg, f12)
                proj_shift(13, tg, f13)

            if stop == 'lora':
                S.barrier(); S.emit(); return nc
            rT, kT, vT = A.f32(512), A.f32(512), A.f32(512)
            sw, ia, gT4 = A.f32(512), A.f32(512), A.f32(S_LEN)
            Ls, D1, D2, kk0, kk, kp, bv = [A.f32(512) for _ in range(7)]
            E1, E2, E3, E4 = [A.f32(512) for _ in range(4)]
            rn = A.f32(512)
            sqb, rkr = A.bf(512), A.bf(512)
            bonus4 = A.f32(S_LEN)
            ART = A.bf(4 * 256)
            ART4 = ART.rearrange("p (c w t) -> p c w t", w=2, t=128)
            KBT = A.bf(4 * 256)
            KBT4 = KBT.rearrange("p (c w t) -> p c w t", w=2, t=128)
            kbarT, bbarT, vbT = A.bf(512), A.bf(512), A.bf(512)
            KBtm = A.bf(4 * 256)
            KBtm4 = KBtm.rearrange("p (c w n) -> p c w n", w=2, n=128)
            Vtm = A.bf(4 * 128)
            Vtm3 = Vtm.rearrange("p (c n) -> p c n", n=128)
            VtmZ = A.bf(4 * 256)
            VtmZ4 = VtmZ.rearrange("p (c h n) -> p c h n", h=2, n=128)
            sc = A.f32(8)
            sc3 = sc.rearrange("p (c w) -> p c w", w=2)
            AM3s = [A.bf(2 * 512).rearrange("p (h n) -> p h n", n=512) for _ in range(4)]
            NT3s = [A.bf(256).rearrange("p (h n) -> p h n", n=128) for _ in range(4)]
            ZT3s = [A.bf(512).rearrange("p (h n) -> p h n", n=256) for _ in range(4)]
            Wb, Ub = A.bf(128), A.bf(128)
            UbZ = A.bf(256)
            UbZ3 = UbZ.rearrange("p (h n) -> p h n", n=128)
            Hf = A.f32(64)
            HbP = A.bf(128)
            YT = A.f32(512)
            yb, sq2 = A.bf(512), A.bf(512)
            cen, sd = A.f32(512), A.f32(512)

            for i in range(4):
                S.op("pool", lambda e: e.memset(VtmZ, 0.0), (), ["VtmZ"])
                S.op("pool", lambda e: e.memset(UbZ, 0.0), (), ["UbZ"])
                S.op("pool", lambda e: e.memset(HbP, 0.0), (), ["HbP"])
                S.op("pool", lambda e: e.memset(Hf, 0.0), (), ["Hf"])
                for tg in range(4):
                    sl = slice(tg * 512, (tg + 1) * 512)

                    def mk(dst, key):
                        def f(d, mu, p, rd):
                            stt(dst, d, mu, p, ALU.mult, ALU.add, rd, [key])
                        return f

                    proj_shift(0 + i, tg, mk(rT, "rT"))
                    proj_shift(4 + i, tg, mk(kT, "kT"))
                    proj_shift(8 + i, tg, mk(vT, "vT"))
                    k = ps()
                    mm(psb[k][:], w2a2[0:64, 128 * i:128 * (i + 1)], lora1[0:64, sl], True, True, ["w2a2a", "lora1a"], [P(k)])
                    act(sw, psb[k][:], AF.Sigmoid, [P(k), "cols"], ["sw"], bias=cols[:, CV_W0 + i:CV_W0 + i + 1])
                    k = ps()
                    mm(psb[k][:], w2a2[64:128, 128 * i:128 * (i + 1)], lora1[64:128, sl], True, True, ["w2a2b", "lora1b"], [P(k)])
                    act(ia, psb[k][:], AF.Sigmoid, [P(k), "cols"], ["ia"], bias=cols[:, CV_A0 + i:CV_A0 + i + 1])
                    k = ps()
                    mm(psb[k][:], g2b[:, 128 * i:128 * (i + 1)], lora2[:, sl], True, True, ["g2b", "lora2"], [P(k)])
                    cp("act", gT4[:, sl], psb[k][:], [P(k)], ["gT4"])
                    S.op("dve", lambda e: e.tensor_tensor_scan(out=Ls, data0=constf[:], data1=sw, initial=0.0, op0=ALU.mult, op1=ALU.add), ["constf", "sw"], ["Ls"])
                    Ls3 = Ls.rearrange("p (c t) -> p c t", t=128)
                    tt("pool", D1.rearrange("p (c t) -> p c t", t=128), Ls3, Ls3[:, :, 63:64].to_broadcast([128, 4, 128]), ALU.subtract, ["Ls"], ["D1"])
                    act(E1, D1, AF.Exp, ["D1"], ["E1"], scale=-C0)
                    act(E3, D1, AF.Exp, ["D1"], ["E3"], scale=C0)
                    tt("pool", D2, D1, sw, ALU.subtract, ["D1", "sw"], ["D2"])
                    act(E2, D2, AF.Exp, ["D2"], ["E2"], scale=-C0)
                    tt("dve", D2.rearrange("p (c t) -> p c t", t=128), Ls3, Ls3[:, :, 127:128].to_broadcast([128, 4, 128]), ALU.subtract, ["Ls", "E2"], ["D2"])
                    act(E4, D2, AF.Exp, ["D2"], ["E4"], scale=C0)
                    act(sc3[:, :, 0:1], Ls3[:, :, 63:64], AF.Exp, ["Ls"], ["sc"], scale=-C0)
                    act(sc3[:, :, 1:2], Ls3[:, :, 127:128], AF.Exp, ["Ls"], ["sc"], scale=-C0)
                    chk("e1")
                    ts("dve", kk0, kT, cols[:, CV_KK + i:CV_KK + i + 1], ALU.mult, ["kT", "cols"], ["kk0"])
                    tt("pool", sqb, kk0, kk0, ALU.mult, ["kk0"], ["sqb"])
                    k = ps()
                    mm(psb[k][:], blk, sqb, True, True, ["sqb", "constb"], [P(k)])
                    ts("dve", rn, psb[k][:], 1e-24, ALU.max, [P(k)], ["rn"])
                    act(rn, rn, AF.Ln, ["rn"], ["rn"])
                    act(rn, rn, AF.Exp, ["rn"], ["rn"], scale=-0.5)
                    tt("dve", kk, kk0, rn, ALU.mult, ["kk0", "rn"], ["kk"])
                    ts("dve", kp, ia, -1.0, ALU.add, ["ia", "cols"], ["kp"], s2=cols[:, CV_KA + i:CV_KA + i + 1], op1=ALU.mult)
                    stt(kp, kp, 1.0, kT, ALU.add, ALU.mult, ["kp", "kT"], ["kp"])
                    tt("pool", bv, kk, ia, ALU.mult, ["kk", "ia"], ["bv"])
                    c3 = lambda ap: ap.rearrange("p (c t) -> p c t", t=128)
                    tt("dve", ART4[:, :, 1, :], c3(rT), c3(E1), ALU.mult, ["rT", "E1"], ["ART"])
                    stt(kk0, kk, -1.0, E2, ALU.mult, ALU.mult, ["kk", "E2"], ["kk0"])
                    cp("pool", ART4[:, :, 0, :], c3(kk0), ["kk0"], ["ART"])
                    tt("pool", KBT4[:, :, 1, :], c3(kp), c3(E3), ALU.mult, ["kp", "E3"], ["KBT"])
                    tt("dve", KBT4[:, :, 0, :], c3(bv), c3(E3), ALU.mult, ["bv", "E3"], ["KBT"])
                    tt("pool", kbarT, kp, E4, ALU.mult, ["kp", "E4"], ["kbarT"])
                    tt("dve", bbarT, bv, E4, ALU.mult, ["bv", "E4"], ["bbarT"])
                    cp("pool", vbT, vT, ["vT"], ["vbT"])
                    stt(rkr, rT, cols[:, CV_RK + i:CV_RK + i + 1], kp, ALU.mult, ALU.mult, ["rT", "kp", "cols"], ["rkr"])
                    k = ps()
                    mm(psb[k][:], blk, rkr, True, True, ["rkr", "constb"], [P(k)])
                    tt("dve", bonus4[:, sl], psb[k][:], vT, ALU.mult, [P(k), "vT"], ["bonus4"])
                    chk("e2")
                    for c in range(4):
                        k = ps()
                        pv = psb[k][:].bitcast(BF16)
                        cs = slice(c * 128, (c + 1) * 128)
                        tr(pv[:, 0:128], kbarT[:, cs], ["kbarT", "constb"], [P(k)])
                        tr(pv[:, 128:256], bbarT[:, cs], ["bbarT", "constb"], [P(k)])
                        tr(pv[:, 256:384], vbT[:, cs], ["vbT", "constb"], [P(k)])
                        cp("act", KBtm4[:, c, :, :], pv[:, 0:256].rearrange("p (w n) -> p w n", n=128), [P(k)], ["KBtm"])
                        cp("dve", Vtm3[:, c, :], pv[:, 256:384], [P(k)], ["Vtm"])
                        cp("act", VtmZ4[:, c, 0, 0:64], pv[:, 256:320], [P(k)], ["VtmZ"])
                        cp("dve", VtmZ4[:, c, 1, 64:128], pv[:, 320:384], [P(k)], ["VtmZ"])
                    chk("e3")
                    I2v = I2.rearrange("p (h n) -> p h n", n=128)
                    for c in range(4):
                        AM3, NT3, ZT3 = AM3s[c], NT3s[c], ZT3s[c]
                        for hh in range(2):
                            pb = 64 * hh
                            kk_b = ps()
                            knh = ps()
                            rhs_ar = ART4[pb:pb + 64, c, :, :].rearrange("p w t -> p (w t)")
                            mm(psb[kk_b][:, 0:256], KBT4[pb:pb + 64, c, 0, :], rhs_ar, True, False, ["KBT", "ART"], [P(kk_b)])
                            mm(psb[kk_b][:, 256:512], KBT4[pb:pb + 64, c, 1, :], rhs_ar, False, True, ["KBT", "ART"], [P(kk_b)])
                            mm(psb[knh][:, 0:128], ART4[pb:pb + 64, c, 0, :], KBT4[pb:pb + 64, c, 0, :], True, True, ["KBT", "ART"], [P(knh)])
                            tt("dve", AM3[:, hh, :], psb[kk_b][:], M4, ALU.mult, [P(kk_b), "constb"], ["AM%d" % c])
                            tt("dve", NT3[:, hh, :], psb[knh][:, 0:128], ML2[:, 0:128], ALU.mult, [P(knh), "constb"], ["NT%d" % c])
                        cp("pool", ZT3[:, 0, 0:128], AM3[:, 0, 0:128], ["AM%d" % c], ["ZT%d" % c])
                        cp("pool", ZT3[:, 1, 0:128], AM3[:, 1, 0:128], ["AM%d" % c], ["ZT%d" % c])
                        tt("dve", ZT3[:, :, 128:256], AM3[:, :, 0:128], I2v, ALU.add, ["AM%d" % c, "constb"], ["ZT%d" % c])
                    for m in range(7):
                        banks = []
                        for c in range(4):
                            NT3, ZT3 = NT3s[c], ZT3s[c]
                            kz = ps()
                            kn2 = ps() if m < 6 else None
                            banks.append((kz, kn2))
                            rd, = [["NT%d" % c, "ZT%d" % c]]
                            if m == 0:
                                for hh in range(2):
                                    mm(psb[kz][:, 256 * hh:256 * hh + 128], NT3[:, hh, :], ZT3[:, hh, 0:128], hh == 0, hh == 1, rd, [P(kz)])
                            elif m < 6:
                                for hh in range(2):
                                    mm(psb[kz][:, 256 * hh:256 * hh + 256], NT3[:, hh, :], ZT3[:, hh, :], hh == 0, hh == 1, rd, [P(kz)])
                            else:
                                for hh in range(2):
                                    mm(psb[kz][:, 256 * hh + 128:256 * hh + 256], NT3[:, hh, :], ZT3[:, hh, 128:256], hh == 0, hh == 1, rd, [P(kz)])
                            if m < 6:
                                for hh in range(2):
                                    mm(psb[kn2][:, 128 * hh:128 * hh + 128], ZT3[:, hh, 0:128], NT3[:, hh, :], hh == 0, hh == 1, rd, [P(kn2)])
                        for c in range(4):
                            NT3, ZT3 = NT3s[c], ZT3s[c]
                            kz, kn2 = banks[c]
                            pz3 = psb[kz][:].rearrange("p (h n) -> p h n", n=256)
                            if m >= 1:
                                tt("dve", ZT3[:, :, 128:256], ZT3[:, :, 128:256], pz3[:, :, 128:256], ALU.add, [P(kz), "ZT%d" % c], ["ZT%d" % c])
                            if m < 6:
                                cp("act", ZT3[:, :, 0:128], pz3[:, :, 0:128], [P(kz)], ["ZT%d" % c])
                                cp("act" if c % 2 else "dve", NT3[:, :, :], psb[kn2][:, 0:256].rearrange("p (h n) -> p h n", n=128), [P(kn2)], ["NT%d" % c])
                    for c in range(4):
                        AM3, ZT3 = AM3s[c], ZT3s[c]
                        AMk, ZTk = "AM%d" % c, "ZT%d" % c
                        if not (tg == 0 and c == 0):
                            nsc = sc3[:, c, 0:1]
                            ts("dve", HbP[0:64, 0:64], Hf[0:64, :], nsc[0:64], ALU.mult, ["Hf", "sc"], ["HbP"])
                            ts("dve", HbP[64:128, 64:128], Hf[64:128, :], nsc[64:128], ALU.mult, ["Hf", "sc"], ["HbP"])
                        kw = ps()
                        mm(psb[kw][:, 0:128], ART4[:, c, 0, :], HbP, True, False, ["ART", "HbP"], [P(kw)])
                        for hh in range(2):
                            mm(psb[kw][:, 64 * hh:64 * hh + 64], AM3[:, hh, 256:384], Vtm3[:, c, 64 * hh:64 * hh + 64], False, hh == 1, [AMk, "Vtm"], [P(kw)])
                        cp("act", Wb, psb[kw][:, 0:128], [P(kw)], ["Wb"])
                        ku = ps()
                        for hh in range(2):
                            mm(psb[ku][:, 64 * hh:64 * hh + 64], ZT3[:, hh, 128:256], Wb[:, 64 * hh:64 * hh + 64], hh == 0, hh == 1, [ZTk, "Wb"], [P(ku)])
                        cp("act", Ub, psb[ku][:, 0:128], [P(ku)], ["Ub"])
                        kh = ps()
                        mm(psb[kh][:, 0:128], KBtm4[:, c, 1, :], Ub, True, False, ["KBtm", "Ub"], [P(kh)])
                        mm(psb[kh][:, 0:128], KBtm4[:, c, 0, :], Vtm3[:, c, :], False, True, ["KBtm", "Vtm"], [P(kh)])
                        ts("dve", Hf, Hf, sc3[:, c, 1:2], ALU.mult, ["Hf", "sc"], ["Hf"])
                        tt("dve", Hf[0:64, :], Hf[0:64, :], psb[kh][0:64, 0:64], ALU.add, ["Hf", P(kh)], ["Hf"])
                        tt("dve", Hf[64:128, :], Hf[64:128, :], psb[kh][64:128, 64:128], ALU.add, ["Hf", P(kh)], ["Hf"])
                        cp("pool", UbZ3[:, 0, 0:64], Ub[:, 0:64], ["Ub"], ["UbZ"])
                        cp("pool", UbZ3[:, 1, 64:128], Ub[:, 64:128], ["Ub"], ["UbZ"])
                        ky = ps()
                        mm(psb[ky][:, 0:128], HbP, ART4[:, c, 1, :], True, False, ["HbP", "ART"], [P(ky)])
                        for hh in range(2):
                            mm(psb[ky][:, 0:128], UbZ3[:, hh, :], AM3[:, hh, 128:256], False, False, ["UbZ", AMk], [P(ky)])
                            mm(psb[ky][:, 0:128], VtmZ4[:, c, hh, :], AM3[:, hh, 384:512], False, hh == 1, ["VtmZ", AMk], [P(ky)])
                        cp("act", YT[:, c * 128:(c + 1) * 128], psb[ky][:, 0:128], [P(ky)], ["YT"])
                    chk("e7")
                    cp("pool", yb, YT, ["YT"], ["yb"])
                    k = ps()
                    mm(psb[k][:], blk, yb, True, True, ["yb", "constb"], [P(k)])
                    stt(cen, psb[k][:], -1.0 / 64, YT, ALU.mult, ALU.add, [P(k), "YT"], ["cen"])
                    tt("pool", sq2, cen, cen, ALU.mult, ["cen"], ["sq2"])
                    k = ps()
                    mm(psb[k][:], blk, sq2, True, True, ["sq2", "constb"], [P(k)])
                    act(sd, psb[k][:], AF.Ln, [P(k)], ["sd"], bias=64e-5, scale=1.0 / 64)
                    act(sd, sd, AF.Exp, ["sd"], ["sd"], scale=-0.5)
                    tt("dve", cen, cen, sd, ALU.mult, ["cen", "sd"], ["cen"])
                    ts("dve", cen, cen, cols[:, CV_LG + i:CV_LG + i + 1], ALU.mult, ["cen", "cols"], ["cen"], s2=cols[:, CV_LB + i:CV_LB + i + 1], op1=ALU.add)
                    tt("pool", cen, cen, bonus4[:, sl], ALU.add, ["cen", "bonus4"], ["cen"])
                    tt("dve", mix3[:, 4 + i, sl], cen, gT4[:, sl], ALU.mult, ["cen", "gT4"], ["mixT"])
            if stop == 'pr':
                S.barrier(); A.reset(); dbgf = A.f32(S_LEN)
                for cc in range(8):
                    cp("dve", dbgf, mix3[:, cc, :], ["mixT"], ["dbgf"])
                    dma("sp", dbg_mix[:, cc * S_LEN:(cc + 1) * S_LEN], dbgf, reads=["dbgf"])
                S.barrier(); S.emit(); return nc
            if dbg and s == 0:
                S.barrier()
                A.reset()
                dbgf = A.f32(S_LEN)
                for cc in range(8):
                    cp("dve", dbgf, mix3[:, cc, :], ["mixT"], ["dbgf"])
                    dma("sp", dbg_mix[:, cc * S_LEN:(cc + 1) * S_LEN], dbgf, reads=["dbgf"])

            S.barrier()
            A.reset()
            wo = A.bf(8 * D)
            wo3 = wo.rearrange("p (c n) -> p c n", n=D)
            for kc in range(8):
                dma("pool", wo3[:, kc, :], w_out[kc * 128:(kc + 1) * 128, :], writes=["wo%d" % kc])
            WO = ["wo%d" % kc for kc in range(8)]
            gbc2 = A.f32(D)
            dma("sp", gbc2, g2v[0, :].partition_broadcast(128), writes=["gbc"])
            x1 = A.f32(8 * D)
            x13 = x1.rearrange("p (t n) -> p t n", n=D)
            xn2T = A.bf(8 * 1024)
            xn23 = xn2T.rearrange("p (c t) -> p c t", t=1024)
            hT = A.bf(11 * 1024)
            hT3 = hT.rearrange("p (f t) -> p f t", t=1024)
            wd = A.bf(11 * D)
            wd3 = wd.rearrange("p (f n) -> p f n", n=D)
            wgs = [A.bf(8 * 128) for _ in range(2)]
            wus = [A.bf(8 * 128) for _ in range(2)]
            scr2 = A.bf(D)
            sms2 = [A.f32(4) for _ in range(2)]
            xsb2 = [A.bf(D) for _ in range(2)]
            sgs = [A.f32(512) for _ in range(2)]
            for half in range(2):
                ht0 = tok0 + half * 1024
                for tt_ in range(8):
                    t0 = half * 1024 + tt_ * 128
                    xk = "x1_%d" % tt_
                    dma("sp", x13[:, tt_, :], x[ht0 + tt_ * 128: ht0 + (tt_ + 1) * 128, :], writes=[xk])
                    for nh in range(2):
                        k = ps()
                        for kc in range(8):
                            mm(psb[k][:], mix3[:, kc, t0:t0 + 128], wo3[:, kc, nh * 512:(nh + 1) * 512], kc == 0, kc == 7, ["mixT"] + WO, [P(k)])
                        tt("dve", x13[:, tt_, nh * 512:(nh + 1) * 512], x13[:, tt_, nh * 512:(nh + 1) * 512], psb[k][:], ALU.add, [xk, P(k)], [xk])
                    b = tt_ % 2
                    rms_to_T(x13[:, tt_, :], xk, gbc2, xn23, "xn2T", tt_ * 128, scr2, "scr2", sms2[b], "sm2%d" % b, xsb2[b], "xsb2%d" % b)
                for fh in range(2):
                    for fi in range(11):
                        f = fh * 11 + fi
                        dma("pool", wd3[:, fi, :], w_down[f * 128:(f + 1) * 128, :], writes=["wd%d" % fi])
                    for fi in range(11):
                        f = fh * 11 + fi
                        b = fi % 2
                        wg3 = wgs[b].rearrange("p (c n) -> p c n", n=128)
                        wu3 = wus[b].rearrange("p (c n) -> p c n", n=128)
                        dma("pool", wg3, w_gate[:, f * 128:(f + 1) * 128].rearrange("(c p) n -> p c n", p=128), writes=["wg%d" % b])
                        dma("pool", wu3, w_up[:, f * 128:(f + 1) * 128].rearrange("(c p) n -> p c n", p=128), writes=["wu%d" % b])
                        for tg in range(2):
                            kg = ps()
                            ku = ps()
                            for kc in range(8):
                                mm(psb[kg][:], wg3[:, kc, :], xn23[:, kc, tg * 512:(tg + 1) * 512], kc == 0, kc == 7, ["wg%d" % b, "xn2T"], [P(kg)])
                            for kc in range(8):
                                mm(psb[ku][:], wu3[:, kc, :], xn23[:, kc, tg * 512:(tg + 1) * 512], kc == 0, kc == 7, ["wu%d" % b, "xn2T"], [P(ku)])
                            sb_ = tg
                            act(sgs[sb_], psb[kg][:], AF.Silu, [P(kg)], ["sg%d" % sb_])
                            tt("dve", hT3[:, fi, tg * 512:(tg + 1) * 512], sgs[sb_], psb[ku][:], ALU.mult, ["sg%d" % sb_, P(ku)], ["hT%d" % fi])
                    for tt_ in range(8):
                        xk = "x1_%d" % tt_
                        for nh in range(2):
                            k = ps()
                            for fi in range(11):
                                mm(psb[k][:], hT3[:, fi, tt_ * 128:(tt_ + 1) * 128], wd3[:, fi, nh * 512:(nh + 1) * 512], fi == 0, fi == 10, ["hT%d" % fi, "wd%d" % fi], [P(k)])
                            tt("dve", x13[:, tt_, nh * 512:(nh + 1) * 512], x13[:, tt_, nh * 512:(nh + 1) * 512], psb[k][:], ALU.add, [xk, P(k)], [xk])
                        if fh == 1:
                            dma("sp", out[ht0 + tt_ * 128: ht0 + (tt_ + 1) * 128, :], x13[:, tt_, :], reads=[xk])
        S.barrier()
        S.emit()
    return nc


_NC_CACHE = {}


def kernel(**inputs):
    inp = {k: np.asarray(v) for k, v in inputs.items()}
    ncores = 8
    nseq = 2
    if "nc" not in _NC_CACHE:
        _NC_CACHE["nc"] = build(nseq)
    nc = _NC_CACHE["nc"]
    x = np.ascontiguousarray(inp["x"], dtype=np.float32)
    B = x.shape[0]
    sq = lambda k: np.ascontiguousarray(inp[k][0], dtype=np.float32)
    shared = {
        "w_in": sq("w_in"), "w_out": sq("w_out"), "w_gate": sq("w_gate"), "w_up": sq("w_up"), "w_down": sq("w_down"),
        "norm1_g": sq("norm1_g").reshape(1, D), "norm2_g": sq("norm2_g").reshape(1, D),
        "w2": sq("w2"), "a2": sq("a2"), "g2": sq("g2"),
        "consts": make_consts(), "amask": make_amask(),
        "cols": make_cols({k: inp[k][0] for k in ("rwkv_mu", "w0", "a0", "k_k", "k_a", "r_k", "lnx_g", "lnx_b", "q_norm_g", "k_norm_g", "attn_out_g")}),
    }
    in_maps = []
    for c in range(ncores):
        m = dict(shared)
        m["x"] = x[c * nseq:(c + 1) * nseq].reshape(nseq * S_LEN, D)
        in_maps.append(m)
    res = run_bass_kernel_spmd(nc, in_maps, core_ids=list(range(ncores)))
    outs = [np.asarray(r["out"]).reshape(nseq, S_LEN, D) for r in res.results]
    return np.concatenate(outs, axis=0).astype(np.float32)
```

```python
import contextlib
import numpy as np
import concourse.bass as bass
import concourse.mybir as mybir
from concourse.bass_utils import run_bass_kernel_spmd

F32 = mybir.dt.float32
BF16 = mybir.dt.bfloat16
ALU = mybir.AluOpType
AF = mybir.ActivationFunctionType
AX = mybir.AxisListType
COMPUTE = ("pe", "act", "dve", "pool")

S_LEN = 2048
D = 1024
DFF = 2816
C0 = float(np.exp(-0.5))


class Sched:
    def __init__(self, nc, stack, n_dma_sems=24):
        self.nc = nc
        self.eng_names = ["pe", "act", "dve", "pool", "sp"]
        self.ops = {e: [] for e in self.eng_names}
        self.sem = {e: stack.enter_context(nc.semaphore("s_" + e)) for e in COMPUTE}
        self.cnt = {e: 0 for e in COMPUTE}
        self.dma_sems = [stack.enter_context(nc.semaphore("s_dma%d" % i)) for i in range(n_dma_sems)]
        self.dma_val = [0] * n_dma_sems
        self.dma_rr = 0
        self.res_w = {}
        self.res_r = {}
        self.waited = {e: {} for e in self.eng_names}

    def _need(self, eng, waits, ev):
        sid, sh, val = ev
        if self.waited[eng].get(sid, 0) >= val:
            return
        cur = waits.get(sid)
        if cur is None or cur[1] < val:
            waits[sid] = (sh, val)

    def op(self, eng, fn, reads=(), writes=(), dma=False):
        waits = {}
        own = None if dma else ("c", eng)
        for k in reads:
            w = self.res_w.get(k)
            if w is not None:
                self._need(eng, waits, w)
            if isinstance(k, tuple) and k[0] == "ps":
                for r in self.res_r.get(k, ()):
                    if r[0] != own:
                        self._need(eng, waits, r)
        for k in writes:
            w = self.res_w.get(k)
            if w is not None and (dma or w[0] != own):
                self._need(eng, waits, w)
            for r in self.res_r.get(k, ()):
                if dma or r[0] != own:
                    self._need(eng, waits, r)
        if dma:
            k = self.dma_rr
            self.dma_rr = (self.dma_rr + 1) % len(self.dma_sems)
            if self.dma_val[k] > 0:
                self._need(eng, waits, (("d", k), self.dma_sems[k], self.dma_val[k]))
            self.dma_val[k] += 16
            ev = (("d", k), self.dma_sems[k], self.dma_val[k])
            inc = 16
        else:
            self.cnt[eng] += 1
            ev = (("c", eng), self.sem[eng], self.cnt[eng])
            inc = 1
        for sid, (sh, val) in waits.items():
            self.waited[eng][sid] = val
        self.ops[eng].append((fn, list(waits.values()), ev[1], inc))
        for k in reads:
            self.res_r.setdefault(k, []).append(ev)
        for k in writes:
            self.res_w[k] = ev
            self.res_r[k] = []
        return ev

    def barrier(self):
        for eng in self.eng_names:
            waits = {}
            for e2 in COMPUTE:
                if self.cnt[e2] > 0 and e2 != eng:
                    self._need(eng, waits, (("c", e2), self.sem[e2], self.cnt[e2]))
            for k, v in enumerate(self.dma_val):
                if v > 0:
                    self._need(eng, waits, (("d", k), self.dma_sems[k], v))
            for sid, (sh, val) in waits.items():
                self.waited[eng][sid] = val
            self.ops[eng].append((None, list(waits.values()), None, 0))
        self.res_w = {}
        self.res_r = {}

    def emit(self):
        sched = self

        def run(engname, engine):
            for fn, waits, sh, inc in sched.ops[engname]:
                for (wsh, val) in waits:
                    engine.wait_ge(wsh, val)
                if fn is not None:
                    fn(engine).then_inc(sh, inc)

        with self.nc.Block() as block:
            @block.tensor
            def _(e):
                run("pe", e)

            @block.scalar
            def _(e):
                run("act", e)

            @block.vector
            def _(e):
                run("dve", e)

            @block.gpsimd
            def _(e):
                run("pool", e)

            @block.sync
            def _(e):
                run("sp", e)


class Arena:
    def __init__(self, t, n):
        self.t, self.n, self.off = t, n, 0

    def reset(self):
        self.off = 0

    def bf(self, n):
        n = (n + 1) // 2 * 2
        a = self.t[:, self.off:self.off + n]
        self.off += n
        assert self.off <= self.n, ("arena overflow", self.off, self.n)
        return a

    def f32(self, n):
        return self.bf(2 * n).bitcast(F32)


NC_ID, NC_BLK, NC_M4, NC_ML4, NC_I2, NC_RST, NC_WN = 0, 128, 256, 768, 1280, 1536, 2048
NCONST = 2048 + 64


def make_consts():
    c = np.zeros((128, NCONST), np.float32)
    c[:, NC_ID:NC_ID + 128] = np.eye(128)
    c[0:64, NC_BLK:NC_BLK + 64] = 1.0
    c[64:128, NC_BLK + 64:NC_BLK + 128] = 1.0
    p = np.arange(128)[:, None]
    f = np.arange(128)[None, :]
    mus = (p < f).astype(np.float32)
    mui = (p <= f).astype(np.float32)
    mls = (p > f).astype(np.float32)
    c[:, NC_M4:NC_M4 + 512] = np.concatenate([mus, mui, mus, mui], 1)
    c[:, NC_ML4:NC_ML4 + 512] = np.concatenate([mls] * 4, 1)
    c[:, NC_I2:NC_I2 + 256] = np.concatenate([np.eye(128)] * 2, 1)
    r = np.ones((128, 512), np.float32)
    r[:, 0::128] = 0.0
    c[:, NC_RST:NC_RST + 512] = r
    c[0:64, NC_WN:NC_WN + 64] = 1.0 / 64
    c[64, NC_WN:NC_WN + 64] = 1e-6
    return c


def make_amask():
    slopes = np.exp2(-8.0 * np.arange(1, 9, dtype=np.float64) / 8)
    ki = np.arange(128)[:, None]
    cc = np.arange(2048)[None, :]
    dl = cc - ki
    m = ((dl >= 0) & (dl <= 128)).astype(np.float64) + ((dl >= 0) & (dl % 4 == 0) & (dl <= 512)) + ((dl >= 0) & (dl % 16 == 0))
    out = np.zeros((8, 128, 2048), np.float32)
    for h in range(8):
        out[h] = m * np.exp(-slopes[h] * np.maximum(dl, 0))
    return out


CV_MU, CV_W0, CV_A0, CV_KK, CV_KA, CV_RK, CV_LG, CV_LB, CV_GQ, CV_GK, CV_GA = 0, 14, 18, 22, 26, 30, 34, 38, 42, 43, 44
NCV = 52


def make_cols(inp):
    cv = np.zeros((128, NCV), np.float32)

    def put(off, v):
        v = np.asarray(v, np.float32).reshape(-1, 128)
        cv[:, off:off + v.shape[0]] = v.T

    put(CV_MU, inp["rwkv_mu"])
    put(CV_W0, inp["w0"])
    put(CV_A0, inp["a0"])
    put(CV_KK, inp["k_k"])
    put(CV_KA, inp["k_a"])
    put(CV_RK, inp["r_k"])
    put(CV_LG, inp["lnx_g"])
    put(CV_LB, inp["lnx_b"])
    put(CV_GQ, np.tile(np.asarray(inp["q_norm_g"], np.float32).reshape(-1), 2))
    put(CV_GK, np.tile(np.asarray(inp["k_norm_g"], np.float32).reshape(-1), 2))
    ga = np.asarray(inp["attn_out_g"], np.float32).reshape(8, 64)
    cv[0:64, CV_GA:CV_GA + 8] = ga.T
    return cv


class StopBuild(Exception):
    pass


def build(NSEQ=2, dbg=False, stop=None):
    try:
        return _build(NSEQ, dbg, stop)
    except StopBuild as e:
        return e.args[0]


def _build(NSEQ=2, dbg=False, stop=None):
    nc = bass.Bass("TRN2", target_bir_lowering=False)
    NT = NSEQ * S_LEN

    def din(name, shape):
        return nc.dram_tensor(name, shape, F32, kind="ExternalInput").ap()

    x = din("x", [NT, D])
    w_in = din("w_in", [D, 3328])
    w_out = din("w_out", [D, D])
    w_gate = din("w_gate", [D, DFF])
    w_up = din("w_up", [D, DFF])
    w_down = din("w_down", [DFF, D])
    g1v = din("norm1_g", [1, D])
    g2v = din("norm2_g", [1, D])
    w2d = din("w2", [64, 512])
    a2d = din("a2", [64, 512])
    g2d = din("g2", [128, 512])
    consts_d = din("consts", [128, NCONST])
    amask_d = din("amask", [8, 128, 2048])
    cols_d = din("cols", [128, NCV])
    out = nc.dram_tensor("out", [NT, D], F32, kind="ExternalOutput").ap()
    if dbg:
        dbg_mix = nc.dram_tensor("dbg_mix", [128, 8 * S_LEN], F32, kind="ExternalOutput").ap()

    with contextlib.ExitStack() as st:
        S = Sched(nc, st)
        T = lambda n, s, d: st.enter_context(nc.sbuf_tensor(n, s, d))
        psb = [st.enter_context(nc.psum_tensor("ps%d" % i, [128, 512], F32)) for i in range(8)]
        rr = {"i": 0}

        def ps(pool=(0, 1, 2, 3, 4, 5, 6, 7)):
            k = pool[rr["i"] % len(pool)]
            rr["i"] += 1
            return k

        ARN = 86000
        arena_t = T("arena", [128, ARN], BF16)
        A = Arena(arena_t, ARN)
        mixT = T("mixT", [128, 8 * S_LEN], BF16)
        mix3 = mixT[:].rearrange("p (c t) -> p c t", t=S_LEN)
        constb = T("constb", [128, NCONST], BF16)
        constf = T("constf", [128, 512], F32)
        cols = T("cols_sb", [128, NCV], F32)
        gq8 = T("gq8", [128, 1], F32)
        identb = constb[:, NC_ID:NC_ID + 128]
        blk = constb[:, NC_BLK:NC_BLK + 128]

        def dma(eng, o, i, reads=(), writes=()):
            S.op(eng, lambda e: e.dma_start(out=o, in_=i), reads, writes, dma=True)

        def mm(o, l, r, start, stop, reads, writes):
            S.op("pe", lambda e: e.matmul(o, lhsT=l, rhs=r, start=start, stop=stop, skip_group_check=True), reads, writes)

        def tr(o, i, reads, writes):
            S.op("pe", lambda e: e.transpose(o, i, identb), reads, writes)

        def act(o, i, func, reads, writes, bias=0.0, scale=1.0, accum=None):
            if accum is None:
                S.op("act", lambda e: e.activation(out=o, in_=i, func=func, bias=bias, scale=scale), reads, writes)
            else:
                S.op("act", lambda e: e.activation(out=o, in_=i, func=func, bias=bias, scale=scale, accum_out=accum), reads, writes)

        def tt(eng, o, a, b, op, reads, writes):
            S.op(eng, lambda e: e.tensor_tensor(out=o, in0=a, in1=b, op=op), reads, writes)

        def ts(eng, o, a, s1, op0, reads, writes, s2=None, op1=None):
            if op1 is None:
                S.op(eng, lambda e: e.tensor_scalar(out=o, in0=a, scalar1=s1, scalar2=None, op0=op0), reads, writes)
            else:
                S.op(eng, lambda e: e.tensor_scalar(out=o, in0=a, scalar1=s1, scalar2=s2, op0=op0, op1=op1), reads, writes)

        def stt(o, a, s, b, op0, op1, reads, writes):
            S.op("dve", lambda e: e.scalar_tensor_tensor(out=o, in0=a, scalar=s, in1=b, op0=op0, op1=op1), reads, writes)

        def cp(eng, o, i, reads, writes):
            if eng == "act":
                act(o, i, AF.Copy, reads, writes)
            else:
                S.op(eng, lambda e: e.tensor_copy(out=o, in_=i), reads, writes)

        def recip(o, i, reads, writes):
            S.op("dve", lambda e: e.reciprocal(out=o, in_=i), reads, writes)

        def P(k):
            return ("ps", k)

        def chk(name):
            if stop == name:
                S.barrier(); S.emit()
                raise StopBuild(nc)

        dma("pool", constb[:], consts_d, writes=["constb"])
        dma("sp", constf[:], consts_d[:, NC_RST:NC_RST + 512], writes=["constf"])
        dma("sp", cols[:], cols_d, writes=["cols"])
        ts("dve", gq8[:], cols[:, CV_GQ:CV_GQ + 1], 0.125, ALU.mult, ["cols"], ["gq8"])

        def rms_to_T(xt, xt_key, gbc, dstT, dst_key, t0, scr, scr_key, sm, sm_key, xsb, xsb_key):
            act(scr, xt, AF.Square, [xt_key], [scr_key, sm_key], accum=sm[:, 0:1])
            ts("dve", sm[:, 1:2], sm[:, 0:1], 1.0 / D, ALU.mult, [sm_key], [sm_key], s2=1e-6, op1=ALU.add)
            act(sm[:, 2:3], sm[:, 1:2], AF.Sqrt, [sm_key], [sm_key])
            recip(sm[:, 3:4], sm[:, 2:3], [sm_key], [sm_key])
            stt(xsb, xt, sm[:, 3:4], gbc, ALU.mult, ALU.mult, [xt_key, sm_key, "gbc"], [xsb_key])
            k = ps()
            pv = psb[k][:].bitcast(BF16)
            for kc in range(8):
                tr(pv[:, kc * 128:(kc + 1) * 128], xsb[:, kc * 128:(kc + 1) * 128], [xsb_key, "constb"], [P(k)])
            cp("act", dstT[:, :, t0:t0 + 128], pv.rearrange("p (c t) -> p c t", t=128), [P(k)], [dst_key])

        for s in range(NSEQ):
            tok0 = s * S_LEN
            S.barrier()
            A.reset()
            xnT = A.bf(8 * S_LEN)
            xn3 = xnT.rearrange("p (c t) -> p c t", t=S_LEN)
            mark_pa = A.off
            gbc = A.f32(D)
            dma("sp", gbc, g1v[0, :].partition_broadcast(128), writes=["gbc"])
            xts = [A.f32(D) for _ in range(2)]
            scr = A.bf(D)
            sms = [A.f32(4) for _ in range(2)]
            xsbs = [A.bf(D) for _ in range(2)]
            wqkv = A.bf(8 * 1536)
            wq3 = wqkv.rearrange("p (c n) -> p c n", n=1536)
            for kc in range(8):
                dma("pool", wq3[:, kc, :], w_in[kc * 128:(kc + 1) * 128, 0:1536], writes=["wqkv%d" % kc])
            amask = A.bf(8 * 2048)
            am3 = amask.rearrange("p (h c) -> p h c", c=2048)
            for h in range(8):
                dma("pool", am3[:, h, :], amask_d[h], writes=["amask%d" % h])
            for ti in range(16):
                b = ti % 2
                dma("sp", xts[b], x[tok0 + ti * 128: tok0 + (ti + 1) * 128, :], writes=["xt%d" % b])
                rms_to_T(xts[b], "xt%d" % b, gbc, xn3, "xnT", ti * 128, scr, "scr", sms[b], "sm%d" % b, xsbs[b], "xsb%d" % b)
            if stop in ('p0', 'p0d'):
                if stop == 'p0d':
                    S.barrier(); A.reset(); dbgf = A.f32(S_LEN)
                    for cc in range(8):
                        cp("dve", dbgf, mix3[:, cc, :], ["mixT"], ["dbgf"])
                        dma("sp", dbg_mix[:, cc * S_LEN:(cc + 1) * S_LEN], dbgf, reads=["dbgf"])
                S.barrier(); S.emit(); return nc
            WQ = ["wqkv%d" % kc for kc in range(8)]
            vaug = A.bf(16 * 8 * 65)
            va4 = vaug.rearrange("p (j h e) -> p j h e", h=8, e=65)
            S.op("pool", lambda e: e.memset(vaug, 1.0), (), ["vaug"])
            for j in range(16):
                k = ps((0, 1, 2, 3))
                for kc in range(8):
                    mm(psb[k][:], xn3[:, kc, j * 128:(j + 1) * 128], wq3[:, kc, 1024:1536], kc == 0, kc == 7, ["xnT"] + WQ, [P(k)])
                cp("act" if j % 2 else "dve", va4[:, j, :, 0:64], psb[k][:].rearrange("p (h e) -> p h e", e=64), [P(k)], ["vaug"])
            if stop == 'v':
                S.barrier(); S.emit(); return nc
            qts = [A.bf(S_LEN) for _ in range(2)]
            kts = [A.bf(S_LEN) for _ in range(2)]
            sqs = [A.bf(512) for _ in range(4)]
            rss = [A.f32(512) for _ in range(4)]
            ebs = [A.bf(512) for _ in range(4)]
            pbs = [A.bf(512) for _ in range(4)]
            wn = constb[0:65, NC_WN:NC_WN + 64]
            cnt = 0
            for i in range(4):
                qt, kt = qts[i % 2], kts[i % 2]
                qk, kk_ = "qt%d" % (i % 2), "kt%d" % (i % 2)
                jobs = []
                for which, dst, dkey, gcol, c0 in ((0, qt, qk, gq8[:, 0:1], 0), (1, kt, kk_, cols[:, CV_GK:CV_GK + 1], 512)):
                    for tg in range(4):
                        jobs.append((dst, dkey, gcol, c0, tg))
                live = {}
                for t in range(len(jobs) + 1):
                    if t < len(jobs):
                        dst, dkey, gcol, c0, tg = jobs[t]
                        k = ps((0, 1, 2, 3))
                        for kc in range(8):
                            mm(psb[k][:], wq3[:, kc, c0 + 128 * i: c0 + 128 * (i + 1)], xn3[:, kc, tg * 512:(tg + 1) * 512], kc == 0, kc == 7, ["xnT"] + WQ, [P(k)])
                        b = t % 4
                        act(sqs[b], psb[k][:], AF.Square, [P(k)], ["sq%d" % b])
                        live[t] = (k, b)
                    if t >= 1:
                        dst, dkey, gcol, c0, tg = jobs[t - 1]
                        k, b = live.pop(t - 1)
                        k2 = ps((0, 1, 2, 3))
                        mm(psb[k2][:], blk, sqs[b], True, True, ["sq%d" % b, "constb"], [P(k2)])
                        act(rss[b], psb[k2][:], AF.Ln, [P(k2)], ["rs%d" % b], bias=1e-6, scale=1.0 / 64)
                        act(rss[b], rss[b], AF.Exp, ["rs%d" % b], ["rs%d" % b], scale=-0.5)
                        stt(dst[:, tg * 512:(tg + 1) * 512], psb[k][:], gcol, rss[b], ALU.mult, ALU.mult, [P(k), "rs%d" % b, "gq8", "cols"], [dkey])
                for hh in range(2):
                    h = 2 * i + hh
                    pb = 64 * hh
                    ob = (4, 5, 6, 7)
                    ec = 0
                    groups = []
                    for j in range(16):
                        for g in range(j // 4, 4):
                            cst = max(128 * j, 512 * g)
                            groups.append((j, g, cst, 512 * (g + 1)))
                    LA = 3
                    for t in range(len(groups) + LA):
                        if t < len(groups):
                            j, g, cst, cen = groups[t]
                            n = cen - cst
                            k = ps((0, 1, 2, 3))
                            mm(psb[k][:, 0:n], kt[pb:pb + 64, j * 128:(j + 1) * 128], qt[pb:pb + 64, cst:cen], True, True, [qk, kk_], [P(k)])
                            b = t % 4
                            act(ebs[b][:, 0:n], psb[k][:, 0:n], AF.Exp, [P(k)], ["eb%d" % b])
                            tt("pool" if t % 2 else "dve", pbs[b][:, 0:n], ebs[b][:, 0:n], am3[:, h, cst - 128 * j: cen - 128 * j], ALU.mult, ["eb%d" % b, "amask%d" % h], ["pb%d" % b])
                        if t >= LA:
                            j, g, cst, cen = groups[t - LA]
                            n = cen - cst
                            b = (t - LA) % 4
                            mm(psb[ob[g]][0:65, cst - 512 * g: cen - 512 * g], va4[:, j, h, :], pbs[b][:, 0:n], j == 0, j == 4 * g + 3, ["vaug", "pb%d" % b], [P(ob[g])])
                    k2s = []
                    for g in range(4):
                        act(sqs[g][0:65, :], psb[ob[g]][0:65, :], AF.Square, [P(ob[g])], ["sq%d" % g])
                    for g in range(4):
                        k2 = ps((0, 1, 2, 3))
                        k2s.append(k2)
                        mm(psb[k2][0:64, :], wn, sqs[g][0:65, :], True, True, ["sq%d" % g, "constb"], [P(k2)])
                    for g in range(4):
                        act(rss[g][0:64, :], psb[k2s[g]][0:64, :], AF.Ln, [P(k2s[g])], ["rs%d" % g])
                    for g in range(4):
                        act(rss[g][0:64, :], rss[g][0:64, :], AF.Exp, ["rs%d" % g], ["rs%d" % g], scale=-0.5)
                    for g in range(4):
                        stt(mix3[pb:pb + 64, i, g * 512:(g + 1) * 512], psb[ob[g]][0:64, :], cols[0:64, CV_GA + h:CV_GA + h + 1], rss[g][0:64, :], ALU.mult, ALU.mult,
                            [P(ob[g]), "rs%d" % g, "cols"], ["mixT"])

            if stop == 'pa':
                S.barrier(); A.reset(); dbgf = A.f32(S_LEN)
                for cc in range(8):
                    cp("dve", dbgf, mix3[:, cc, :], ["mixT"], ["dbgf"])
                    dma("sp", dbg_mix[:, cc * S_LEN:(cc + 1) * S_LEN], dbgf, reads=["dbgf"])
                S.barrier(); S.emit(); return nc
            S.barrier()
            A.off = mark_pa
            wr = A.bf(8 * 1792)
            wr3 = wr.rearrange("p (c n) -> p c n", n=1792)
            for kc in range(8):
                dma("pool", wr3[:, kc, :], w_in[kc * 128:(kc + 1) * 128, 1536:3328], writes=["wr%d" % kc])
            WR = ["wr%d" % kc for kc in range(8)]
            w2a2 = A.bf(512)
            g2b = A.bf(512)
            dma("pool", w2a2[0:64, :], w2d, writes=["w2a2a"])
            dma("pool", w2a2[64:128, :], a2d, writes=["w2a2b"])
            dma("pool", g2b, g2d, writes=["g2b"])
            lora1 = A.bf(S_LEN)
            lora2 = A.bf(S_LEN)
            halo = A.f32(16)
            ptsb = [A.f32(514) for _ in range(2)]
            dtmp = [A.f32(512) for _ in range(2)]
            M4 = constb[:, NC_M4:NC_M4 + 512]
            ML2 = constb[:, NC_ML4:NC_ML4 + 256]
            I2 = constb[:, NC_I2:NC_I2 + 256]
            pcount = [0]

            def proj_shift(cj, tg, dst_fn):
                k = ps()
                for kc in range(8):
                    mm(psb[k][:], wr3[:, kc, cj * 128:(cj + 1) * 128], xn3[:, kc, tg * 512:(tg + 1) * 512], kc == 0, kc == 7, ["xnT"] + WR, [P(k)])
                b = pcount[0] % 2
                pcount[0] += 1
                pk = "ptsb%d" % b
                if tg == 0:
                    S.op("pool", lambda e: e.memset(ptsb[b][:, 0:1], 0.0), (), [pk + "h"])
                else:
                    cp("pool", ptsb[b][:, 0:1], halo[:, cj:cj + 1], ["halo%d" % cj], [pk + "h"])
                cp("act", ptsb[b][:, 1:513], psb[k][:], [P(k)], [pk])
                cp("pool", halo[:, cj:cj + 1], ptsb[b][:, 512:513], [pk], ["halo%d" % cj])
                tt("pool", dtmp[b], ptsb[b][:, 0:512], ptsb[b][:, 1:513], ALU.subtract, [pk, pk + "h"], ["dtmp%d" % b])
                dst_fn(dtmp[b], cols[:, CV_MU + cj:CV_MU + cj + 1], ptsb[b][:, 1:513], ["dtmp%d" % b, pk, "cols"])

            A_l12 = A.f32(512)
            for tg in range(4):
                sl = slice(tg * 512, (tg + 1) * 512)
                tmpx = dtmp

                def f12(d, mu, p, rd, sl=sl):
                    xs = A_l12
                    stt(xs, d, mu, p, ALU.mult, ALU.add, rd, ["xs12"])
                    act(lora1[0:64, sl], xs[0:64, :], AF.Tanh, ["xs12"], ["lora1a"])
                    act(lora1[64:128, sl], xs[64:128, :], AF.Copy, ["xs12"], ["lora1b"])

                def f13(d, mu, p, rd, sl=sl):
                    xs = A_l12
                    stt(xs, d, mu, p, ALU.mult, ALU.add, rd, ["xs12"])
                    act(lora2[:, sl], xs, AF.Sigmoid, ["xs12"], ["lora2"])

                proj_shift(12, tg, f12)
                proj_shift(13, tg, f13)

            if stop == 'lora':
                S.barrier(); S.emit(); return nc
            rT, kT, vT = A.f32(512), A.f32(512), A.f32(512)
            sw, ia, gT4 = A.f32(512), A.f32(512), A.f32(S_LEN)
            Ls, D1, D2, kk0, kk, kp, bv = [A.f32(512) for _ in range(7)]
            E1, E2, E3, E4 = [A.f32(512) for _ in range(4)]
            rn = A.f32(512)
            sqb, rkr = A.bf(512), A.bf(512)
            bonus4 = A.f32(S_LEN)
            ART = A.bf(4 * 256)
            ART4 = ART.rearrange("p (c w t) -> p c w t", w=2, t=128)
            KBT = A.bf(4 * 256)
            KBT4 = KBT.rearrange("p (c w t) -> p c w t", w=2, t=128)
            kbarT, bbarT, vbT = A.bf(512), A.bf(512), A.bf(512)
            KBtm = A.bf(4 * 256)
            KBtm4 = KBtm.rearrange("p (c w n) -> p c w n", w=2, n=128)
            Vtm = A.bf(4 * 128)
            Vtm3 = Vtm.rearrange("p (c n) -> p c n", n=128)
            VtmZ = A.bf(4 * 256)
            VtmZ4 = VtmZ.rearrange("p (c h n) -> p c h n", h=2, n=128)
            sc = A.f32(8)
            sc3 = sc.rearrange("p (c w) -> p c w", w=2)
            AM3s = [A.bf(2 * 512).rearrange("p (h n) -> p h n", n=512) for _ in range(4)]
            NT3s = [A.bf(256).rearrange("p (h n) -> p h n", n=128) for _ in range(4)]
            ZT3s = [A.bf(512).rearrange("p (h n) -> p h n", n=256) for _ in range(4)]
            Wb, Ub = A.bf(128), A.bf(128)
            UbZ = A.bf(256)
            UbZ3 = UbZ.rearrange("p (h n) -> p h n", n=128)
            Hf = A.f32(64)
            HbP = A.bf(128)
            YT = A.f32(512)
            yb, sq2 = A.bf(512), A.bf(512)
            cen, sd = A.f32(512), A.f32(512)

            for i in range(4):
                S.op("pool", lambda e: e.memset(VtmZ, 0.0), (), ["VtmZ"])
                S.op("pool", lambda e: e.memset(UbZ, 0.0), (), ["UbZ"])
                S.op("pool", lambda e: e.memset(HbP, 0.0), (), ["HbP"])
                S.op("pool", lambda e: e.memset(Hf, 0.0), (), ["Hf"])
                for tg in range(4):
                    sl = slice(tg * 512, (tg + 1) * 512)

                    def mk(dst, key):
                        def f(d, mu, p, rd):
                            stt(dst, d, mu, p, ALU.mult, ALU.add, rd, [key])
                        return f

                    proj_shift(0 + i, tg, mk(rT, "rT"))
                    proj_shift(4 + i, tg, mk(kT, "kT"))
                    proj_shift(8 + i, tg, mk(vT, "vT"))
                    k = ps()
                    mm(psb[k][:], w2a2[0:64, 128 * i:128 * (i + 1)], lora1[0:64, sl], True, True, ["w2a2a", "lora1a"], [P(k)])
                    act(sw, psb[k][:], AF.Sigmoid, [P(k), "cols"], ["sw"], bias=cols[:, CV_W0 + i:CV_W0 + i + 1])
                    k = ps()
                    mm(psb[k][:], w2a2[64:128, 128 * i:128 * (i + 1)], lora1[64:128, sl], True, True, ["w2a2b", "lora1b"], [P(k)])
                    act(ia, psb[k][:], AF.Sigmoid, [P(k), "cols"], ["ia"], bias=cols[:, CV_A0 + i:CV_A0 + i + 1])
                    k = ps()
                    mm(psb[k][:], g2b[:, 128 * i:128 * (i + 1)], lora2[:, sl], True, True, ["g2b", "lora2"], [P(k)])
                    cp("act", gT4[:, sl], psb[k][:], [P(k)], ["gT4"])
                    S.op("dve", lambda e: e.tensor_tensor_scan(out=Ls, data0=constf[:], data1=sw, initial=0.0, op0=ALU.mult, op1=ALU.add), ["constf", "sw"], ["Ls"])
                    Ls3 = Ls.rearrange("p (c t) -> p c t", t=128)
                    tt("dve", D1.rearrange("p (c t) -> p c t", t=128), Ls3, Ls3[:, :, 63:64].to_broadcast([128, 4, 128]), ALU.subtract, ["Ls"], ["D1"])
                    act(E1, D1, AF.Exp, ["D1"], ["E1"], scale=-C0)
                    act(E3, D1, AF.Exp, ["D1"], ["E3"], scale=C0)
                    tt("dve", D2, D1, sw, ALU.subtract, ["D1", "sw"], ["D2"])
                    act(E2, D2, AF.Exp, ["D2"], ["E2"], scale=-C0)
                    tt("dve", D2.rearrange("p (c t) -> p c t", t=128), Ls3, Ls3[:, :, 127:128].to_broadcast([128, 4, 128]), ALU.subtract, ["Ls", "E2"], ["D2"])
                    act(E4, D2, AF.Exp, ["D2"], ["E4"], scale=C0)
                    act(sc3[:, :, 0:1], Ls3[:, :, 63:64], AF.Exp, ["Ls"], ["sc"], scale=-C0)
                    act(sc3[:, :, 1:2], Ls3[:, :, 127:128], AF.Exp, ["Ls"], ["sc"], scale=-C0)
                    chk("e1")
                    ts("dve", kk0, kT, cols[:, CV_KK + i:CV_KK + i + 1], ALU.mult, ["kT", "cols"], ["kk0"])
                    act(sqb, kk0, AF.Square, ["kk0"], ["sqb"])
                    k = ps()
                    mm(psb[k][:], blk, sqb, True, True, ["sqb", "constb"], [P(k)])
                    ts("dve", rn, psb[k][:], 1e-24, ALU.max, [P(k)], ["rn"])
                    act(rn, rn, AF.Ln, ["rn"], ["rn"])
                    act(rn, rn, AF.Exp, ["rn"], ["rn"], scale=-0.5)
                    tt("dve", kk, kk0, rn, ALU.mult, ["kk0", "rn"], ["kk"])
                    ts("dve", kp, ia, -1.0, ALU.add, ["ia", "cols"], ["kp"], s2=cols[:, CV_KA + i:CV_KA + i + 1], op1=ALU.mult)
                    stt(kp, kp, 1.0, kT, ALU.add, ALU.mult, ["kp", "kT"], ["kp"])
                    tt("pool", bv, kk, ia, ALU.mult, ["kk", "ia"], ["bv"])
                    c3 = lambda ap: ap.rearrange("p (c t) -> p c t", t=128)
                    tt("dve", ART4[:, :, 1, :], c3(rT), c3(E1), ALU.mult, ["rT", "E1"], ["ART"])
                    stt(kk0, kk, -1.0, E2, ALU.mult, ALU.mult, ["kk", "E2"], ["kk0"])
                    cp("act", ART4[:, :, 0, :], c3(kk0), ["kk0"], ["ART"])
                    tt("pool", KBT4[:, :, 1, :], c3(kp), c3(E3), ALU.mult, ["kp", "E3"], ["KBT"])
                    tt("dve", KBT4[:, :, 0, :], c3(bv), c3(E3), ALU.mult, ["bv", "E3"], ["KBT"])
                    tt("pool", kbarT, kp, E4, ALU.mult, ["kp", "E4"], ["kbarT"])
                    tt("dve", bbarT, bv, E4, ALU.mult, ["bv", "E4"], ["bbarT"])
                    cp("act", vbT, vT, ["vT"], ["vbT"])
                    stt(rkr, rT, cols[:, CV_RK + i:CV_RK + i + 1], kp, ALU.mult, ALU.mult, ["rT", "kp", "cols"], ["rkr"])
                    k = ps()
                    mm(psb[k][:], blk, rkr, True, True, ["rkr", "constb"], [P(k)])
                    tt("dve", bonus4[:, sl], psb[k][:], vT, ALU.mult, [P(k), "vT"], ["bonus4"])
                    chk("e2")
                    for c in range(4):
                        k = ps()
                        pv = psb[k][:].bitcast(BF16)
                        cs = slice(c * 128, (c + 1) * 128)
                        tr(pv[:, 0:128], kbarT[:, cs], ["kbarT", "constb"], [P(k)])
                        tr(pv[:, 128:256], bbarT[:, cs], ["bbarT", "constb"], [P(k)])
                        tr(pv[:, 256:384], vbT[:, cs], ["vbT", "constb"], [P(k)])
                        cp("act", KBtm4[:, c, :, :], pv[:, 0:256].rearrange("p (w n) -> p w n", n=128), [P(k)], ["KBtm"])
                        cp("dve", Vtm3[:, c, :], pv[:, 256:384], [P(k)], ["Vtm"])
                        cp("act", VtmZ4[:, c, 0, 0:64], pv[:, 256:320], [P(k)], ["VtmZ"])
                        cp("dve", VtmZ4[:, c, 1, 64:128], pv[:, 320:384], [P(k)], ["VtmZ"])
                    chk("e3")
                    I2v = I2.rearrange("p (h n) -> p h n", n=128)
                    for c in range(4):
                        AM3, NT3, ZT3 = AM3s[c], NT3s[c], ZT3s[c]
                        for hh in range(2):
                            pb = 64 * hh
                            kk_b = ps()
                            knh = ps()
                            rhs_ar = ART4[pb:pb + 64, c, :, :].rearrange("p w t -> p (w t)")
                            mm(psb[kk_b][:, 0:256], KBT4[pb:pb + 64, c, 0, :], rhs_ar, True, False, ["KBT", "ART"], [P(kk_b)])
                            mm(psb[kk_b][:, 256:512], KBT4[pb:pb + 64, c, 1, :], rhs_ar, False, True, ["KBT", "ART"], [P(kk_b)])
                            mm(psb[knh][:, 0:128], ART4[pb:pb + 64, c, 0, :], KBT4[pb:pb + 64, c, 0, :], True, True, ["KBT", "ART"], [P(knh)])
                            tt("dve", AM3[:, hh, :], psb[kk_b][:], M4, ALU.mult, [P(kk_b), "constb"], ["AM%d" % c])
                            tt("dve", NT3[:, hh, :], psb[knh][:, 0:128], ML2[:, 0:128], ALU.mult, [P(knh), "constb"], ["NT%d" % c])
                        cp("pool", ZT3[:, 0, 0:128], AM3[:, 0, 0:128], ["AM%d" % c], ["ZT%d" % c])
                        cp("pool", ZT3[:, 1, 0:128], AM3[:, 1, 0:128], ["AM%d" % c], ["ZT%d" % c])
                        tt("dve", ZT3[:, :, 128:256], AM3[:, :, 0:128], I2v, ALU.add, ["AM%d" % c, "constb"], ["ZT%d" % c])
                    for m in range(7):
                        banks = []
                        for c in range(4):
                            NT3, ZT3 = NT3s[c], ZT3s[c]
                            kz = ps()
                            kn2 = ps() if m < 6 else None
                            banks.append((kz, kn2))
                            rd, = [["NT%d" % c, "ZT%d" % c]]
                            if m == 0:
                                for hh in range(2):
                                    mm(psb[kz][:, 256 * hh:256 * hh + 128], NT3[:, hh, :], ZT3[:, hh, 0:128], hh == 0, hh == 1, rd, [P(kz)])
                            elif m < 6:
                                for hh in range(2):
                                    mm(psb[kz][:, 256 * hh:256 * hh + 256], NT3[:, hh, :], ZT3[:, hh, :], hh == 0, hh == 1, rd, [P(kz)])
                            else:
                                for hh in range(2):
                                    mm(psb[kz][:, 256 * hh + 128:256 * hh + 256], NT3[:, hh, :], ZT3[:, hh, 128:256], hh == 0, hh == 1, rd, [P(kz)])
                            if m < 6:
                                for hh in range(2):
                                    mm(psb[kn2][:, 128 * hh:128 * hh + 128], ZT3[:, hh, 0:128], NT3[:, hh, :], hh == 0, hh == 1, rd, [P(kn2)])
                        for c in range(4):
                            NT3, ZT3 = NT3s[c], ZT3s[c]
                            kz, kn2 = banks[c]
                            pz3 = psb[kz][:].rearrange("p (h n) -> p h n", n=256)
                            if m >= 1:
                                tt("dve", ZT3[:, :, 128:256], ZT3[:, :, 128:256], pz3[:, :, 128:256], ALU.add, [P(kz), "ZT%d" % c], ["ZT%d" % c])
                            if m < 6:
                                cp("act", ZT3[:, :, 0:128], pz3[:, :, 0:128], [P(kz)], ["ZT%d" % c])
                                cp("act" if c % 2 else "dve", NT3[:, :, :], psb[kn2][:, 0:256].rearrange("p (h n) -> p h n", n=128), [P(kn2)], ["NT%d" % c])
                    for c in range(4):
                        AM3, ZT3 = AM3s[c], ZT3s[c]
                        AMk, ZTk = "AM%d" % c, "ZT%d" % c
                        if not (tg == 0 and c == 0):
                            nsc = sc3[:, c, 0:1]
                            ts("dve", HbP[0:64, 0:64], Hf[0:64, :], nsc[0:64], ALU.mult, ["Hf", "sc"], ["HbP"])
                            ts("dve", HbP[64:128, 64:128], Hf[64:128, :], nsc[64:128], ALU.mult, ["Hf", "sc"], ["HbP"])
                        kw = ps()
                        mm(psb[kw][:, 0:128], ART4[:, c, 0, :], HbP, True, False, ["ART", "HbP"], [P(kw)])
                        for hh in range(2):
                            mm(psb[kw][:, 64 * hh:64 * hh + 64], AM3[:, hh, 256:384], Vtm3[:, c, 64 * hh:64 * hh + 64], False, hh == 1, [AMk, "Vtm"], [P(kw)])
                        cp("act", Wb, psb[kw][:, 0:128], [P(kw)], ["Wb"])
                        ku = ps()
                        for hh in range(2):
                            mm(psb[ku][:, 64 * hh:64 * hh + 64], ZT3[:, hh, 128:256], Wb[:, 64 * hh:64 * hh + 64], hh == 0, hh == 1, [ZTk, "Wb"], [P(ku)])
                        cp("act", Ub, psb[ku][:, 0:128], [P(ku)], ["Ub"])
                        kh = ps()
                        mm(psb[kh][:, 0:128], KBtm4[:, c, 1, :], Ub, True, False, ["KBtm", "Ub"], [P(kh)])
                        mm(psb[kh][:, 0:128], KBtm4[:, c, 0, :], Vtm3[:, c, :], False, True, ["KBtm", "Vtm"], [P(kh)])
                        ts("dve", Hf, Hf, sc3[:, c, 1:2], ALU.mult, ["Hf", "sc"], ["Hf"])
                        tt("dve", Hf[0:64, :], Hf[0:64, :], psb[kh][0:64, 0:64], ALU.add, ["Hf", P(kh)], ["Hf"])
                        tt("dve", Hf[64:128, :], Hf[64:128, :], psb[kh][64:128, 64:128], ALU.add, ["Hf", P(kh)], ["Hf"])
                        cp("pool", UbZ3[:, 0, 0:64], Ub[:, 0:64], ["Ub"], ["UbZ"])
                        cp("pool", UbZ3[:, 1, 64:128], Ub[:, 64:128], ["Ub"], ["UbZ"])
                        ky = ps()
                        mm(psb[ky][:, 0:128], HbP, ART4[:, c, 1, :], True, False, ["HbP", "ART"], [P(ky)])
                        for hh in range(2):
                            mm(psb[ky][:, 0:128], UbZ3[:, hh, :], AM3[:, hh, 128:256], False, False, ["UbZ", AMk], [P(ky)])
                            mm(psb[ky][:, 0:128], VtmZ4[:, c, hh, :], AM3[:, hh, 384:512], False, hh == 1, ["VtmZ", AMk], [P(ky)])
                        cp("act", YT[:, c * 128:(c + 1) * 128], psb[ky][:, 0:128], [P(ky)], ["YT"])
                    chk("e7")
                    cp("act", yb, YT, ["YT"], ["yb"])
                    k = ps()
                    mm(psb[k][:], blk, yb, True, True, ["yb", "constb"], [P(k)])
                    stt(cen, psb[k][:], -1.0 / 64, YT, ALU.mult, ALU.add, [P(k), "YT"], ["cen"])
                    act(sq2, cen, AF.Square, ["cen"], ["sq2"])
                    k = ps()
                    mm(psb[k][:], blk, sq2, True, True, ["sq2", "constb"], [P(k)])
                    act(sd, psb[k][:], AF.Ln, [P(k)], ["sd"], bias=64e-5, scale=1.0 / 64)
                    act(sd, sd, AF.Exp, ["sd"], ["sd"], scale=-0.5)
                    tt("dve", cen, cen, sd, ALU.mult, ["cen", "sd"], ["cen"])
                    ts("dve", cen, cen, cols[:, CV_LG + i:CV_LG + i + 1], ALU.mult, ["cen", "cols"], ["cen"], s2=cols[:, CV_LB + i:CV_LB + i + 1], op1=ALU.add)
                    tt("pool", cen, cen, bonus4[:, sl], ALU.add, ["cen", "bonus4"], ["cen"])
                    tt("dve", mix3[:, 4 + i, sl], cen, gT4[:, sl], ALU.mult, ["cen", "gT4"], ["mixT"])
            if stop == 'pr':
                S.barrier(); A.reset(); dbgf = A.f32(S_LEN)
                for cc in range(8):
                    cp("dve", dbgf, mix3[:, cc, :], ["mixT"], ["dbgf"])
                    dma("sp", dbg_mix[:, cc * S_LEN:(cc + 1) * S_LEN], dbgf, reads=["dbgf"])
                S.barrier(); S.emit(); return nc
            if dbg and s == 0:
                S.barrier()
                A.reset()
                dbgf = A.f32(S_LEN)
                for cc in range(8):
                    cp("dve", dbgf, mix3[:, cc, :], ["mixT"], ["dbgf"])
                    dma("sp", dbg_mix[:, cc * S_LEN:(cc + 1) * S_LEN], dbgf, reads=["dbgf"])

            S.barrier()
            A.reset()
            wo = A.bf(8 * D)
            wo3 = wo.rearrange("p (c n) -> p c n", n=D)
            for kc in range(8):
                dma("pool", wo3[:, kc, :], w_out[kc * 128:(kc + 1) * 128, :], writes=["wo%d" % kc])
            WO = ["wo%d" % kc for kc in range(8)]
            gbc2 = A.f32(D)
            dma("sp", gbc2, g2v[0, :].partition_broadcast(128), writes=["gbc"])
            x1 = A.f32(8 * D)
            x13 = x1.rearrange("p (t n) -> p t n", n=D)
            xn2T = A.bf(8 * 1024)
            xn23 = xn2T.rearrange("p (c t) -> p c t", t=1024)
            hT = A.bf(11 * 1024)
            hT3 = hT.rearrange("p (f t) -> p f t", t=1024)
            wd = A.bf(11 * D)
            wd3 = wd.rearrange("p (f n) -> p f n", n=D)
            wgs = [A.bf(8 * 128) for _ in range(2)]
            wus = [A.bf(8 * 128) for _ in range(2)]
            scr2 = A.bf(D)
            sms2 = [A.f32(4) for _ in range(2)]
            xsb2 = [A.bf(D) for _ in range(2)]
            sgs = [A.f32(512) for _ in range(2)]
            for half in range(2):
                ht0 = tok0 + half * 1024
                for tt_ in range(8):
                    t0 = half * 1024 + tt_ * 128
                    xk = "x1_%d" % tt_
                    dma("sp", x13[:, tt_, :], x[ht0 + tt_ * 128: ht0 + (tt_ + 1) * 128, :], writes=[xk])
                    for nh in range(2):
                        k = ps()
                        for kc in range(8):
                            mm(psb[k][:], mix3[:, kc, t0:t0 + 128], wo3[:, kc, nh * 512:(nh + 1) * 512], kc == 0, kc == 7, ["mixT"] + WO, [P(k)])
                        tt("dve", x13[:, tt_, nh * 512:(nh + 1) * 512], x13[:, tt_, nh * 512:(nh + 1) * 512], psb[k][:], ALU.add, [xk, P(k)], [xk])
                    b = tt_ % 2
                    rms_to_T(x13[:, tt_, :], xk, gbc2, xn23, "xn2T", tt_ * 128, scr2, "scr2", sms2[b], "sm2%d" % b, xsb2[b], "xsb2%d" % b)
                for fh in range(2):
                    for fi in range(11):
                        f = fh * 11 + fi
                        dma("pool", wd3[:, fi, :], w_down[f * 128:(f + 1) * 128, :], writes=["wd%d" % fi])
                    for fi in range(11):
                        f = fh * 11 + fi
                        b = fi % 2
                        wg3 = wgs[b].rearrange("p (c n) -> p c n", n=128)
                        wu3 = wus[b].rearrange("p (c n) -> p c n", n=128)
                        dma("pool", wg3, w_gate[:, f * 128:(f + 1) * 128].rearrange("(c p) n -> p c n", p=128), writes=["wg%d" % b])
                        dma("pool", wu3, w_up[:, f * 128:(f + 1) * 128].rearrange("(c p) n -> p c n", p=128), writes=["wu%d" % b])
                        for tg in range(2):
                            kg = ps()
                            ku = ps()
                            for kc in range(8):
                                mm(psb[kg][:], wg3[:, kc, :], xn23[:, kc, tg * 512:(tg + 1) * 512], kc == 0, kc == 7, ["wg%d" % b, "xn2T"], [P(kg)])
                            for kc in range(8):
                                mm(psb[ku][:], wu3[:, kc, :], xn23[:, kc, tg * 512:(tg + 1) * 512], kc == 0, kc == 7, ["wu%d" % b, "xn2T"], [P(ku)])
                            sb_ = tg
                            act(sgs[sb_], psb[kg][:], AF.Silu, [P(kg)], ["sg%d" % sb_])
                            tt("dve", hT3[:, fi, tg * 512:(tg + 1) * 512], sgs[sb_], psb[ku][:], ALU.mult, ["sg%d" % sb_, P(ku)], ["hT%d" % fi])
                    for tt_ in range(8):
                        xk = "x1_%d" % tt_
                        for nh in range(2):
                            k = ps()
                            for fi in range(11):
                                mm(psb[k][:], hT3[:, fi, tt_ * 128:(tt_ + 1) * 128], wd3[:, fi, nh * 512:(nh + 1) * 512], fi == 0, fi == 10, ["hT%d" % fi, "wd%d" % fi], [P(k)])
                            tt("dve", x13[:, tt_, nh * 512:(nh + 1) * 512], x13[:, tt_, nh * 512:(nh + 1) * 512], psb[k][:], ALU.add, [xk, P(k)], [xk])
                        if fh == 1:
                            dma("sp", out[ht0 + tt_ * 128: ht0 + (tt_ + 1) * 128, :], x13[:, tt_, :], reads=[xk])
        S.barrier()
        S.emit()
    return nc


_NC_CACHE = {}


def kernel(**inputs):
    inp = {k: np.asarray(v) for k, v in inputs.items()}
    ncores = 8
    nseq = 2
    if "nc" not in _NC_CACHE:
        _NC_CACHE["nc"] = build(nseq)
    nc = _NC_CACHE["nc"]
    x = np.ascontiguousarray(inp["x"], dtype=np.float32)
    B = x.shape[0]
    sq = lambda k: np.ascontiguousarray(inp[k][0], dtype=np.float32)
    shared = {
        "w_in": sq("w_in"), "w_out": sq("w_out"), "w_gate": sq("w_gate"), "w_up": sq("w_up"), "w_down": sq("w_down"),
        "norm1_g": sq("norm1_g").reshape(1, D), "norm2_g": sq("norm2_g").reshape(1, D),
        "w2": sq("w2"), "a2": sq("a2"), "g2": sq("g2"),
        "consts": make_consts(), "amask": make_amask(),
        "cols": make_cols({k: inp[k][0] for k in ("rwkv_mu", "w0", "a0", "k_k", "k_a", "r_k", "lnx_g", "lnx_b", "q_norm_g", "k_norm_g", "attn_out_g")}),
    }
    in_maps = []
    for c in range(ncores):
        m = dict(shared)
        m["x"] = x[c * nseq:(c + 1) * nseq].reshape(nseq * S_LEN, D)
        in_maps.append(m)
    res = run_bass_kernel_spmd(nc, in_maps, core_ids=list(range(ncores)))
    outs = [np.asarray(r["out"]).reshape(nseq, S_LEN, D) for r in res.results]
    return np.concatenate(outs, axis=0).astype(np.float32)
```
